# Optimizing a Trainium2 kernel written in Bass

```python
import jax, jax.numpy as jnp
from jax import lax
import numpy as np

D_MODEL = 1024
BATCH = 4
SEQ = 8192
DEPTH = 1

N_META = 16
D_MIX = D_MODEL
D_CONV = D_MIX // 2
D_POOL = D_MIX - D_CONV
CONV_HEADS = 8
CONV_WIDTH = 3
POOL_WINDOWS = (2, 4, 8, 16)
N_POOL_GROUPS = len(POOL_WINDOWS)
POOL_GROUP_DIM = D_POOL // N_POOL_GROUPS
D_IN_PROJ = 3 * D_CONV + D_POOL
N_EXPERT_GROUPS = 4
EXPERTS_PER_GROUP = 8
N_EXPERTS = N_EXPERT_GROUPS * EXPERTS_PER_GROUP
TOP_K = 2
D_EXPERT = D_MODEL // 4
EPS = 1e-6

kernel_name = 'hymba_conv_pool_hier_moe_layer'


def rmsnorm(x, g):
    xf = x.astype(jnp.float32)
    y = xf * lax.rsqrt(jnp.mean(xf * xf, axis=-1, keepdims=True) + EPS)
    return (y * g.astype(jnp.float32)).astype(x.dtype)


def short_conv_mixer(b_gate, c_gate, v, conv_w):
    L = v.shape[1]
    u = c_gate * v
    up = jnp.pad(u, ((0, 0), (CONV_WIDTH - 1, 0), (0, 0)))
    conv = conv_w[0] * up[:, 0:L]
    for k in range(1, CONV_WIDTH):
        conv = conv + conv_w[k] * up[:, k:k + L]
    return b_gate * conv


def pool_mixer(v, pool_w, pool_scale):
    Bn, L, _ = v.shape
    vf = v.astype(jnp.float32)
    cs = jnp.concatenate([jnp.zeros((Bn, 1, D_POOL), jnp.float32),
                          jnp.cumsum(vf, axis=1)], axis=1)
    pos = jnp.arange(L, dtype=jnp.int32)
    outs = []
    for g, w in enumerate(POOL_WINDOWS):
        sl = slice(g * POOL_GROUP_DIM, (g + 1) * POOL_GROUP_DIM)
        csg = cs[:, :, sl]
        upper = csg[:, 1:]
        lower = jnp.pad(csg[:, :L + 1 - w], ((0, 0), (w - 1, 0), (0, 0)))
        count = jnp.minimum(pos + 1, w).astype(jnp.float32)[None, :, None]
        outs.append((upper - lower) / count - vf[:, :, sl])
    pooled = jnp.stack(outs, axis=2).astype(v.dtype)
    mixed = jnp.einsum('blgc,gcd->blgd', pooled, pool_w)
    return mixed.reshape(Bn, L, D_POOL) * pool_scale


def hierarchical_moe(h, w_router_group, w_router_expert, w_gate, w_up, w_down):
    T = h.shape[0]
    lg = jnp.einsum('td,dg->tg', h, w_router_group).astype(jnp.float32)
    pg = jax.nn.softmax(lg, axis=-1)
    gsel = jnp.argmax(lg, axis=-1)
    g1 = jnp.take_along_axis(pg, gsel[:, None], axis=-1)
    le = jnp.einsum('td,de->te', h, w_router_expert).astype(jnp.float32)
    le = le.reshape(T, N_EXPERT_GROUPS, EXPERTS_PER_GROUP)
    le_sel = jnp.take_along_axis(le, gsel[:, None, None], axis=1)[:, 0]
    pe = jax.nn.softmax(le_sel, axis=-1)
    topv, topi = lax.top_k(pe, TOP_K)
    topv = topv / jnp.sum(topv, axis=-1, keepdims=True)
    within = jnp.sum(jax.nn.one_hot(topi, EXPERTS_PER_GROUP, dtype=jnp.float32)
                     * topv[..., None], axis=1)
    group_gate = jax.nn.one_hot(gsel, N_EXPERT_GROUPS, dtype=jnp.float32) * g1
    gates = (group_gate[:, :, None] * within[:, None, :]).reshape(T, N_EXPERTS).astype(h.dtype)
    out = jnp.zeros_like(h)
    for e in range(N_EXPERTS):
        a = jax.nn.silu(h @ w_gate[e]) * (h @ w_up[e])
        out = out + gates[:, e:e + 1] * (a @ w_down[e])
    return out


def setup_inputs(seed: int = 0) -> dict:
    key = jax.random.key(seed)
    ks = jax.random.split(key, 20)
    f32 = jnp.float32
    nrm = lambda k, shape, s: jax.random.normal(k, shape, f32) * s
    gain = lambda k, shape: 1.0 + 0.02 * jax.random.normal(k, shape, f32)
    return {
        'x': jax.random.normal(ks[0], (BATCH, SEQ, D_MODEL), f32),
        'meta_tokens': nrm(ks[1], (N_META, D_MODEL), 1.0),
        'norm_mix': gain(ks[2], (DEPTH, D_MODEL)),
        'w_in': nrm(ks[3], (DEPTH, D_MODEL, D_IN_PROJ), D_MODEL ** -0.5),
        'conv_w': nrm(ks[4], (DEPTH, CONV_WIDTH, D_CONV), CONV_WIDTH ** -0.5),
        'norm_conv_out': gain(ks[5], (DEPTH, D_CONV)),
        'pool_w': nrm(ks[6], (DEPTH, N_POOL_GROUPS, POOL_GROUP_DIM, POOL_GROUP_DIM), POOL_GROUP_DIM ** -0.5),
        'pool_scale': gain(ks[7], (DEPTH, D_POOL)) + 0.1 * jax.random.normal(ks[8], (DEPTH, D_POOL), f32),
        'norm_pool_out': gain(ks[9], (DEPTH, D_POOL)),
        'w_out': nrm(ks[10], (DEPTH, D_MIX, D_MODEL), D_MIX ** -0.5),
        'norm_ffn': gain(ks[11], (DEPTH, D_MODEL)),
        'w_router_group': nrm(ks[12], (DEPTH, D_MODEL, N_EXPERT_GROUPS), D_MODEL ** -0.5),
        'w_router_expert': nrm(ks[13], (DEPTH, D_MODEL, N_EXPERTS), D_MODEL ** -0.5),
        'w_gate': nrm(ks[14], (DEPTH, N_EXPERTS, D_MODEL, D_EXPERT), D_MODEL ** -0.5),
        'w_up': nrm(ks[15], (DEPTH, N_EXPERTS, D_MODEL, D_EXPERT), D_MODEL ** -0.5),
        'w_down': nrm(ks[16], (DEPTH, N_EXPERTS, D_EXPERT, D_MODEL), D_EXPERT ** -0.5),
        'final_norm': gain(ks[17], (D_MODEL,)),
    }


def reference(x, meta_tokens, norm_mix, w_in, conv_w, norm_conv_out, pool_w, pool_scale,
              norm_pool_out, w_out, norm_ffn, w_router_group, w_router_expert,
              w_gate, w_up, w_down, final_norm):
    Bn = x.shape[0]
    meta = jnp.broadcast_to(meta_tokens.astype(x.dtype)[None], (Bn, N_META, D_MODEL))
    h = jnp.concatenate([meta, x], axis=1)
    L = h.shape[1]
    for layer in range(DEPTH):
        hn = rmsnorm(h, norm_mix[layer])
        proj = jnp.einsum('bld,dc->blc', hn, w_in[layer])
        b_gate = proj[..., 0:D_CONV]
        c_gate = proj[..., D_CONV:2 * D_CONV]
        v_conv = proj[..., 2 * D_CONV:3 * D_CONV]
        v_pool = proj[..., 3 * D_CONV:]
        y_conv = rmsnorm(short_conv_mixer(b_gate, c_gate, v_conv, conv_w[layer]), norm_conv_out[layer])
        y_pool = rmsnorm(pool_mixer(v_pool, pool_w[layer], pool_scale[layer]), norm_pool_out[layer])
        y = jnp.concatenate([y_conv, y_pool], axis=-1)
        h = h + jnp.einsum('blc,cd->bld', y, w_out[layer])
        hf = rmsnorm(h, norm_ffn[layer]).reshape(Bn * L, D_MODEL)
        moe = hierarchical_moe(hf, w_router_group[layer], w_router_expert[layer],
                               w_gate[layer], w_up[layer], w_down[layer])
        h = h + moe.reshape(Bn, L, D_MODEL)
    out = rmsnorm(h, final_norm)
    return out[:, N_META:]
```

```python
import numpy as np
from contextlib import ExitStack
import concourse.bass as bass
import concourse.mybir as mybir
from concourse.bass_utils import run_bass_kernel_spmd

F32 = mybir.dt.float32
I32 = mybir.dt.int32
BF16 = mybir.dt.bfloat16
ALU = mybir.AluOpType
AF = mybir.ActivationFunctionType
AX = mybir.AxisListType

P = 128
D = 1024
NB = 256
HALO = 16
NE = 32
EPS = 1e-6
NCORES = 8
SEQ_PER_CORE = 4096

ENGS = ("pe", "act", "dve", "pool", "sp")


class Sched:
    def __init__(self, nc, stack):
        self.nc = nc
        self.stack = stack
        self.ops = []
        self.last_w = {}
        self.readers = {}

    def op(self, eng, fn, reads=(), writes=(), dma=None):
        i = len(self.ops)
        deps = set()
        for t in tuple(reads) + tuple(writes):
            if t in self.last_w:
                deps.add(self.last_w[t])
        for t in writes:
            r = self.readers.get(t)
            if r:
                deps.update(r[0].values())
                deps.update(r[1])
        deps.discard(i)
        for t in reads:
            r = self.readers.setdefault(t, ({}, []))
            if dma is None:
                r[0][eng] = i
            else:
                r[1].append(i)
        for t in writes:
            self.last_w[t] = i
            self.readers[t] = ({}, [])
        self.ops.append(dict(eng=eng, fn=fn, deps=deps, dma=dma, sig=False))
        return i

    def emit(self):
        nc, ops = self.nc, self.ops
        for o in ops:
            for d in o["deps"]:
                p = ops[d]
                if p["dma"] is None and o["dma"] is None and p["eng"] == "pe" and o["eng"] == "pe":
                    continue
                p["sig"] = True
        eng_sem = {e: self.stack.enter_context(nc.semaphore("prog_" + e)) for e in ENGS}
        cnt = {e: 0 for e in ENGS}
        dma_sems, dcnt = {}, {}
        for o in ops:
            if o["dma"] is not None:
                k = o["dma"]
                if k not in dma_sems:
                    dma_sems[k] = self.stack.enter_context(nc.semaphore("dma_" + k))
                    dcnt[k] = 0
                dcnt[k] += 16
                o["signal"] = (dma_sems[k], dcnt[k], "D" + k)
            elif o["sig"]:
                cnt[o["eng"]] += 1
                o["signal"] = (eng_sem[o["eng"]], cnt[o["eng"]], "E" + o["eng"])
        per_eng = {e: [] for e in ENGS}
        for o in ops:
            per_eng[o["eng"]].append(o)
        block = self.stack.enter_context(nc.Block())

        def run(engname):
            def body(eng):
                w = {}
                for o in per_eng[engname]:
                    need = {}
                    for d in o["deps"]:
                        p = ops[d]
                        if "signal" not in p:
                            continue
                        sem, val, key = p["signal"]
                        if need.get(key, (None, 0))[1] < val:
                            need[key] = (sem, val)
                    for key, (sem, val) in need.items():
                        if w.get(key, 0) >= val:
                            continue
                        eng.wait_ge(sem, val)
                        w[key] = val
                    ins = o["fn"](eng)
                    if "signal" in o:
                        ins.then_inc(o["signal"][0], 16 if o["dma"] is not None else 1)
                if engname == "sp":
                    for k, sem in dma_sems.items():
                        eng.wait_ge(sem, dcnt[k])
            return body

        block.tensor(run("pe"))
        block.scalar(run("act"))
        block.vector(run("dve"))
        block.gpsimd(run("pool"))
        block.sync(run("sp"))


def build(NT, MM_DT=F32):
    NTT = NT // P
    NBLK = NT // NB
    NST = 2 * NTT + NE
    SUB = NB // P
    nc = bass.Bass("TRN2", target_bir_lowering=False)

    def din(name, shape, dt=F32):
        return nc.dram_tensor(name, shape, dt, kind="ExternalInput").ap()

    x_d = din("x", [NT, D])
    xp_d = din("xp", [P, D])
    w_in_d = din("w_in", [D, 2048])
    w_out_d = din("w_out", [D, D])
    wr_d = din("wr", [D, 36])
    poolw_d = din("poolw", [P, 4 * P])
    colv_d = din("colv", [P, 32])
    rowv_d = din("rowv", [P, 2 * D])
    consts_d = din("consts", [P, 3 * P])
    wgu_d = din("wall_gu", [NE * P, 4096])
    wdn_d = din("wall_dn", [NE * P, 2048])
    out_d = nc.dram_tensor("out", [NT, D], F32, kind="ExternalOutput").ap()
    hf_d = nc.dram_tensor("hf_scr", [NT, D], F32, kind="Internal").ap()
    h1_d = nc.dram_tensor("h1_scr", [NT, D], F32, kind="Internal").ap()
    y_d = nc.dram_tensor("y_scr", [NST * P, D], F32, kind="Internal").ap()

    with ExitStack() as st:
        def sb(name, shape, dt=F32):
            return st.enter_context(nc.sbuf_tensor("s_" + name, shape, dt))

        def psum(name, shape):
            return st.enter_context(nc.psum_tensor("p_" + name, shape, F32))

        S = Sched(nc, st)
        big = sb("big", [P, 16384])
        w_in = big[:, :].rearrange("p (k c) -> p k c", k=8)
        w_out = sb("w_out", [P, 8, D])
        wr = sb("wr_sb", [P, 8, 36])
        poolw = sb("poolw_sb", [P, 4, P])
        poolw_s = sb("poolw_s", [P, 4, P])
        poolw_n = sb("poolw_n", [P, 4, P])
        colv = sb("colv_sb", [P, 32])
        rowv = sb("rowv_sb", [P, D])
        consts = sb("consts_sb", [P, 3 * P])
        ident, utri, ones = consts[:, 0:P], consts[:, P:2 * P], consts[:, 2 * P:3 * P]
        xt = [sb("xt0", [P, D]), sb("xt1", [P, D])]
        x2 = [sb("x20", [P, D]), sb("x21", [P, D])]
        hft = [sb("hft0", [P, D]), sb("hft1", [P, D])]
        junk = sb("junk", [P, D], BF16)
        hnT = sb("hnT", [P, 8, NB])
        bS = sb("bS", [P, 4, NB])
        cS = sb("cS", [P, 4, NB])
        vS = sb("vS", [P, 4, NB])
        uS = sb("uS", [P, 4, NB + HALO])
        vpS = sb("vpS", [P, 4, NB + HALO])
        acc = [sb("acc0", [P, NB]), sb("acc1", [P, NB])]
        sq4 = sb("sq4", [P, 4, NB])
        pooled4 = sb("pooled4", [P, 4, NB])
        tA = sb("tA", [P, NB + HALO])
        tB = sb("tB", [P, NB + HALO])
        rsn = sb("rsn", [P, 2, NB])
        yT = sb("yT", [P, 8, NB])
        hfT = sb("hfT", [P, 8, P])
        sm2 = sb("sm", [P, 2, 256])
        lgs = sb("lgs", [P, 2, 36])
        m12a = sb("m12", [P, 2, NE])
        srun = sb("srun", [P, NE])
        M1all = sb("M1all", [P, NTT, NE], BF16)
        M2all = sb("M2all", [P, NTT, NE], BF16)
        RT = sb("RT", [P, NTT, 4])
        ssb = sb("ssb", [P, 16])
        fin = hft[1]
        yidx = sb("yidx", [P, NTT, 2], I32)
        toki = sb("toki", [P, NST], I32)
        idxw = sb("idxw", [P, NST], I32)
        sgS = sb("sgS", [P, 256])
        aS = sb("aS", [P, 256])
        aT = sb("aT", [P, 2, P])

        pin = [psum("pin%d" % i, [P, 512]) for i in range(3)]
        pm = [psum("pm%d" % i, [P, 512]) for i in range(3)]
        po = psum("po", [P, 1024])
        mctr = [0]

        def mbank():
            i = mctr[0] % 3
            mctr[0] += 1
            return i

        ssc = [0]

        def sscol():
            i = ssc[0] % 16
            ssc[0] += 1
            return i

        S.op("sp", lambda e: e.dma_start(out=consts[:, :], in_=consts_d[:, :]), writes=["consts"], dma="c0")
        S.op("sp", lambda e: e.dma_start(out=colv[:, :], in_=colv_d[:, :]), writes=["colv"], dma="c1")
        def setup_weights():
            for k in range(8):
                S.op("sp", lambda e, k=k: e.dma_start(out=w_in[:, k, :], in_=w_in_d[k * P:(k + 1) * P, :]),
                     writes=["w_in%d" % k], dma="win%d" % k)
                if k % 2:
                    S.op("act", lambda e, k=k: e.mul(out=w_in[:, k, :], in_=w_in[:, k, :], mul=colv[:, k:k + 1]),
                         reads=["colv"], writes=["w_in%d" % k])
                else:
                    S.op("dve", lambda e, k=k: e.tensor_scalar(out=w_in[:, k, :], in0=w_in[:, k, :],
                                                              scalar1=colv[:, k:k + 1], scalar2=None, op0=ALU.mult),
                         reads=["colv"], writes=["w_in%d" % k])
            S.op("sp", lambda e: e.dma_start(out=poolw[:, :, :], in_=poolw_d[:, :].rearrange("p (g d) -> p g d", g=4)),
                 writes=["poolw"], dma="c2")
            for g in range(4):
                S.op("dve", lambda e, g=g: e.tensor_scalar(out=poolw_s[:, g, :], in0=poolw[:, g, :], scalar1=1.0 / (2 << g),
                                                          scalar2=None, op0=ALU.mult), reads=["poolw"], writes=["poolw_s"])
            S.op("dve", lambda e: e.tensor_scalar(out=poolw_n[:, :, :], in0=poolw[:, :, :], scalar1=-1.0, scalar2=None,
                                                  op0=ALU.mult), reads=["poolw"], writes=["poolw_n"])
            S.op("sp", lambda e: e.dma_start(out=wr[:, :, :], in_=wr_d[:, :].rearrange("(k p) n -> p k n", p=P)),
                 writes=["wr"], dma="c3")
            S.op("sp", lambda e: e.dma_start(out=rowv[:, :], in_=rowv_d[:, 0:D]), writes=["rowv"], dma="c4")
            S.op("dve", lambda e: e.memset(srun[:, :], 0.0), writes=["srun"])
        WIN = ["w_in%d" % k for k in range(8)]

        def rstd_ops(src_ap, dst_ap, n, rtok, wtok):
            S.op("act", lambda e: e.activation(out=dst_ap, in_=src_ap, func=AF.Ln, bias=EPS, scale=1.0 / n),
                 reads=rtok, writes=wtok)
            S.op("act", lambda e: e.activation(out=dst_ap, in_=dst_ap, func=AF.Exp, scale=-0.5), reads=wtok,
                 writes=wtok)

        def stage_a_act(src_rows, slot):
            xs = xt[slot]
            tk = "xt%d" % slot
            S.op("sp", lambda e: e.dma_start(out=xs[:, :], in_=src_rows), writes=[tk], dma=tk)
            c = sscol()
            sst = "ss%d" % c
            S.op("act", lambda e: e.activation(out=junk[:, :], in_=xs[:, :], func=AF.Square,
                                               accum_out=ssb[:, c:c + 1]), reads=[tk], writes=["junk", sst])
            rstd_ops(ssb[:, c:c + 1], ssb[:, c:c + 1], float(D), [sst], [sst])
            S.op("act", lambda e: e.mul(out=xs[:, :], in_=xs[:, :], mul=ssb[:, c:c + 1]), reads=[sst], writes=[tk])

        def stage_a_tr(slot, col0):
            xs = xt[slot]
            tk = "xt%d" % slot
            for h in range(2):
                b = mbank()
                for kk in range(4):
                    k = h * 4 + kk
                    S.op("pe", lambda e, b=b, kk=kk, k=k: e.transpose(out=pm[b][:, kk * P:(kk + 1) * P],
                                                                      in_=xs[:, k * P:(k + 1) * P], identity=ident),
                         reads=[tk, "consts"], writes=["pm%d" % b])
                S.op("act", lambda e, b=b, h=h: e.copy(out=hnT[:, h * 4:(h + 1) * 4, col0:col0 + P],
                                                       in_=pm[b][:, :].rearrange("p (k t) -> p k t", k=4)),
                     reads=["pm%d" % b], writes=["hnT"])

        def stage_a(src_rows, slot, col0):
            stage_a_act(src_rows, slot)
            stage_a_tr(slot, col0)

        pinc = [0]

        def inproj_group(g, ncols, prefix=False):
            b = pinc[0] % 3
            pinc[0] += 1
            for cc in range(2):
                c = 2 * g + cc
                for k in range(8):
                    S.op("pe", lambda e, b=b, cc=cc, c=c, k=k: e.matmul(
                        pin[b][:, cc * ncols:(cc + 1) * ncols], lhsT=w_in[:, k, c * P:(c + 1) * P],
                        rhs=hnT[:, k, 0:ncols], start=(k == 0), stop=(k == 7)),
                        reads=["hnT", WIN[k]], writes=["pin%d" % b])
            src = pin[b][:, 0:2 * ncols].rearrange("p (c t) -> p c t", c=2)
            j = 2 * (g % 2)
            if not prefix:
                if g < 2:
                    dst, tok = bS[:, j:j + 2, :], "bS"
                elif g < 4:
                    dst, tok = cS[:, j:j + 2, :], "cS"
                elif g < 6:
                    dst, tok = vS[:, j:j + 2, :], "vS"
                else:
                    dst, tok = vpS[:, j:j + 2, HALO:HALO + NB], "vpm"
                S.op("act", lambda e: e.copy(out=dst, in_=src), reads=["pin%d" % b], writes=[tok])
            else:
                srch = src[:, :, ncols - HALO:ncols]
                if g in (2, 3):
                    S.op("act", lambda e: e.copy(out=cS[:, j:j + 2, 0:HALO], in_=srch), reads=["pin%d" % b], writes=["cS"])
                elif g in (4, 5):
                    S.op("dve", lambda e: e.tensor_tensor(out=uS[:, j:j + 2, 0:HALO], in0=cS[:, j:j + 2, 0:HALO],
                                                          in1=srch, op=ALU.mult),
                         reads=["pin%d" % b, "cS"], writes=["uh"])
                else:
                    S.op("act", lambda e: e.copy(out=vpS[:, j:j + 2, 0:HALO], in_=srch), reads=["pin%d" % b], writes=["vph"])

        def mix_part1():
            S.op("dve", lambda e: e.tensor_tensor(out=uS[:, :, HALO:HALO + NB], in0=cS[:, :, :], in1=vS[:, :, :],
                                                  op=ALU.mult), reads=["cS", "vS"], writes=["um"])
            for ch in range(4):
                a = acc[ch % 2]
                at = "acc%d" % (ch % 2)
                S.op("dve", lambda e, a=a, ch=ch: e.tensor_scalar(out=a[:, :], in0=uS[:, ch, HALO - 2:HALO - 2 + NB],
                                                                 scalar1=colv[:, 8 + ch * 3:9 + ch * 3], scalar2=None,
                                                                 op0=ALU.mult), reads=["um", "uh", "colv"], writes=[at])
                for tap in (1, 2):
                    S.op("dve", lambda e, a=a, ch=ch, tap=tap: e.scalar_tensor_tensor(
                        out=a[:, :], in0=uS[:, ch, HALO - 2 + tap:HALO - 2 + tap + NB],
                        scalar=colv[:, 8 + ch * 3 + tap:9 + ch * 3 + tap], in1=a[:, :], op0=ALU.mult, op1=ALU.add),
                        reads=["um", "uh", at], writes=[at])
                S.op("dve", lambda e, a=a, ch=ch: e.tensor_tensor(out=yT[:, ch, :], in0=bS[:, ch, :], in1=a[:, :],
                                                                 op=ALU.mult), reads=["bS", at], writes=["yTc%d" % ch])
            S.op("dve", lambda e: e.tensor_copy(out=uS[:, :, HALO - 2:HALO], in_=uS[:, :, NB + HALO - 2:NB + HALO]),
                 reads=["um"], writes=["uh"])
            W = NB + HALO
            for g in range(4):
                w = 2 << g
                src = vpS[:, g, :]
                cur, curtok = src, None
                lo_needed = HALO
                lvl = 1
                bufs = [tA, tB]
                bi = 0
                while lvl < w:
                    nl = lvl * 2
                    lo = HALO - (w - nl)
                    last = nl == w
                    rt = ["vpm", "vph"] if curtok is None else [curtok]
                    if last:
                        S.op("pool", lambda e, cur=cur, lvl=lvl, g=g: e.tensor_tensor(
                            out=pooled4[:, g, :], in0=cur[:, HALO:W], in1=cur[:, HALO - lvl:W - lvl], op=ALU.add),
                            reads=rt, writes=["pooled%d" % g])
                    else:
                        dstb = bufs[bi]
                        dtok = "tA" if bi == 0 else "tB"
                        S.op("pool", lambda e, dstb=dstb, cur=cur, lo=lo, lvl=lvl: e.tensor_tensor(
                            out=dstb[:, lo:W], in0=cur[:, lo:W], in1=cur[:, lo - lvl:W - lvl], op=ALU.add),
                            reads=rt, writes=[dtok])
                        cur, curtok = dstb, dtok
                        bi ^= 1
                    lvl = nl
            S.op("pool", lambda e: e.tensor_copy(out=vpS[:, :, 0:HALO], in_=vpS[:, :, NB:NB + HALO]),
                 reads=["vpm"], writes=["vph"])

        def mix_part1b():
            for g in range(4):
                pb = pooled4[:, g, :]
                pt = "pooled%d" % g
                if g % 2 == 0:
                    mix_part1.bank[g // 2] = mbank()
                mb_ = mix_part1.bank[g // 2]
                S.op("pe", lambda e, mb_=mb_, g=g, pb=pb: e.matmul(pm[mb_][:, (g % 2) * NB:(g % 2 + 1) * NB],
                                                                   lhsT=poolw_s[:, g, :], rhs=pb, start=True, stop=False),
                     reads=[pt, "poolw_s"], writes=["pm%d" % mb_])
                S.op("pe", lambda e, mb_=mb_, g=g: e.matmul(pm[mb_][:, (g % 2) * NB:(g % 2 + 1) * NB],
                                                            lhsT=poolw_n[:, g, :], rhs=vpS[:, g, HALO:HALO + NB],
                                                            start=False, stop=True),
                     reads=["vpm", "poolw_n"], writes=["pm%d" % mb_])
            for g in range(4):
                mb_ = mix_part1.bank[g // 2]
                S.op("dve", lambda e, mb_=mb_, g=g: e.tensor_scalar(out=yT[:, 4 + g, :],
                                                                     in0=pm[mb_][:, (g % 2) * NB:(g % 2 + 1) * NB],
                                                                     scalar1=colv[:, 24 + g:25 + g], scalar2=None,
                                                                     op0=ALU.mult),
                     reads=["pm%d" % mb_, "colv"], writes=["yTp%d" % g])

        mix_part1.bank = [0, 0]

        mix2_bank = [0]

        def mix2_pre(half):
            pre = "yTc" if half == 0 else "yTp"
            S.op("act", lambda e: e.activation(out=sq4[:, :, :], in_=yT[:, half * 4:half * 4 + 4, :], func=AF.Square),
                 reads=[pre + "%d" % i for i in range(4)], writes=["sq4"])
            ab = acc[half]
            S.op("dve", lambda e: e.tensor_reduce(out=ab[:, :], in_=sq4[:, :, :].rearrange("p c t -> p t c"),
                                                  axis=AX.X, op=ALU.add), reads=["sq4"], writes=["acc%d" % half])

        def mix2_mm():
            sb_ = mbank()
            for half in range(2):
                S.op("pe", lambda e, half=half: e.matmul(pm[sb_][:, half * NB:(half + 1) * NB], lhsT=ones,
                                                         rhs=acc[half][:, :], start=True, stop=True),
                     reads=["acc%d" % half, "consts"], writes=["pm%d" % sb_])
            for half in range(2):
                rstd_ops(pm[sb_][:, half * NB:(half + 1) * NB], rsn[:, half, :], 512.0, ["pm%d" % sb_], ["rsn%d" % half])

        def mix_part3():
            for half, pre, gcol in ((0, "yTc", 20), (1, "yTp", 28)):
                for ch in range(4):
                    S.op("dve", lambda e, half=half, ch=ch, gcol=gcol: e.scalar_tensor_tensor(
                        out=yT[:, half * 4 + ch, :], in0=yT[:, half * 4 + ch, :], scalar=colv[:, gcol + ch:gcol + ch + 1],
                        in1=rsn[:, half, :], op0=ALU.mult, op1=ALU.mult),
                        reads=[pre + "%d" % ch, "rsn%d" % half, "colv"], writes=[pre + "%d" % ch])

        YT_ALL = ["yTc%d" % i for i in range(4)] + ["yTp%d" % i for i in range(4)]

        def stage_d(tt, s):
            r0 = tt * P
            slot = tt % 2
            xb, xtok = x2[slot], "x2%d" % slot
            hb, htok = hft[slot], "hft%d" % slot
            S.op("sp", lambda e: e.dma_start(out=xb[:, :], in_=x_d[r0:r0 + P, :]), writes=[xtok], dma=xtok)
            for half in range(2):
                for k in range(8):
                    S.op("pe", lambda e, half=half, k=k: e.matmul(
                        po[:, half * 512:(half + 1) * 512], lhsT=yT[:, k, s * P:(s + 1) * P],
                        rhs=w_out[:, k, half * 512:(half + 1) * 512], start=(k == 0), stop=(k == 7)),
                        reads=YT_ALL + ["w_out"], writes=["po"])
            S.op("dve", lambda e: e.tensor_tensor(out=xb[:, :], in0=xb[:, :], in1=po[:, :], op=ALU.add),
                 reads=["po", xtok], writes=[xtok])
            c = sscol()
            sst = "ss%d" % c
            S.op("act", lambda e: e.activation(out=junk[:, :], in_=xb[:, :], func=AF.Square,
                                               accum_out=ssb[:, c:c + 1]), reads=[xtok], writes=["junk", sst])
            rstd_ops(ssb[:, c:c + 1], ssb[:, c:c + 1], float(D), [sst], [sst])
            S.op("dve", lambda e: e.scalar_tensor_tensor(out=hb[:, :], in0=xb[:, :], scalar=ssb[:, c:c + 1],
                                                         in1=rowv[:, :], op0=ALU.mult, op1=ALU.mult),
                 reads=[xtok, sst, "rowv"], writes=[htok])
            S.op("act", lambda e: e.dma_start(out=h1_d[r0:r0 + P, :], in_=xb[:, :]), reads=[xtok, "h1_d"],
                 dma="h1s%d" % slot)
            S.op("act", lambda e: e.dma_start(out=hf_d[r0:r0 + P, :], in_=hb[:, :]), reads=[htok, "hf_d"],
                 dma="hfs%d" % slot)

        def stage_dtr(tt, s):
            slot = tt % 2
            hb, htok = hft[slot], "hft%d" % slot
            for h in range(2):
                b = mbank()
                for kk in range(4):
                    k = h * 4 + kk
                    S.op("pe", lambda e, b=b, kk=kk, k=k: e.transpose(out=pm[b][:, kk * P:(kk + 1) * P],
                                                                      in_=hb[:, k * P:(k + 1) * P], identity=ident),
                         reads=[htok, "consts"], writes=["pm%d" % b])
                S.op("act", lambda e, b=b, h=h: e.copy(out=hfT[:, h * 4:(h + 1) * 4, :],
                                                       in_=pm[b][:, :].rearrange("p (k t) -> p k t", k=4)),
                     reads=["pm%d" % b], writes=["hfT"])

        def stage_drt(tt, s):
            rb = mbank()
            rbt = "pm%d" % rb
            for k in range(8):
                S.op("pe", lambda e, k=k: e.matmul(pm[rb][:, 0:36], lhsT=hfT[:, k, :], rhs=wr[:, k, :], start=(k == 0),
                                                   stop=(k == 7)), reads=["hfT", "wr"], writes=[rbt])
            S.op("dve", lambda e: e.tensor_copy(out=lgs[:, s, :], in_=pm[rb][:, 0:36]), reads=[rbt],
                 writes=["lg%d" % s])

        def stage_d2(tt, s):
            lg = lgs[:, s, :]
            sm = sm2[:, s, :]
            m12 = m12a[:, s, :]
            M12T = "m12_%d" % s
            gmax, ngmax, sume, g1 = sm[:, 40:41], sm[:, 41:42], sm[:, 42:43], sm[:, 43:44]
            goh = sm[:, 44:48]
            eg = sm[:, 48:52]
            sel = sm[:, 56:64]
            top8 = sm[:, 64:72]
            mask2 = sm[:, 72:80]
            mask1 = sm[:, 80:88]
            dd, ed = sm[:, 88:89], sm[:, 89:90]
            prod = sm[:, 96:128]
            m1f = sm[:, 128:160]
            m2f = sm[:, 160:192]
            T = "sm%d" % s
            S.op("dve", lambda e: e.tensor_reduce(out=gmax, in_=lg[:, 0:4], axis=AX.X, op=ALU.max),
                 reads=[T, "lg%d" % s], writes=[T])
            S.op("dve", lambda e: e.tensor_scalar(out=goh, in0=lg[:, 0:4], scalar1=gmax, scalar2=None, op0=ALU.is_ge),
                 reads=[T], writes=[T])
            S.op("dve", lambda e: e.tensor_scalar(out=ngmax, in0=gmax, scalar1=-1.0, scalar2=None, op0=ALU.mult),
                 reads=[T], writes=[T])
            S.op("act", lambda e: e.activation(out=eg, in_=lg[:, 0:4], func=AF.Exp, bias=ngmax, accum_out=sume),
                 reads=[T], writes=[T])
            S.op("dve", lambda e: e.reciprocal(out=g1, in_=sume), reads=[T], writes=[T])
            S.op("dve", lambda e: e.tensor_scalar(out=sel, in0=lg[:, 4:12], scalar1=goh[:, 0:1], scalar2=None,
                                                  op0=ALU.mult), reads=[T], writes=[T])
            for g in range(1, 4):
                S.op("dve", lambda e, g=g: e.scalar_tensor_tensor(out=sel, in0=lg[:, 4 + 8 * g:12 + 8 * g],
                                                                  scalar=goh[:, g:g + 1], in1=sel, op0=ALU.mult,
                                                                  op1=ALU.add), reads=[T], writes=[T])
            S.op("dve", lambda e: e.max(out=top8, in_=sel), reads=[T], writes=[T])
            S.op("dve", lambda e: e.tensor_scalar(out=mask2, in0=sel, scalar1=top8[:, 1:2], scalar2=None, op0=ALU.is_ge),
                 reads=[T], writes=[T])
            S.op("dve", lambda e: e.tensor_scalar(out=mask1, in0=sel, scalar1=top8[:, 0:1], scalar2=None, op0=ALU.is_ge),
                 reads=[T], writes=[T])
            S.op("dve", lambda e: e.tensor_tensor(out=dd, in0=top8[:, 1:2], in1=top8[:, 0:1], op=ALU.subtract),
                 reads=[T], writes=[T])
            S.op("act", lambda e: e.activation(out=ed, in_=dd, func=AF.Exp), reads=[T], writes=[T])
            S.op("dve", lambda e: e.tensor_scalar(out=ed, in0=ed, scalar1=1.0, scalar2=None, op0=ALU.add),
                 reads=[T], writes=[T])
            S.op("dve", lambda e: e.reciprocal(out=ed, in_=ed), reads=[T], writes=[T])
            S.op("dve", lambda e: e.tensor_tensor(out=RT[:, tt, 2:3], in0=g1, in1=ed, op=ALU.mult), reads=[T],
                 writes=["RT"])
            S.op("dve", lambda e: e.tensor_tensor(out=RT[:, tt, 3:4], in0=g1, in1=RT[:, tt, 2:3], op=ALU.subtract),
                 reads=[T, "RT"], writes=["RT"])
            for g in range(4):
                S.op("dve", lambda e, g=g: e.tensor_scalar(out=m12[:, 8 * g:8 * g + 8], in0=mask2, scalar1=goh[:, g:g + 1],
                                                           scalar2=None, op0=ALU.mult), reads=[T], writes=[M12T])
                S.op("dve", lambda e, g=g: e.tensor_scalar(out=m1f[:, 8 * g:8 * g + 8], in0=mask1, scalar1=goh[:, g:g + 1],
                                                           scalar2=None, op0=ALU.mult), reads=[T], writes=[T])
            S.op("dve", lambda e: e.tensor_tensor(out=m2f, in0=m12, in1=m1f, op=ALU.subtract),
                 reads=[T, M12T], writes=[T])
            S.op("dve", lambda e: e.tensor_copy(out=M1all[:, tt, :], in_=m1f), reads=[T], writes=["Mall"])
            S.op("dve", lambda e: e.tensor_copy(out=M2all[:, tt, :], in_=m2f), reads=[T], writes=["Mall"])

        def stage_d2b(tt, s):
            lg = lgs[:, s, :]
            sm = sm2[:, s, :]
            m12 = m12a[:, s, :]
            M12T = "m12_%d" % s
            gmax, ngmax, sume, g1 = sm[:, 40:41], sm[:, 41:42], sm[:, 42:43], sm[:, 43:44]
            goh = sm[:, 44:48]
            eg = sm[:, 48:52]
            sel = sm[:, 56:64]
            top8 = sm[:, 64:72]
            mask2 = sm[:, 72:80]
            mask1 = sm[:, 80:88]
            dd, ed = sm[:, 88:89], sm[:, 89:90]
            prod = sm[:, 96:128]
            m1f = sm[:, 128:160]
            m2f = sm[:, 160:192]
            T = "sm%d" % s
            kb = mbank()
            kbt = "pm%d" % kb
            S.op("pe", lambda e: e.matmul(pm[kb][:, 0:NE], lhsT=utri, rhs=m12, start=True, stop=False),
                 reads=[M12T, "consts"], writes=[kbt])
            S.op("pe", lambda e: e.matmul(pm[kb][:, 0:NE], lhsT=ones, rhs=srun[:, :], start=False, stop=True),
                 reads=["srun", "consts"], writes=[kbt])
            for kk, mf in ((0, m1f), (1, m2f)):
                S.op("dve", lambda e, mf=mf: e.tensor_tensor(out=prod, in0=mf, in1=pm[kb][:, 0:NE], op=ALU.mult),
                     reads=[T, kbt], writes=[T])
                S.op("dve", lambda e, kk=kk: e.tensor_reduce(out=RT[:, tt, kk:kk + 1], in_=prod, axis=AX.X, op=ALU.add),
                     reads=[T], writes=["RT"])
            S.op("dve", lambda e: e.tensor_tensor(out=srun[:, :], in0=srun[:, :], in1=m12, op=ALU.add),
                 reads=[M12T, "srun"], writes=["srun"])

        stage_a_act(xp_d[:, :], 0)
        setup_weights()
        stage_a_tr(0, 0)
        for g in range(2, 8):
            inproj_group(g, P, prefix=True)
        for s in range(SUB):
            stage_a(x_d[s * P:(s + 1) * P, :], s % 2, s * P)
        for g in range(8):
            inproj_group(g, NB)
        for k in range(8):
            S.op("sp", lambda e, k=k: e.dma_start(out=w_out[:, k, :], in_=w_out_d[k * P:(k + 1) * P, :]),
                 writes=["w_out"], dma="wout%d" % k)
        def a_act_block(bi):
            for s in range(SUB):
                r0 = bi * NB + s * P
                stage_a_act(x_d[r0:r0 + P, :], s % 2)

        if NBLK > 1:
            a_act_block(1)
        pending = [None]
        for i in range(NBLK):
            nxt = i + 1 < NBLK
            if nxt:
                for s in range(SUB):
                    stage_a_tr(s % 2, s * P)
            if pending[0] is not None:
                stage_drt(*pending[0])
                pending[0] = None
            mix_part1()
            mix2_pre(0)
            if i > 0:
                stage_d2((i - 1) * SUB, 0)
            if nxt:
                inproj_group(0, NB)
                inproj_group(1, NB)
            mix_part1b()
            mix2_pre(1)
            if i > 0:
                stage_d2((i - 1) * SUB + 1, 1)
            if nxt:
                inproj_group(2, NB)
                inproj_group(3, NB)
            mix2_mm()
            if i + 2 < NBLK:
                a_act_block(i + 2)
            if nxt:
                inproj_group(4, NB)
            mix_part3()
            if i > 0:
                stage_d2b((i - 1) * SUB, 0)
            if nxt:
                inproj_group(5, NB)
            if i > 0:
                stage_d2b((i - 1) * SUB + 1, 1)
            assert SUB == 2
            stage_d(i * SUB, 0)
            if nxt:
                inproj_group(6, NB)
            stage_dtr(i * SUB, 0)
            stage_d(i * SUB + 1, 1)
            stage_drt(i * SUB, 0)
            if nxt:
                inproj_group(7, NB)
            stage_dtr(i * SUB + 1, 1)
            pending[0] = (i * SUB + 1, 1)
        stage_drt(*pending[0])
        for s in range(SUB):
            stage_d2((NBLK - 1) * SUB + s, s)
        for s in range(SUB):
            stage_d2b((NBLK - 1) * SUB + s, s)

        cb = mbank()
        cbt = "pm%d" % cb
        S.op("pe", lambda e: e.matmul(pm[cb][:, 0:NE], lhsT=ones, rhs=srun[:, :], start=True, stop=True),
             reads=["srun", "consts"], writes=[cbt])
        F = "hft1"
        cnt = fin[:, 0:32]
        tl = fin[:, 32:64]
        ca = fin[:, 64:96]
        cb2 = fin[:, 96:128]
        tstart = fin[:, 128:160]
        S.op("dve", lambda e: e.tensor_copy(out=cnt, in_=pm[cb][:, 0:NE]), reads=[cbt], writes=[F])
        S.op("dve", lambda e: e.tensor_scalar(out=tl, in0=cnt, scalar1=0.0, scalar2=None, op0=ALU.is_gt), reads=[F],
             writes=[F])
        for m in range(1, NTT):
            S.op("dve", lambda e, m=m: e.scalar_tensor_tensor(out=tl, in0=cnt, scalar=float(P * m), in1=tl,
                                                              op0=ALU.is_gt, op1=ALU.add), reads=[F], writes=[F])
        S.op("dve", lambda e: e.tensor_copy(out=ca, in_=tl), reads=[F], writes=[F])
        cur, oth = ca, cb2
        sh = 1
        while sh < NE:
            S.op("dve", lambda e, cur=cur, oth=oth, sh=sh: e.tensor_copy(out=oth[:, 0:sh], in_=cur[:, 0:sh]), reads=[F],
                 writes=[F])
            S.op("dve", lambda e, cur=cur, oth=oth, sh=sh: e.tensor_tensor(out=oth[:, sh:NE], in0=cur[:, sh:NE],
                                                                           in1=cur[:, 0:NE - sh], op=ALU.add),
                 reads=[F], writes=[F])
            cur, oth = oth, cur
            sh *= 2
        tend = cur
        S.op("dve", lambda e: e.tensor_tensor(out=tstart, in0=tend, in1=tl, op=ALU.subtract), reads=[F], writes=[F])
        tsl = fin[:, 160:160 + 2 * NTT].rearrange("p (t k) -> p t k", k=2)
        flr = fin[:, 256:256 + 2 * NTT].rearrange("p (t k) -> p t k", k=2)
        sres = fin[:, 384:384 + 2 * NTT].rearrange("p (t k) -> p t k", k=2)
        tmp = fin[:, 512:512 + 2 * NTT].rearrange("p (t k) -> p t k", k=2)
        prodA = hft[0][:, 0:NTT * NE].rearrange("p (t e) -> p t e", e=NE)
        tsb = tstart.unsqueeze(1).broadcast_to([P, NTT, NE])
        for kk, Mx in ((0, M1all), (1, M2all)):
            S.op("dve", lambda e, Mx=Mx: e.tensor_tensor(out=prodA, in0=Mx[:, :, :], in1=tsb, op=ALU.mult),
                 reads=[F, "Mall", "hft0"], writes=["hft0"])
            S.op("dve", lambda e, kk=kk: e.tensor_reduce(out=tsl[:, :, kk], in_=prodA, axis=AX.X, op=ALU.add),
                 reads=["hft0"], writes=[F])
        rank = RT[:, :, 0:2]
        S.op("dve", lambda e: e.tensor_scalar(out=flr, in0=rank, scalar1=float(P), scalar2=None, op0=ALU.is_ge),
             reads=["RT", F], writes=[F])
        for m in range(2, NTT):
            S.op("dve", lambda e, m=m: e.scalar_tensor_tensor(out=flr, in0=rank, scalar=float(P * m), in1=flr,
                                                              op0=ALU.is_ge, op1=ALU.add), reads=["RT", F], writes=[F])
        S.op("dve", lambda e: e.tensor_tensor(out=tsl, in0=tsl, in1=flr, op=ALU.add), reads=[F], writes=[F])
        S.op("dve", lambda e: e.scalar_tensor_tensor(out=sres, in0=flr, scalar=-float(P), in1=rank, op0=ALU.mult,
                                                     op1=ALU.add), reads=[F, "RT"], writes=[F])
        S.op("dve", lambda e: e.scalar_tensor_tensor(out=tmp, in0=tsl, scalar=float(P), in1=sres, op0=ALU.mult,
                                                     op1=ALU.add), reads=[F], writes=[F])
        S.op("dve", lambda e: e.tensor_copy(out=yidx[:, :, :], in_=tmp), reads=[F], writes=["yidx"])
        jio = fin[:, 640:640 + NST]
        ej = fin[:, 768:768 + NST]
        pio = fin[:, 900:901]
        S.op("pool", lambda e: e.iota(jio, pattern=[[1, NST]], base=0, channel_multiplier=0,
                                      allow_small_or_imprecise_dtypes=True), writes=[F])
        S.op("pool", lambda e: e.iota(pio, pattern=[[0, 1]], base=0, channel_multiplier=1,
                                      allow_small_or_imprecise_dtypes=True), writes=[F])
        S.op("dve", lambda e: e.tensor_scalar(out=ej, in0=jio, scalar1=tend[:, 0:1], scalar2=None, op0=ALU.is_ge),
             reads=[F], writes=[F])
        for ee in range(1, NE):
            S.op("dve", lambda e, ee=ee: e.scalar_tensor_tensor(out=ej, in0=jio, scalar=tend[:, ee:ee + 1], in1=ej,
                                                                op0=ALU.is_ge, op1=ALU.add), reads=[F],
                 writes=[F])
        S.op("dve", lambda e: e.tensor_scalar(out=ej, in0=ej, scalar1=float(NE - 1), scalar2=float(P), op0=ALU.min,
                                              op1=ALU.mult), reads=[F], writes=[F])
        S.op("dve", lambda e: e.tensor_scalar(out=ej, in0=ej, scalar1=pio, scalar2=None, op0=ALU.add),
             reads=[F], writes=[F])
        S.op("dve", lambda e: e.tensor_copy(out=idxw[:, :], in_=ej), reads=[F], writes=["idxw"])
        hflat = hnT[:, :, :].rearrange("p k t -> p (k t)")
        OS = [hflat[:, 0:P], hflat[:, P:2 * P]]
        OT = [hflat[:, 256:256 + NST], hflat[:, 256 + NST:256 + 2 * NST]]
        io128 = hflat[:, 512:512 + P]
        tokf = hflat[:, 640:640 + NTT]
        S.op("pool", lambda e: e.iota(io128, pattern=[[1, P]], base=0, channel_multiplier=0,
                                      allow_small_or_imprecise_dtypes=True), writes=["hnT"])
        S.op("pool", lambda e: e.iota(tokf, pattern=[[P, NTT]], base=0, channel_multiplier=1,
                                      allow_small_or_imprecise_dtypes=True), writes=["hnT"])
        tb = mbank()
        tbt = "pm%d" % tb
        n = 0
        for tt in range(NTT):
            for kk in range(2):
                sl = n % 2
                S.op("dve", lambda e, sl=sl, tt=tt, kk=kk: e.tensor_scalar(
                    out=OS[sl], in0=io128, scalar1=sres[:, tt, kk:kk + 1], scalar2=None, op0=ALU.is_equal),
                    reads=[F, "hnT"], writes=["OS%d" % sl])
                S.op("dve", lambda e, sl=sl, tt=tt, kk=kk: e.tensor_scalar(
                    out=OT[sl], in0=jio, scalar1=tsl[:, tt, kk:kk + 1], scalar2=tokf[:, tt:tt + 1], op0=ALU.is_equal,
                    op1=ALU.mult), reads=[F, "hnT"], writes=["OT%d" % sl])
                S.op("pe", lambda e, sl=sl, n=n: e.matmul(pm[tb][:, 0:NST], lhsT=OS[sl], rhs=OT[sl], start=(n == 0),
                                                          stop=(n == 2 * NTT - 1)),
                     reads=["OS%d" % sl, "OT%d" % sl], writes=[tbt])
                n += 1
        S.op("dve", lambda e: e.tensor_copy(out=toki[:, :], in_=pm[tb][:, 0:NST]), reads=[tbt],
             writes=["toki", "hf_d", "h1_d"])

        NSL = 4
        wof = w_out[:, :, :].rearrange("p k d -> p (k d)")
        wgu = [big[:, 0:4096], big[:, 4096:8192], big[:, 8192:12288], wof[:, 4096:8192]]
        wdn = [big[:, 12288:14336], big[:, 14336:16384], wof[:, 0:2048], wof[:, 2048:4096]]
        xg = [xt[0], xt[1], hft[0]]
        xgt = ["xt0", "xt1", "hft0"]
        NXS = 3

        def gather_x(j):
            sl = j % NXS
            S.op("pool", lambda e: e.indirect_dma_start(
                out=xg[sl][:, :], out_offset=None, in_=hf_d[:, :],
                in_offset=bass.IndirectOffsetOnAxis(ap=toki[:, j:j + 1], axis=0)),
                reads=["toki", "hf_d"], writes=[xgt[sl]], dma="xg%d" % sl)

        def gather_gu(j):
            sl = j % NSL
            first = (WIN if sl < 3 else ["w_out"]) if j < NSL else []
            S.op("pool", lambda e: e.indirect_dma_start(
                out=wgu[sl], out_offset=None, in_=wgu_d[:, :],
                in_offset=bass.IndirectOffsetOnAxis(ap=idxw[:, j:j + 1], axis=0)),
                reads=["idxw"], writes=["wgu%d" % sl] + first, dma="wgu%d" % sl)

        def gather_dn(j):
            sl = j % NSL
            first = (WIN if sl < 2 else ["w_out"]) if j < NSL else []
            S.op("pool", lambda e: e.indirect_dma_start(
                out=wdn[sl], out_offset=None, in_=wdn_d[:, :],
                in_offset=bass.IndirectOffsetOnAxis(ap=idxw[:, j:j + 1], axis=0)),
                reads=["idxw"], writes=["wdn%d" % sl] + first, dma="wdn%d" % sl)

        def xt_b(j):
            sl = j % NXS
            for h in range(2):
                b = mbank()
                for kk in range(4):
                    k = h * 4 + kk
                    S.op("pe", lambda e, b=b, kk=kk, k=k: e.transpose(out=pm[b][:, kk * P:(kk + 1) * P],
                                                                      in_=xg[sl][:, k * P:(k + 1) * P], identity=ident),
                         reads=[xgt[sl], "consts"], writes=["pm%d" % b])
                S.op("act" if h == 0 else "dve", lambda e, b=b, h=h: (e.copy if h == 0 else e.tensor_copy)(
                    out=hfT[:, h * 4:(h + 1) * 4, :], in_=pm[b][:, :].rearrange("p (k t) -> p k t", k=4)),
                    reads=["pm%d" % b], writes=["hfT"])

        def gu_b(j):
            sl = j % NSL
            for k in range(8):
                S.op("pe", lambda e, k=k: e.matmul(pin[0][:, :], lhsT=hfT[:, k, :], rhs=wgu[sl][:, k * 512:(k + 1) * 512],
                                                   start=(k == 0), stop=(k == 7)),
                     reads=["hfT", "wgu%d" % sl], writes=["pin0"])
            S.op("act", lambda e: e.activation(out=sgS[:, :], in_=pin[0][:, 0:256], func=AF.Silu), reads=["pin0"],
                 writes=["sgS"])
            S.op("dve", lambda e: e.tensor_tensor(out=aS[:, :], in0=sgS[:, :], in1=pin[0][:, 256:512], op=ALU.mult),
                 reads=["sgS", "pin0"], writes=["aS"])

        def at_b(j):
            for c in range(2):
                S.op("pe", lambda e, c=c: e.transpose(out=pin[1][:, c * P:(c + 1) * P], in_=aS[:, c * P:(c + 1) * P],
                                                      identity=ident), reads=["aS", "consts"], writes=["pin1"])
            S.op("act", lambda e: e.copy(out=aT[:, :, :], in_=pin[1][:, 0:2 * P].rearrange("p (c t) -> p c t", c=2)),
                 reads=["pin1"], writes=["aT"])

        def down_b(j):
            sl = j % NSL
            for half in range(2):
                for c in range(2):
                    S.op("pe", lambda e, half=half, c=c: e.matmul(
                        po[:, half * 512:(half + 1) * 512], lhsT=aT[:, c, :],
                        rhs=wdn[sl][:, c * 1024 + half * 512:c * 1024 + (half + 1) * 512],
                        start=(c == 0), stop=(c == 1)), reads=["aT", "wdn%d" % sl], writes=["po"])
            ys = j % 2
            yb, ytok = x2[ys], "x2%d" % ys
            S.op("act", lambda e: e.copy(out=yb[:, 0:512], in_=po[:, 0:512]), reads=["po"], writes=[ytok])
            S.op("dve", lambda e: e.tensor_copy(out=yb[:, 512:1024], in_=po[:, 512:1024]), reads=["po"], writes=[ytok])
            S.op("sp", lambda e: e.dma_start(out=y_d[j * P:(j + 1) * P, :], in_=yb[:, :]), reads=[ytok, "y_d"],
                 dma="ys%d" % ys)

        for j in range(NXS):
            gather_x(j)
        for j in range(NSL):
            gather_gu(j)
            gather_dn(j)
        xt_b(0)
        gu_b(0)
        if NSL < NST:
            gather_gu(NSL)
        for j in range(NST):
            if j + 1 < NST:
                xt_b(j + 1)
            if j >= 1:
                down_b(j - 1)
                if j - 1 + NSL < NST:
                    gather_dn(j - 1 + NSL)
            at_b(j)
            if j + 1 < NST:
                gu_b(j + 1)
                if j + 1 + NSL < NST:
                    gather_gu(j + 1 + NSL)
            if j + NXS < NST:
                gather_x(j + NXS)
        down_b(NST - 1)

        fn_row = yT[:, :, :].rearrange("p k t -> p (k t)")[:, 0:D]
        S.op("sp", lambda e: e.dma_start(out=fn_row, in_=rowv_d[:, D:2 * D]), writes=YT_ALL + ["y_d"], dma="c5")
        wo = [w_out[:, k, :] for k in range(8)]
        NCS = 4
        cy1 = [xt[0][:, :], xt[1][:, :], wo[0], wo[1]]
        cy2 = [x2[0][:, :], x2[1][:, :], wo[2], wo[3]]
        chb = [hft[0][:, :], hft[1][:, :], wo[4], wo[5]]
        cout = [wo[6], wo[7]]
        first_c = set()

        def c_loads(tt):
            sl = tt % NCS
            r0 = tt * P
            extra = ["wgu3", "wdn2", "wdn3"] if (sl >= 2 and sl not in first_c) else []
            first_c.add(sl)
            S.op("pool", lambda e: e.indirect_dma_start(
                out=cy1[sl], out_offset=None, in_=y_d[:, :],
                in_offset=bass.IndirectOffsetOnAxis(ap=yidx[:, tt, 0:1], axis=0)),
                reads=["yidx", "y_d"], writes=["cy1%d" % sl] + (["xt%d" % sl] if sl < 2 else extra), dma="cy1%d" % sl)
            S.op("pool", lambda e: e.indirect_dma_start(
                out=cy2[sl], out_offset=None, in_=y_d[:, :],
                in_offset=bass.IndirectOffsetOnAxis(ap=yidx[:, tt, 1:2], axis=0)),
                reads=["yidx", "y_d"], writes=["cy2%d" % sl] + (["x2%d" % sl] if sl < 2 else extra), dma="cy2%d" % sl)
            S.op("sp", lambda e: e.dma_start(out=chb[sl], in_=h1_d[r0:r0 + P, :]), reads=["h1_d"],
                 writes=["chb%d" % sl] + (["hft%d" % sl] if sl < 2 else extra), dma="ch%d" % sl)

        c_loads(0)
        c_loads(1)
        for tt in range(NTT):
            sl = tt % NCS
            r0 = tt * P
            y1, y1t = cy1[sl], "cy1%d" % sl
            y2, y2t = cy2[sl], "cy2%d" % sl
            hb, hbt = chb[sl], "chb%d" % sl
            ob, obt = cout[tt % 2], "cout%d" % (tt % 2)
            if tt + 2 < NTT:
                c_loads(tt + 2)
            S.op("dve", lambda e, tt=tt, y1=y1, hb=hb: e.scalar_tensor_tensor(
                out=hb, in0=y1, scalar=RT[:, tt, 2:3], in1=hb, op0=ALU.mult, op1=ALU.add),
                reads=[y1t, hbt, "RT"], writes=[hbt])
            S.op("dve", lambda e, tt=tt, y2=y2, hb=hb: e.scalar_tensor_tensor(
                out=hb, in0=y2, scalar=RT[:, tt, 3:4], in1=hb, op0=ALU.mult, op1=ALU.add),
                reads=[y2t, hbt, "RT"], writes=[hbt])
            c = sscol()
            sst = "ss%d" % c
            S.op("act", lambda e, hb=hb, c=c: e.activation(out=junk[:, :], in_=hb, func=AF.Square,
                                                           accum_out=ssb[:, c:c + 1]), reads=[hbt],
                 writes=["junk", sst])
            rstd_ops(ssb[:, c:c + 1], ssb[:, c:c + 1], float(D), [sst], [sst])
            first_o = ["wgu3", "wdn2", "wdn3"] if tt < 2 else []
            S.op("dve", lambda e, hb=hb, c=c, ob=ob: e.scalar_tensor_tensor(
                out=ob, in0=hb, scalar=ssb[:, c:c + 1], in1=fn_row, op0=ALU.mult, op1=ALU.mult),
                reads=[hbt, sst] + YT_ALL, writes=[obt] + first_o)
            S.op("act", lambda e, ob=ob, r0=r0: e.dma_start(out=out_d[r0:r0 + P, :], in_=ob), reads=[obt],
                 dma="os%d" % (tt % 2))
        S.emit()
    return nc


def prep_shared(inp):
    f = np.float32
    w_in = np.ascontiguousarray(inp["w_in"][0], f)
    w_out = np.ascontiguousarray(inp["w_out"][0], f)
    wr = np.ascontiguousarray(np.concatenate([inp["w_router_group"][0], inp["w_router_expert"][0]], axis=1), f)
    poolw = np.ascontiguousarray(np.transpose(inp["pool_w"][0], (1, 0, 2)).reshape(P, 4 * P), f)
    colv = np.zeros((P, 32), f)
    colv[:, 0:8] = inp["norm_mix"][0].reshape(8, P).T
    cw = inp["conv_w"][0]
    for ch in range(4):
        for tap in range(3):
            colv[:, 8 + ch * 3 + tap] = cw[tap, ch * P:(ch + 1) * P]
    colv[:, 20:24] = inp["norm_conv_out"][0].reshape(4, P).T
    colv[:, 24:28] = inp["pool_scale"][0].reshape(4, P).T
    colv[:, 28:32] = inp["norm_pool_out"][0].reshape(4, P).T
    rowv = np.empty((P, 2 * D), f)
    rowv[:, 0:D] = inp["norm_ffn"][0][None, :]
    rowv[:, D:] = inp["final_norm"][None, :]
    consts = np.zeros((P, 3 * P), f)
    consts[:, 0:P] = np.eye(P, dtype=f)
    consts[:, P:2 * P] = np.triu(np.ones((P, P), f), 1)
    consts[:, 2 * P:] = 1.0
    wg, wu, wd = inp["w_gate"][0], inp["w_up"][0], inp["w_down"][0]
    wall_gu = np.empty((NE, P, 8, 512), f)
    wall_gu[:, :, :, 0:256] = wg.reshape(NE, 8, P, 256).transpose(0, 2, 1, 3)
    wall_gu[:, :, :, 256:512] = wu.reshape(NE, 8, P, 256).transpose(0, 2, 1, 3)
    wall_dn = np.ascontiguousarray(wd.reshape(NE, 2, P, D).transpose(0, 2, 1, 3))
    return dict(w_in=w_in, w_out=w_out, wr=wr, poolw=poolw, colv=colv, rowv=rowv, consts=consts,
                wall_gu=wall_gu.reshape(NE * P, 4096), wall_dn=wall_dn.reshape(NE * P, 2048))


def core_inputs(x, meta, shared, NT):
    B, Sq, _ = x.shape
    nh = Sq // NT
    maps = []
    for c in range(B * nh):
        b, h = divmod(c, nh)
        xm = np.ascontiguousarray(x[b, h * NT:(h + 1) * NT], np.float32)
        xp = np.zeros((P, D), np.float32)
        xp[P - HALO:] = meta if h == 0 else x[b, h * NT - HALO:h * NT]
        m = dict(shared)
        m["x"] = xm
        m["xp"] = xp
        maps.append(m)
    return maps


_NC_CACHE = {}


def kernel(x, meta_tokens, norm_mix, w_in, conv_w, norm_conv_out, pool_w, pool_scale, norm_pool_out, w_out,
           norm_ffn, w_router_group, w_router_expert, w_gate, w_up, w_down, final_norm):
    inp = dict(norm_mix=norm_mix, w_in=w_in, conv_w=conv_w, norm_conv_out=norm_conv_out, pool_w=pool_w,
               pool_scale=pool_scale, norm_pool_out=norm_pool_out, w_out=w_out, norm_ffn=norm_ffn,
               w_router_group=w_router_group, w_router_expert=w_router_expert, w_gate=w_gate, w_up=w_up,
               w_down=w_down, final_norm=final_norm)
    inp = {k: np.asarray(v, np.float32) for k, v in inp.items()}
    x = np.asarray(x, np.float32)
    meta = np.asarray(meta_tokens, np.float32)
    B, Sq, _ = x.shape
    NT = B * Sq // NCORES
    shared = prep_shared(inp)
    maps = core_inputs(x, meta, shared, NT)
    if NT not in _NC_CACHE:
        _NC_CACHE[NT] = build(NT)
    res = run_bass_kernel_spmd(_NC_CACHE[NT], maps, core_ids=list(range(NCORES)))
    out = np.empty((B, Sq, D), np.float32)
    nh = Sq // NT
    for c in range(NCORES):
        b, h = divmod(c, nh)
        out[b, h * NT:(h + 1) * NT] = res.results[c]["out"]
    return out
```

```python
import numpy as np
from contextlib import ExitStack
import concourse.bass as bass
import concourse.mybir as mybir
from concourse.bass_utils import run_bass_kernel_spmd

F32 = mybir.dt.float32
I32 = mybir.dt.int32
BF16 = mybir.dt.bfloat16
ALU = mybir.AluOpType
AF = mybir.ActivationFunctionType
AX = mybir.AxisListType

P = 128
D = 1024
NB = 256
HALO = 16
NE = 32
EPS = 1e-6
NCORES = 8
SEQ_PER_CORE = 4096

ENGS = ("pe", "act", "dve", "pool", "sp")


class Sched:
    def __init__(self, nc, stack):
        self.nc = nc
        self.stack = stack
        self.ops = []
        self.last_w = {}
        self.readers = {}

    def op(self, eng, fn, reads=(), writes=(), dma=None):
        i = len(self.ops)
        deps = set()
        for t in tuple(reads) + tuple(writes):
            if t in self.last_w:
                deps.add(self.last_w[t])
        for t in writes:
            r = self.readers.get(t)
            if r:
                deps.update(r[0].values())
                deps.update(r[1])
        deps.discard(i)
        for t in reads:
            r = self.readers.setdefault(t, ({}, []))
            if dma is None:
                r[0][eng] = i
            else:
                r[1].append(i)
        for t in writes:
            self.last_w[t] = i
            self.readers[t] = ({}, [])
        self.ops.append(dict(eng=eng, fn=fn, deps=deps, dma=dma, sig=False))
        return i

    def emit(self):
        nc, ops = self.nc, self.ops
        for o in ops:
            for d in o["deps"]:
                p = ops[d]
                if p["dma"] is None and o["dma"] is None and p["eng"] == "pe" and o["eng"] == "pe":
                    continue
                p["sig"] = True
        eng_sem = {e: self.stack.enter_context(nc.semaphore("prog_" + e)) for e in ENGS}
        cnt = {e: 0 for e in ENGS}
        dma_sems, dcnt = {}, {}
        for o in ops:
            if o["dma"] is not None:
                k = o["dma"]
                if k not in dma_sems:
                    dma_sems[k] = self.stack.enter_context(nc.semaphore("dma_" + k))
                    dcnt[k] = 0
                dcnt[k] += 16
                o["signal"] = (dma_sems[k], dcnt[k], "D" + k)
            elif o["sig"]:
                cnt[o["eng"]] += 1
                o["signal"] = (eng_sem[o["eng"]], cnt[o["eng"]], "E" + o["eng"])
        per_eng = {e: [] for e in ENGS}
        for o in ops:
            per_eng[o["eng"]].append(o)
        block = self.stack.enter_context(nc.Block())

        def run(engname):
            def body(eng):
                w = {}
                for o in per_eng[engname]:
                    need = {}
                    for d in o["deps"]:
                        p = ops[d]
                        if "signal" not in p:
                            continue
                        sem, val, key = p["signal"]
                        if need.get(key, (None, 0))[1] < val:
                            need[key] = (sem, val)
                    for key, (sem, val) in need.items():
                        if w.get(key, 0) >= val:
                            continue
                        eng.wait_ge(sem, val)
                        w[key] = val
                    ins = o["fn"](eng)
                    if "signal" in o:
                        ins.then_inc(o["signal"][0], 16 if o["dma"] is not None else 1)
                if engname == "sp":
                    for k, sem in dma_sems.items():
                        eng.wait_ge(sem, dcnt[k])
            return body

        block.tensor(run("pe"))
        block.scalar(run("act"))
        block.vector(run("dve"))
        block.gpsimd(run("pool"))
        block.sync(run("sp"))


def build(NT, MM_DT=F32):
    NTT = NT // P
    NBLK = NT // NB
    NST = 2 * NTT + NE
    SUB = NB // P
    nc = bass.Bass("TRN2", target_bir_lowering=False)

    def din(name, shape, dt=F32):
        return nc.dram_tensor(name, shape, dt, kind="ExternalInput").ap()

    x_d = din("x", [NT, D])
    xp_d = din("xp", [P, D])
    w_in_d = din("w_in", [D, 2048])
    w_out_d = din("w_out", [D, D])
    wr_d = din("wr", [D, 36])
    poolw_d = din("poolw", [P, 4 * P])
    colv_d = din("colv", [P, 32])
    rowv_d = din("rowv", [P, 2 * D])
    consts_d = din("consts", [P, 3 * P])
    wgu_d = din("wall_gu", [NE * P, 4096])
    wdn_d = din("wall_dn", [NE * P, 2048])
    out_d = nc.dram_tensor("out", [NT, D], F32, kind="ExternalOutput").ap()
    hf_d = nc.dram_tensor("hf_scr", [NT, D], F32, kind="Internal").ap()
    h1_d = nc.dram_tensor("h1_scr", [NT, D], F32, kind="Internal").ap()
    y_d = nc.dram_tensor("y_scr", [NST * P, D], F32, kind="Internal").ap()

    with ExitStack() as st:
        def sb(name, shape, dt=F32):
            return st.enter_context(nc.sbuf_tensor("s_" + name, shape, dt))

        def psum(name, shape):
            return st.enter_context(nc.psum_tensor("p_" + name, shape, F32))

        S = Sched(nc, st)
        big = sb("big", [P, 16384])
        w_in = big[:, :].rearrange("p (k c) -> p k c", k=8)
        w_out = sb("w_out", [P, 8, D])
        wr = sb("wr_sb", [P, 8, 36])
        poolw = sb("poolw_sb", [P, 4, P])
        poolw_s = sb("poolw_s", [P, 4, P])
        poolw_n = sb("poolw_n", [P, 4, P])
        colv = sb("colv_sb", [P, 32])
        rowv = sb("rowv_sb", [P, D])
        consts = sb("consts_sb", [P, 3 * P])
        ident, utri, ones = consts[:, 0:P], consts[:, P:2 * P], consts[:, 2 * P:3 * P]
        xt = [sb("xt0", [P, D]), sb("xt1", [P, D])]
        x2 = [sb("x20", [P, D]), sb("x21", [P, D])]
        hft = [sb("hft0", [P, D]), sb("hft1", [P, D])]
        junk = sb("junk", [P, D], BF16)
        hnT = sb("hnT", [P, 8, NB])
        bS = sb("bS", [P, 4, NB])
        cS = sb("cS", [P, 4, NB])
        vS = sb("vS", [P, 4, NB])
        uS = sb("uS", [P, 4, NB + HALO])
        vpS = sb("vpS", [P, 4, NB + HALO])
        acc = [sb("acc0", [P, NB]), sb("acc1", [P, NB])]
        sq4 = sb("sq4", [P, 4, NB])
        pooled4 = sb("pooled4", [P, 4, NB])
        tA = sb("tA", [P, NB + HALO])
        tB = sb("tB", [P, NB + HALO])
        rsn = sb("rsn", [P, 2, NB])
        yT = sb("yT", [P, 8, NB])
        hfT = sb("hfT", [P, 8, P])
        sm2 = sb("sm", [P, 2, 256])
        lgs = sb("lgs", [P, 2, 36])
        m12a = sb("m12", [P, 2, NE])
        srun = sb("srun", [P, NE])
        M1all = sb("M1all", [P, NTT, NE], BF16)
        M2all = sb("M2all", [P, NTT, NE], BF16)
        RT = sb("RT", [P, NTT, 4])
        ssb = sb("ssb", [P, 16])
        fin = hft[1]
        yidx = sb("yidx", [P, NTT, 2], I32)
        toki = sb("toki", [P, NST], I32)
        bar = sb("bar", [P, 2])
        idxw = sb("idxw", [P, NST], I32)
        sgS = sb("sgS", [P, 256])
        aS = sb("aS", [P, 256])
        aT = sb("aT", [P, 2, P])

        pin = [psum("pin%d" % i, [P, 512]) for i in range(3)]
        pm = [psum("pm%d" % i, [P, 512]) for i in range(3)]
        po = psum("po", [P, 1024])
        mctr = [0]

        def mbank():
            i = mctr[0] % 3
            mctr[0] += 1
            return i

        ssc = [0]

        def sscol():
            i = ssc[0] % 16
            ssc[0] += 1
            return i

        S.op("sp", lambda e: e.dma_start(out=consts[:, :], in_=consts_d[:, :]), writes=["consts"], dma="c0")
        S.op("sp", lambda e: e.dma_start(out=colv[:, :], in_=colv_d[:, :]), writes=["colv"], dma="c1")
        def setup_weights():
            for k in range(8):
                S.op("sp", lambda e, k=k: e.dma_start(out=w_in[:, k, :], in_=w_in_d[k * P:(k + 1) * P, :]),
                     writes=["w_in%d" % k], dma="win%d" % k)
                if k % 2:
                    S.op("act", lambda e, k=k: e.mul(out=w_in[:, k, :], in_=w_in[:, k, :], mul=colv[:, k:k + 1]),
                         reads=["colv"], writes=["w_in%d" % k])
                else:
                    S.op("dve", lambda e, k=k: e.tensor_scalar(out=w_in[:, k, :], in0=w_in[:, k, :],
                                                              scalar1=colv[:, k:k + 1], scalar2=None, op0=ALU.mult),
                         reads=["colv"], writes=["w_in%d" % k])
            S.op("sp", lambda e: e.dma_start(out=poolw[:, :, :], in_=poolw_d[:, :].rearrange("p (g d) -> p g d", g=4)),
                 writes=["poolw"], dma="c2")
            for g in range(4):
                S.op("dve", lambda e, g=g: e.tensor_scalar(out=poolw_s[:, g, :], in0=poolw[:, g, :], scalar1=1.0 / (2 << g),
                                                          scalar2=None, op0=ALU.mult), reads=["poolw"], writes=["poolw_s"])
            S.op("dve", lambda e: e.tensor_scalar(out=poolw_n[:, :, :], in0=poolw[:, :, :], scalar1=-1.0, scalar2=None,
                                                  op0=ALU.mult), reads=["poolw"], writes=["poolw_n"])
            S.op("sp", lambda e: e.dma_start(out=wr[:, :, :], in_=wr_d[:, :].rearrange("(k p) n -> p k n", p=P)),
                 writes=["wr"], dma="c3")
            S.op("sp", lambda e: e.dma_start(out=rowv[:, :], in_=rowv_d[:, 0:D]), writes=["rowv"], dma="c4")
            S.op("dve", lambda e: e.memset(srun[:, :], 0.0), writes=["srun"])
        WIN = ["w_in%d" % k for k in range(8)]

        def rstd_ops(src_ap, dst_ap, n, rtok, wtok):
            S.op("act", lambda e: e.activation(out=dst_ap, in_=src_ap, func=AF.Ln, bias=EPS, scale=1.0 / n),
                 reads=rtok, writes=wtok)
            S.op("act", lambda e: e.activation(out=dst_ap, in_=dst_ap, func=AF.Exp, scale=-0.5), reads=wtok,
                 writes=wtok)

        def stage_a_act(src_rows, slot):
            xs = xt[slot]
            tk = "xt%d" % slot
            S.op("sp", lambda e: e.dma_start(out=xs[:, :], in_=src_rows), writes=[tk], dma=tk)
            c = sscol()
            sst = "ss%d" % c
            S.op("act", lambda e: e.activation(out=junk[:, :], in_=xs[:, :], func=AF.Square,
                                               accum_out=ssb[:, c:c + 1]), reads=[tk], writes=["junk", sst])
            rstd_ops(ssb[:, c:c + 1], ssb[:, c:c + 1], float(D), [sst], [sst])
            S.op("act", lambda e: e.mul(out=xs[:, :], in_=xs[:, :], mul=ssb[:, c:c + 1]), reads=[sst], writes=[tk])

        def stage_a_tr(slot, col0):
            xs = xt[slot]
            tk = "xt%d" % slot
            for h in range(2):
                b = mbank()
                for kk in range(4):
                    k = h * 4 + kk
                    S.op("pe", lambda e, b=b, kk=kk, k=k: e.transpose(out=pm[b][:, kk * P:(kk + 1) * P],
                                                                      in_=xs[:, k * P:(k + 1) * P], identity=ident),
                         reads=[tk, "consts"], writes=["pm%d" % b])
                S.op("act", lambda e, b=b, h=h: e.copy(out=hnT[:, h * 4:(h + 1) * 4, col0:col0 + P],
                                                       in_=pm[b][:, :].rearrange("p (k t) -> p k t", k=4)),
                     reads=["pm%d" % b], writes=["hnT"])

        def stage_a(src_rows, slot, col0):
            stage_a_act(src_rows, slot)
            stage_a_tr(slot, col0)

        pinc = [0]

        def inproj_group(g, ncols, prefix=False):
            b = pinc[0] % 3
            pinc[0] += 1
            for cc in range(2):
                c = 2 * g + cc
                for k in range(8):
                    S.op("pe", lambda e, b=b, cc=cc, c=c, k=k: e.matmul(
                        pin[b][:, cc * ncols:(cc + 1) * ncols], lhsT=w_in[:, k, c * P:(c + 1) * P],
                        rhs=hnT[:, k, 0:ncols], start=(k == 0), stop=(k == 7)),
                        reads=["hnT", WIN[k]], writes=["pin%d" % b])
            src = pin[b][:, 0:2 * ncols].rearrange("p (c t) -> p c t", c=2)
            j = 2 * (g % 2)
            if not prefix:
                if g < 2:
                    dst, tok = bS[:, j:j + 2, :], "bS"
                elif g < 4:
                    dst, tok = cS[:, j:j + 2, :], "cS"
                elif g < 6:
                    dst, tok = vS[:, j:j + 2, :], "vS"
                else:
                    dst, tok = vpS[:, j:j + 2, HALO:HALO + NB], "vpm"
                S.op("act", lambda e: e.copy(out=dst, in_=src), reads=["pin%d" % b], writes=[tok])
            else:
                srch = src[:, :, ncols - HALO:ncols]
                if g in (2, 3):
                    S.op("act", lambda e: e.copy(out=cS[:, j:j + 2, 0:HALO], in_=srch), reads=["pin%d" % b], writes=["cS"])
                elif g in (4, 5):
                    S.op("dve", lambda e: e.tensor_tensor(out=uS[:, j:j + 2, 0:HALO], in0=cS[:, j:j + 2, 0:HALO],
                                                          in1=srch, op=ALU.mult),
                         reads=["pin%d" % b, "cS"], writes=["uh"])
                else:
                    S.op("act", lambda e: e.copy(out=vpS[:, j:j + 2, 0:HALO], in_=srch), reads=["pin%d" % b], writes=["vph"])

        def mix_part1():
            S.op("dve", lambda e: e.tensor_tensor(out=uS[:, :, HALO:HALO + NB], in0=cS[:, :, :], in1=vS[:, :, :],
                                                  op=ALU.mult), reads=["cS", "vS"], writes=["um"])
            for ch in range(4):
                a = acc[ch % 2]
                at = "acc%d" % (ch % 2)
                S.op("dve", lambda e, a=a, ch=ch: e.tensor_scalar(out=a[:, :], in0=uS[:, ch, HALO - 2:HALO - 2 + NB],
                                                                 scalar1=colv[:, 8 + ch * 3:9 + ch * 3], scalar2=None,
                                                                 op0=ALU.mult), reads=["um", "uh", "colv"], writes=[at])
                for tap in (1, 2):
                    S.op("dve", lambda e, a=a, ch=ch, tap=tap: e.scalar_tensor_tensor(
                        out=a[:, :], in0=uS[:, ch, HALO - 2 + tap:HALO - 2 + tap + NB],
                        scalar=colv[:, 8 + ch * 3 + tap:9 + ch * 3 + tap], in1=a[:, :], op0=ALU.mult, op1=ALU.add),
                        reads=["um", "uh", at], writes=[at])
                S.op("dve", lambda e, a=a, ch=ch: e.tensor_tensor(out=yT[:, ch, :], in0=bS[:, ch, :], in1=a[:, :],
                                                                 op=ALU.mult), reads=["bS", at], writes=["yTc%d" % ch])
            S.op("dve", lambda e: e.tensor_copy(out=uS[:, :, HALO - 2:HALO], in_=uS[:, :, NB + HALO - 2:NB + HALO]),
                 reads=["um"], writes=["uh"])
            W = NB + HALO
            for g in range(4):
                w = 2 << g
                src = vpS[:, g, :]
                cur, curtok = src, None
                lo_needed = HALO
                lvl = 1
                bufs = [tA, tB]
                bi = 0
                while lvl < w:
                    nl = lvl * 2
                    lo = HALO - (w - nl)
                    last = nl == w
                    rt = ["vpm", "vph"] if curtok is None else [curtok]
                    if last:
                        S.op("pool", lambda e, cur=cur, lvl=lvl, g=g: e.tensor_tensor(
                            out=pooled4[:, g, :], in0=cur[:, HALO:W], in1=cur[:, HALO - lvl:W - lvl], op=ALU.add),
                            reads=rt, writes=["pooled%d" % g])
                    else:
                        dstb = bufs[bi]
                        dtok = "tA" if bi == 0 else "tB"
                        S.op("pool", lambda e, dstb=dstb, cur=cur, lo=lo, lvl=lvl: e.tensor_tensor(
                            out=dstb[:, lo:W], in0=cur[:, lo:W], in1=cur[:, lo - lvl:W - lvl], op=ALU.add),
                            reads=rt, writes=[dtok])
                        cur, curtok = dstb, dtok
                        bi ^= 1
                    lvl = nl
            S.op("pool", lambda e: e.tensor_copy(out=vpS[:, :, 0:HALO], in_=vpS[:, :, NB:NB + HALO]),
                 reads=["vpm"], writes=["vph"])

        def mix_part1b():
            for g in range(4):
                pb = pooled4[:, g, :]
                pt = "pooled%d" % g
                if g % 2 == 0:
                    mix_part1.bank[g // 2] = mbank()
                mb_ = mix_part1.bank[g // 2]
                S.op("pe", lambda e, mb_=mb_, g=g, pb=pb: e.matmul(pm[mb_][:, (g % 2) * NB:(g % 2 + 1) * NB],
                                                                   lhsT=poolw_s[:, g, :], rhs=pb, start=True, stop=False),
                     reads=[pt, "poolw_s"], writes=["pm%d" % mb_])
                S.op("pe", lambda e, mb_=mb_, g=g: e.matmul(pm[mb_][:, (g % 2) * NB:(g % 2 + 1) * NB],
                                                            lhsT=poolw_n[:, g, :], rhs=vpS[:, g, HALO:HALO + NB],
                                                            start=False, stop=True),
                     reads=["vpm", "poolw_n"], writes=["pm%d" % mb_])
            for g in range(4):
                mb_ = mix_part1.bank[g // 2]
                S.op("dve", lambda e, mb_=mb_, g=g: e.tensor_scalar(out=yT[:, 4 + g, :],
                                                                     in0=pm[mb_][:, (g % 2) * NB:(g % 2 + 1) * NB],
                                                                     scalar1=colv[:, 24 + g:25 + g], scalar2=None,
                                                                     op0=ALU.mult),
                     reads=["pm%d" % mb_, "colv"], writes=["yTp%d" % g])

        mix_part1.bank = [0, 0]

        mix2_bank = [0]

        def mix2_pre(half):
            pre = "yTc" if half == 0 else "yTp"
            S.op("act", lambda e: e.activation(out=sq4[:, :, :], in_=yT[:, half * 4:half * 4 + 4, :], func=AF.Square),
                 reads=[pre + "%d" % i for i in range(4)], writes=["sq4"])
            ab = acc[half]
            S.op("dve", lambda e: e.tensor_reduce(out=ab[:, :], in_=sq4[:, :, :].rearrange("p c t -> p t c"),
                                                  axis=AX.X, op=ALU.add), reads=["sq4"], writes=["acc%d" % half])

        def mix2_mm():
            sb_ = mbank()
            for half in range(2):
                S.op("pe", lambda e, half=half: e.matmul(pm[sb_][:, half * NB:(half + 1) * NB], lhsT=ones,
                                                         rhs=acc[half][:, :], start=True, stop=True),
                     reads=["acc%d" % half, "consts"], writes=["pm%d" % sb_])
            for half in range(2):
                rstd_ops(pm[sb_][:, half * NB:(half + 1) * NB], rsn[:, half, :], 512.0, ["pm%d" % sb_], ["rsn%d" % half])

        def mix_part3():
            for half, pre, gcol in ((0, "yTc", 20), (1, "yTp", 28)):
                for ch in range(4):
                    S.op("dve", lambda e, half=half, ch=ch, gcol=gcol: e.scalar_tensor_tensor(
                        out=yT[:, half * 4 + ch, :], in0=yT[:, half * 4 + ch, :], scalar=colv[:, gcol + ch:gcol + ch + 1],
                        in1=rsn[:, half, :], op0=ALU.mult, op1=ALU.mult),
                        reads=[pre + "%d" % ch, "rsn%d" % half, "colv"], writes=[pre + "%d" % ch])

        YT_ALL = ["yTc%d" % i for i in range(4)] + ["yTp%d" % i for i in range(4)]

        def stage_d(tt, s):
            r0 = tt * P
            slot = tt % 2
            xb, xtok = x2[slot], "x2%d" % slot
            hb, htok = hft[slot], "hft%d" % slot
            S.op("sp", lambda e: e.dma_start(out=xb[:, :], in_=x_d[r0:r0 + P, :]), writes=[xtok], dma=xtok)
            for half in range(2):
                for k in range(8):
                    S.op("pe", lambda e, half=half, k=k: e.matmul(
                        po[:, half * 512:(half + 1) * 512], lhsT=yT[:, k, s * P:(s + 1) * P],
                        rhs=w_out[:, k, half * 512:(half + 1) * 512], start=(k == 0), stop=(k == 7)),
                        reads=YT_ALL + ["w_out"], writes=["po"])
            S.op("dve", lambda e: e.tensor_tensor(out=xb[:, :], in0=xb[:, :], in1=po[:, :], op=ALU.add),
                 reads=["po", xtok], writes=[xtok])
            c = sscol()
            sst = "ss%d" % c
            S.op("act", lambda e: e.activation(out=junk[:, :], in_=xb[:, :], func=AF.Square,
                                               accum_out=ssb[:, c:c + 1]), reads=[xtok], writes=["junk", sst])
            rstd_ops(ssb[:, c:c + 1], ssb[:, c:c + 1], float(D), [sst], [sst])
            S.op("dve", lambda e: e.scalar_tensor_tensor(out=hb[:, :], in0=xb[:, :], scalar=ssb[:, c:c + 1],
                                                         in1=rowv[:, :], op0=ALU.mult, op1=ALU.mult),
                 reads=[xtok, sst, "rowv"], writes=[htok])
            S.op("act", lambda e: e.dma_start(out=h1_d[r0:r0 + P, :], in_=xb[:, :]), reads=[xtok, "h1_d"],
                 dma="h1s%d" % slot)
            S.op("act", lambda e: e.dma_start(out=hf_d[r0:r0 + P, :], in_=hb[:, :]), reads=[htok, "hf_d"],
                 dma="hfs%d" % slot)

        def stage_dtr(tt, s):
            slot = tt % 2
            hb, htok = hft[slot], "hft%d" % slot
            for h in range(2):
                b = mbank()
                for kk in range(4):
                    k = h * 4 + kk
                    S.op("pe", lambda e, b=b, kk=kk, k=k: e.transpose(out=pm[b][:, kk * P:(kk + 1) * P],
                                                                      in_=hb[:, k * P:(k + 1) * P], identity=ident),
                         reads=[htok, "consts"], writes=["pm%d" % b])
                S.op("act", lambda e, b=b, h=h: e.copy(out=hfT[:, h * 4:(h + 1) * 4, :],
                                                       in_=pm[b][:, :].rearrange("p (k t) -> p k t", k=4)),
                     reads=["pm%d" % b], writes=["hfT"])

        def stage_drt(tt, s):
            rb = mbank()
            rbt = "pm%d" % rb
            for k in range(8):
                S.op("pe", lambda e, k=k: e.matmul(pm[rb][:, 0:36], lhsT=hfT[:, k, :], rhs=wr[:, k, :], start=(k == 0),
                                                   stop=(k == 7)), reads=["hfT", "wr"], writes=[rbt])
            S.op("dve", lambda e: e.tensor_copy(out=lgs[:, s, :], in_=pm[rb][:, 0:36]), reads=[rbt],
                 writes=["lg%d" % s])

        def stage_d2(tt, s):
            lg = lgs[:, s, :]
            sm = sm2[:, s, :]
            m12 = m12a[:, s, :]
            M12T = "m12_%d" % s
            gmax, ngmax, sume, g1 = sm[:, 40:41], sm[:, 41:42], sm[:, 42:43], sm[:, 43:44]
            goh = sm[:, 44:48]
            eg = sm[:, 48:52]
            sel = sm[:, 56:64]
            top8 = sm[:, 64:72]
            mask2 = sm[:, 72:80]
            mask1 = sm[:, 80:88]
            dd, ed = sm[:, 88:89], sm[:, 89:90]
            prod = sm[:, 96:128]
            m1f = sm[:, 128:160]
            m2f = sm[:, 160:192]
            T = "sm%d" % s
            S.op("dve", lambda e: e.tensor_reduce(out=gmax, in_=lg[:, 0:4], axis=AX.X, op=ALU.max),
                 reads=[T, "lg%d" % s], writes=[T])
            S.op("dve", lambda e: e.tensor_scalar(out=goh, in0=lg[:, 0:4], scalar1=gmax, scalar2=None, op0=ALU.is_ge),
                 reads=[T], writes=[T])
            S.op("dve", lambda e: e.tensor_scalar(out=ngmax, in0=gmax, scalar1=-1.0, scalar2=None, op0=ALU.mult),
                 reads=[T], writes=[T])
            S.op("act", lambda e: e.activation(out=eg, in_=lg[:, 0:4], func=AF.Exp, bias=ngmax, accum_out=sume),
                 reads=[T], writes=[T])
            S.op("dve", lambda e: e.reciprocal(out=g1, in_=sume), reads=[T], writes=[T])
            S.op("dve", lambda e: e.tensor_scalar(out=sel, in0=lg[:, 4:12], scalar1=goh[:, 0:1], scalar2=None,
                                                  op0=ALU.mult), reads=[T], writes=[T])
            for g in range(1, 4):
                S.op("dve", lambda e, g=g: e.scalar_tensor_tensor(out=sel, in0=lg[:, 4 + 8 * g:12 + 8 * g],
                                                                  scalar=goh[:, g:g + 1], in1=sel, op0=ALU.mult,
                                                                  op1=ALU.add), reads=[T], writes=[T])
            S.op("dve", lambda e: e.max(out=top8, in_=sel), reads=[T], writes=[T])
            S.op("dve", lambda e: e.tensor_scalar(out=mask2, in0=sel, scalar1=top8[:, 1:2], scalar2=None, op0=ALU.is_ge),
                 reads=[T], writes=[T])
            S.op("dve", lambda e: e.tensor_scalar(out=mask1, in0=sel, scalar1=top8[:, 0:1], scalar2=None, op0=ALU.is_ge),
                 reads=[T], writes=[T])
            S.op("dve", lambda e: e.tensor_tensor(out=dd, in0=top8[:, 1:2], in1=top8[:, 0:1], op=ALU.subtract),
                 reads=[T], writes=[T])
            S.op("act", lambda e: e.activation(out=ed, in_=dd, func=AF.Exp), reads=[T], writes=[T])
            S.op("dve", lambda e: e.tensor_scalar(out=ed, in0=ed, scalar1=1.0, scalar2=None, op0=ALU.add),
                 reads=[T], writes=[T])
            S.op("dve", lambda e: e.reciprocal(out=ed, in_=ed), reads=[T], writes=[T])
            S.op("dve", lambda e: e.tensor_tensor(out=RT[:, tt, 2:3], in0=g1, in1=ed, op=ALU.mult), reads=[T],
                 writes=["RT"])
            S.op("dve", lambda e: e.tensor_tensor(out=RT[:, tt, 3:4], in0=g1, in1=RT[:, tt, 2:3], op=ALU.subtract),
                 reads=[T, "RT"], writes=["RT"])
            for g in range(4):
                S.op("dve", lambda e, g=g: e.tensor_scalar(out=m12[:, 8 * g:8 * g + 8], in0=mask2, scalar1=goh[:, g:g + 1],
                                                           scalar2=None, op0=ALU.mult), reads=[T], writes=[M12T])
                S.op("dve", lambda e, g=g: e.tensor_scalar(out=m1f[:, 8 * g:8 * g + 8], in0=mask1, scalar1=goh[:, g:g + 1],
                                                           scalar2=None, op0=ALU.mult), reads=[T], writes=[T])
            S.op("dve", lambda e: e.tensor_tensor(out=m2f, in0=m12, in1=m1f, op=ALU.subtract),
                 reads=[T, M12T], writes=[T])
            S.op("dve", lambda e: e.tensor_copy(out=M1all[:, tt, :], in_=m1f), reads=[T], writes=["Mall"])
            S.op("dve", lambda e: e.tensor_copy(out=M2all[:, tt, :], in_=m2f), reads=[T], writes=["Mall"])

        def stage_d2b(tt, s):
            lg = lgs[:, s, :]
            sm = sm2[:, s, :]
            m12 = m12a[:, s, :]
            M12T = "m12_%d" % s
            gmax, ngmax, sume, g1 = sm[:, 40:41], sm[:, 41:42], sm[:, 42:43], sm[:, 43:44]
            goh = sm[:, 44:48]
            eg = sm[:, 48:52]
            sel = sm[:, 56:64]
            top8 = sm[:, 64:72]
            mask2 = sm[:, 72:80]
            mask1 = sm[:, 80:88]
            dd, ed = sm[:, 88:89], sm[:, 89:90]
            prod = sm[:, 96:128]
            m1f = sm[:, 128:160]
            m2f = sm[:, 160:192]
            T = "sm%d" % s
            kb = mbank()
            kbt = "pm%d" % kb
            S.op("pe", lambda e: e.matmul(pm[kb][:, 0:NE], lhsT=utri, rhs=m12, start=True, stop=False),
                 reads=[M12T, "consts"], writes=[kbt])
            S.op("pe", lambda e: e.matmul(pm[kb][:, 0:NE], lhsT=ones, rhs=srun[:, :], start=False, stop=True),
                 reads=["srun", "consts"], writes=[kbt])
            for kk, mf in ((0, m1f), (1, m2f)):
                S.op("dve", lambda e, mf=mf: e.tensor_tensor(out=prod, in0=mf, in1=pm[kb][:, 0:NE], op=ALU.mult),
                     reads=[T, kbt], writes=[T])
                S.op("dve", lambda e, kk=kk: e.tensor_reduce(out=RT[:, tt, kk:kk + 1], in_=prod, axis=AX.X, op=ALU.add),
                     reads=[T], writes=["RT"])
            S.op("dve", lambda e: e.tensor_tensor(out=srun[:, :], in0=srun[:, :], in1=m12, op=ALU.add),
                 reads=[M12T, "srun"], writes=["srun"])

        stage_a_act(xp_d[:, :], 0)
        setup_weights()
        stage_a_tr(0, 0)
        for g in range(2, 8):
            inproj_group(g, P, prefix=True)
        for s in range(SUB):
            stage_a(x_d[s * P:(s + 1) * P, :], s % 2, s * P)
        for g in range(8):
            inproj_group(g, NB)
        for k in range(8):
            S.op("sp", lambda e, k=k: e.dma_start(out=w_out[:, k, :], in_=w_out_d[k * P:(k + 1) * P, :]),
                 writes=["w_out"], dma="wout%d" % k)
        def a_act_block(bi):
            for s in range(SUB):
                r0 = bi * NB + s * P
                stage_a_act(x_d[r0:r0 + P, :], s % 2)

        if NBLK > 1:
            a_act_block(1)
        pending = [None]
        for i in range(NBLK):
            nxt = i + 1 < NBLK
            if nxt:
                for s in range(SUB):
                    stage_a_tr(s % 2, s * P)
            if pending[0] is not None:
                stage_drt(*pending[0])
                pending[0] = None
            mix_part1()
            mix2_pre(0)
            if i > 0:
                stage_d2((i - 1) * SUB, 0)
            if nxt:
                inproj_group(0, NB)
                inproj_group(1, NB)
            mix_part1b()
            mix2_pre(1)
            if i > 0:
                stage_d2((i - 1) * SUB + 1, 1)
            if nxt:
                inproj_group(2, NB)
                inproj_group(3, NB)
            mix2_mm()
            if i + 2 < NBLK:
                a_act_block(i + 2)
            if nxt:
                inproj_group(4, NB)
            mix_part3()
            if i > 0:
                stage_d2b((i - 1) * SUB, 0)
            if nxt:
                inproj_group(5, NB)
            if i > 0:
                stage_d2b((i - 1) * SUB + 1, 1)
            assert SUB == 2
            stage_d(i * SUB, 0)
            if nxt:
                inproj_group(6, NB)
            stage_dtr(i * SUB, 0)
            stage_d(i * SUB + 1, 1)
            stage_drt(i * SUB, 0)
            if nxt:
                inproj_group(7, NB)
            stage_dtr(i * SUB + 1, 1)
            pending[0] = (i * SUB + 1, 1)
        stage_drt(*pending[0])
        for s in range(SUB):
            stage_d2((NBLK - 1) * SUB + s, s)
        for s in range(SUB):
            stage_d2b((NBLK - 1) * SUB + s, s)

        cb = mbank()
        cbt = "pm%d" % cb
        S.op("pe", lambda e: e.matmul(pm[cb][:, 0:NE], lhsT=ones, rhs=srun[:, :], start=True, stop=True),
             reads=["srun", "consts"], writes=[cbt])
        F = "hft1"
        cnt = fin[:, 0:32]
        tl = fin[:, 32:64]
        ca = fin[:, 64:96]
        cb2 = fin[:, 96:128]
        tstart = fin[:, 128:160]
        S.op("dve", lambda e: e.tensor_copy(out=cnt, in_=pm[cb][:, 0:NE]), reads=[cbt], writes=[F])
        S.op("dve", lambda e: e.tensor_scalar(out=tl, in0=cnt, scalar1=0.0, scalar2=None, op0=ALU.is_gt), reads=[F],
             writes=[F])
        for m in range(1, NTT):
            S.op("dve", lambda e, m=m: e.scalar_tensor_tensor(out=tl, in0=cnt, scalar=float(P * m), in1=tl,
                                                              op0=ALU.is_gt, op1=ALU.add), reads=[F], writes=[F])
        S.op("dve", lambda e: e.tensor_copy(out=ca, in_=tl), reads=[F], writes=[F])
        cur, oth = ca, cb2
        sh = 1
        while sh < NE:
            S.op("dve", lambda e, cur=cur, oth=oth, sh=sh: e.tensor_copy(out=oth[:, 0:sh], in_=cur[:, 0:sh]), reads=[F],
                 writes=[F])
            S.op("dve", lambda e, cur=cur, oth=oth, sh=sh: e.tensor_tensor(out=oth[:, sh:NE], in0=cur[:, sh:NE],
                                                                           in1=cur[:, 0:NE - sh], op=ALU.add),
                 reads=[F], writes=[F])
            cur, oth = oth, cur
            sh *= 2
        tend = cur
        S.op("dve", lambda e: e.tensor_tensor(out=tstart, in0=tend, in1=tl, op=ALU.subtract), reads=[F], writes=[F])
        tsl = fin[:, 160:160 + 2 * NTT].rearrange("p (t k) -> p t k", k=2)
        flr = fin[:, 256:256 + 2 * NTT].rearrange("p (t k) -> p t k", k=2)
        sres = fin[:, 384:384 + 2 * NTT].rearrange("p (t k) -> p t k", k=2)
        tmp = fin[:, 512:512 + 2 * NTT].rearrange("p (t k) -> p t k", k=2)
        prodA = hft[0][:, 0:NTT * NE].rearrange("p (t e) -> p t e", e=NE)
        tsb = tstart.unsqueeze(1).broadcast_to([P, NTT, NE])
        for kk, Mx in ((0, M1all), (1, M2all)):
            S.op("dve", lambda e, Mx=Mx: e.tensor_tensor(out=prodA, in0=Mx[:, :, :], in1=tsb, op=ALU.mult),
                 reads=[F, "Mall", "hft0"], writes=["hft0"])
            S.op("dve", lambda e, kk=kk: e.tensor_reduce(out=tsl[:, :, kk], in_=prodA, axis=AX.X, op=ALU.add),
                 reads=["hft0"], writes=[F])
        rank = RT[:, :, 0:2]
        S.op("dve", lambda e: e.tensor_scalar(out=flr, in0=rank, scalar1=float(P), scalar2=None, op0=ALU.is_ge),
             reads=["RT", F], writes=[F])
        for m in range(2, NTT):
            S.op("dve", lambda e, m=m: e.scalar_tensor_tensor(out=flr, in0=rank, scalar=float(P * m), in1=flr,
                                                              op0=ALU.is_ge, op1=ALU.add), reads=["RT", F], writes=[F])
        S.op("dve", lambda e: e.tensor_tensor(out=tsl, in0=tsl, in1=flr, op=ALU.add), reads=[F], writes=[F])
        S.op("dve", lambda e: e.scalar_tensor_tensor(out=sres, in0=flr, scalar=-float(P), in1=rank, op0=ALU.mult,
                                                     op1=ALU.add), reads=[F, "RT"], writes=[F])
        S.op("dve", lambda e: e.scalar_tensor_tensor(out=tmp, in0=tsl, scalar=float(P), in1=sres, op0=ALU.mult,
                                                     op1=ALU.add), reads=[F], writes=[F])
        S.op("dve", lambda e: e.tensor_copy(out=yidx[:, :, :], in_=tmp), reads=[F], writes=["yidx"])
        jio = fin[:, 640:640 + NST]
        ej = fin[:, 768:768 + NST]
        pio = fin[:, 900:901]
        S.op("pool", lambda e: e.iota(jio, pattern=[[1, NST]], base=0, channel_multiplier=0,
                                      allow_small_or_imprecise_dtypes=True), writes=[F])
        S.op("pool", lambda e: e.iota(pio, pattern=[[0, 1]], base=0, channel_multiplier=1,
                                      allow_small_or_imprecise_dtypes=True), writes=[F])
        S.op("dve", lambda e: e.tensor_scalar(out=ej, in0=jio, scalar1=tend[:, 0:1], scalar2=None, op0=ALU.is_ge),
             reads=[F], writes=[F])
        for ee in range(1, NE):
            S.op("dve", lambda e, ee=ee: e.scalar_tensor_tensor(out=ej, in0=jio, scalar=tend[:, ee:ee + 1], in1=ej,
                                                                op0=ALU.is_ge, op1=ALU.add), reads=[F],
                 writes=[F])
        S.op("dve", lambda e: e.tensor_scalar(out=ej, in0=ej, scalar1=float(NE - 1), scalar2=float(P), op0=ALU.min,
                                              op1=ALU.mult), reads=[F], writes=[F])
        S.op("dve", lambda e: e.tensor_scalar(out=ej, in0=ej, scalar1=pio, scalar2=None, op0=ALU.add),
             reads=[F], writes=[F])
        S.op("dve", lambda e: e.tensor_copy(out=idxw[:, :], in_=ej), reads=[F], writes=["idxw"])
        hflat = hnT[:, :, :].rearrange("p k t -> p (k t)")
        OS = [hflat[:, 0:P], hflat[:, P:2 * P]]
        OT = [hflat[:, 256:256 + NST], hflat[:, 256 + NST:256 + 2 * NST]]
        io128 = hflat[:, 512:512 + P]
        tokf = hflat[:, 640:640 + NTT]
        S.op("pool", lambda e: e.iota(io128, pattern=[[1, P]], base=0, channel_multiplier=0,
                                      allow_small_or_imprecise_dtypes=True), writes=["hnT"])
        S.op("pool", lambda e: e.iota(tokf, pattern=[[P, NTT]], base=0, channel_multiplier=1,
                                      allow_small_or_imprecise_dtypes=True), writes=["hnT"])
        tb = mbank()
        tbt = "pm%d" % tb
        n = 0
        for tt in range(NTT):
            for kk in range(2):
                sl = n % 2
                S.op("dve", lambda e, sl=sl, tt=tt, kk=kk: e.tensor_scalar(
                    out=OS[sl], in0=io128, scalar1=sres[:, tt, kk:kk + 1], scalar2=None, op0=ALU.is_equal),
                    reads=[F, "hnT"], writes=["OS%d" % sl])
                S.op("dve", lambda e, sl=sl, tt=tt, kk=kk: e.tensor_scalar(
                    out=OT[sl], in0=jio, scalar1=tsl[:, tt, kk:kk + 1], scalar2=tokf[:, tt:tt + 1], op0=ALU.is_equal,
                    op1=ALU.mult), reads=[F, "hnT"], writes=["OT%d" % sl])
                S.op("pe", lambda e, sl=sl, n=n: e.matmul(pm[tb][:, 0:NST], lhsT=OS[sl], rhs=OT[sl], start=(n == 0),
                                                          stop=(n == 2 * NTT - 1)),
                     reads=["OS%d" % sl, "OT%d" % sl], writes=[tbt])
                n += 1
        S.op("dve", lambda e: e.tensor_copy(out=toki[:, :], in_=pm[tb][:, 0:NST]), reads=[tbt],
             writes=["toki", "hf_d", "h1_d"])

        NSL = 4
        wof = w_out[:, :, :].rearrange("p k d -> p (k d)")
        wgu = [big[:, 0:4096], big[:, 4096:8192], big[:, 8192:12288], wof[:, 4096:8192]]
        wdn = [big[:, 12288:14336], big[:, 14336:16384], wof[:, 0:2048], wof[:, 2048:4096]]
        xg = [xt[0], xt[1], hft[0]]
        xgt = ["xt0", "xt1", "hft0"]
        NXS = 3

        def gather_x(j):
            sl = j % NXS
            S.op("pool", lambda e: e.indirect_dma_start(
                out=xg[sl][:, :], out_offset=None, in_=hf_d[:, :],
                in_offset=bass.IndirectOffsetOnAxis(ap=toki[:, j:j + 1], axis=0)),
                reads=["toki", "hf_d"], writes=[xgt[sl]], dma="xg%d" % sl)

        def gather_gu(j):
            sl = j % NSL
            first = []
            S.op("pool", lambda e: e.indirect_dma_start(
                out=wgu[sl], out_offset=None, in_=wgu_d[:, :],
                in_offset=bass.IndirectOffsetOnAxis(ap=idxw[:, j:j + 1], axis=0)),
                reads=["idxw"], writes=["wgu%d" % sl] + first, dma="wgu%d" % sl)

        def gather_dn(j):
            sl = j % NSL
            first = []
            S.op("pool", lambda e: e.indirect_dma_start(
                out=wdn[sl], out_offset=None, in_=wdn_d[:, :],
                in_offset=bass.IndirectOffsetOnAxis(ap=idxw[:, j:j + 1], axis=0)),
                reads=["idxw"], writes=["wdn%d" % sl] + first, dma="wdn%d" % sl)

        def xt_b(j):
            sl = j % NXS
            for h in range(2):
                b = mbank()
                for kk in range(4):
                    k = h * 4 + kk
                    S.op("pe", lambda e, b=b, kk=kk, k=k: e.transpose(out=pm[b][:, kk * P:(kk + 1) * P],
                                                                      in_=xg[sl][:, k * P:(k + 1) * P], identity=ident),
                         reads=[xgt[sl], "consts"], writes=["pm%d" % b])
                S.op("act" if h == 0 else "dve", lambda e, b=b, h=h: (e.copy if h == 0 else e.tensor_copy)(
                    out=hfT[:, h * 4:(h + 1) * 4, :], in_=pm[b][:, :].rearrange("p (k t) -> p k t", k=4)),
                    reads=["pm%d" % b], writes=["hfT"])

        def gu_b(j):
            sl = j % NSL
            for k in range(8):
                S.op("pe", lambda e, k=k: e.matmul(pin[0][:, :], lhsT=hfT[:, k, :], rhs=wgu[sl][:, k * 512:(k + 1) * 512],
                                                   start=(k == 0), stop=(k == 7)),
                     reads=["hfT", "wgu%d" % sl], writes=["pin0"])
            S.op("act", lambda e: e.activation(out=sgS[:, :], in_=pin[0][:, 0:256], func=AF.Silu), reads=["pin0"],
                 writes=["sgS"])
            S.op("dve", lambda e: e.tensor_tensor(out=aS[:, :], in0=sgS[:, :], in1=pin[0][:, 256:512], op=ALU.mult),
                 reads=["sgS", "pin0"], writes=["aS"])

        def at_b(j):
            for c in range(2):
                S.op("pe", lambda e, c=c: e.transpose(out=pin[1][:, c * P:(c + 1) * P], in_=aS[:, c * P:(c + 1) * P],
                                                      identity=ident), reads=["aS", "consts"], writes=["pin1"])
            S.op("act", lambda e: e.copy(out=aT[:, :, :], in_=pin[1][:, 0:2 * P].rearrange("p (c t) -> p c t", c=2)),
                 reads=["pin1"], writes=["aT"])

        def down_b(j):
            sl = j % NSL
            for half in range(2):
                for c in range(2):
                    S.op("pe", lambda e, half=half, c=c: e.matmul(
                        po[:, half * 512:(half + 1) * 512], lhsT=aT[:, c, :],
                        rhs=wdn[sl][:, c * 1024 + half * 512:c * 1024 + (half + 1) * 512],
                        start=(c == 0), stop=(c == 1)), reads=["aT", "wdn%d" % sl], writes=["po"])
            ys = j % 2
            yb, ytok = x2[ys], "x2%d" % ys
            S.op("act", lambda e: e.copy(out=yb[:, 0:512], in_=po[:, 0:512]), reads=["po"], writes=[ytok])
            S.op("dve", lambda e: e.tensor_copy(out=yb[:, 512:1024], in_=po[:, 512:1024]), reads=["po"], writes=[ytok])
            S.op("sp", lambda e: e.dma_start(out=y_d[j * P:(j + 1) * P, :], in_=yb[:, :]), reads=[ytok, "y_d"],
                 dma="ys%d" % ys)

        S.op("pool", lambda e: e.memset(bar[:, 0:1], 0.0), writes=WIN + ["w_out", "bar"])
        for j in range(NXS):
            gather_x(j)
        for j in range(NSL):
            gather_gu(j)
            gather_dn(j)
        xt_b(0)
        gu_b(0)
        if NSL < NST:
            gather_gu(NSL)
        for j in range(NST):
            if j + 1 < NST:
                xt_b(j + 1)
            if j >= 1:
                down_b(j - 1)
                if j - 1 + NSL < NST:
                    gather_dn(j - 1 + NSL)
            at_b(j)
            if j + 1 < NST:
                gu_b(j + 1)
                if j + 1 + NSL < NST:
                    gather_gu(j + 1 + NSL)
            if j + NXS < NST:
                gather_x(j + NXS)
        down_b(NST - 1)

        fn_row = yT[:, :, :].rearrange("p k t -> p (k t)")[:, 0:D]
        S.op("sp", lambda e: e.dma_start(out=fn_row, in_=rowv_d[:, D:2 * D]), writes=YT_ALL + ["y_d"], dma="c5")
        wo = [w_out[:, k, :] for k in range(8)]
        NCS = 4
        cy1 = [xt[0][:, :], xt[1][:, :], wo[0], wo[1]]
        cy2 = [x2[0][:, :], x2[1][:, :], wo[2], wo[3]]
        chb = [hft[0][:, :], hft[1][:, :], wo[4], wo[5]]
        cout = [wo[6], wo[7]]
        first_c = set()

        def c_loads(tt):
            sl = tt % NCS
            r0 = tt * P
            extra = []
            first_c.add(sl)
            S.op("pool", lambda e: e.indirect_dma_start(
                out=cy1[sl], out_offset=None, in_=y_d[:, :],
                in_offset=bass.IndirectOffsetOnAxis(ap=yidx[:, tt, 0:1], axis=0)),
                reads=["yidx", "y_d"], writes=["cy1%d" % sl] + (["xt%d" % sl] if sl < 2 else extra), dma="cy1%d" % sl)
            S.op("pool", lambda e: e.indirect_dma_start(
                out=cy2[sl], out_offset=None, in_=y_d[:, :],
                in_offset=bass.IndirectOffsetOnAxis(ap=yidx[:, tt, 1:2], axis=0)),
                reads=["yidx", "y_d"], writes=["cy2%d" % sl] + (["x2%d" % sl] if sl < 2 else extra), dma="cy2%d" % sl)
            S.op("sp", lambda e: e.dma_start(out=chb[sl], in_=h1_d[r0:r0 + P, :]), reads=["h1_d", "barc"],
                 writes=["chb%d" % sl] + (["hft%d" % sl] if sl < 2 else extra), dma="ch%d" % sl)

        S.op("pool", lambda e: e.memset(bar[:, 1:2], 0.0),
             writes=["wgu%d" % i for i in range(4)] + ["wdn%d" % i for i in range(4)] + ["barc"])
        c_loads(0)
        c_loads(1)
        for tt in range(NTT):
            sl = tt % NCS
            r0 = tt * P
            y1, y1t = cy1[sl], "cy1%d" % sl
            y2, y2t = cy2[sl], "cy2%d" % sl
            hb, hbt = chb[sl], "chb%d" % sl
            ob, obt = cout[tt % 2], "cout%d" % (tt % 2)
            if tt + 2 < NTT:
                c_loads(tt + 2)
            S.op("dve", lambda e, tt=tt, y1=y1, hb=hb: e.scalar_tensor_tensor(
                out=hb, in0=y1, scalar=RT[:, tt, 2:3], in1=hb, op0=ALU.mult, op1=ALU.add),
                reads=[y1t, hbt, "RT"], writes=[hbt])
            S.op("dve", lambda e, tt=tt, y2=y2, hb=hb: e.scalar_tensor_tensor(
                out=hb, in0=y2, scalar=RT[:, tt, 3:4], in1=hb, op0=ALU.mult, op1=ALU.add),
                reads=[y2t, hbt, "RT"], writes=[hbt])
            c = sscol()
            sst = "ss%d" % c
            S.op("act", lambda e, hb=hb, c=c: e.activation(out=junk[:, :], in_=hb, func=AF.Square,
                                                           accum_out=ssb[:, c:c + 1]), reads=[hbt],
                 writes=["junk", sst])
            rstd_ops(ssb[:, c:c + 1], ssb[:, c:c + 1], float(D), [sst], [sst])
            first_o = []
            S.op("dve", lambda e, hb=hb, c=c, ob=ob: e.scalar_tensor_tensor(
                out=ob, in0=hb, scalar=ssb[:, c:c + 1], in1=fn_row, op0=ALU.mult, op1=ALU.mult),
                reads=[hbt, sst, "barc"] + YT_ALL, writes=[obt] + first_o)
            S.op("act", lambda e, ob=ob, r0=r0: e.dma_start(out=out_d[r0:r0 + P, :], in_=ob), reads=[obt],
                 dma="os%d" % (tt % 2))
        S.emit()
    return nc


def prep_shared(inp):
    f = np.float32
    w_in = np.ascontiguousarray(inp["w_in"][0], f)
    w_out = np.ascontiguousarray(inp["w_out"][0], f)
    wr = np.ascontiguousarray(np.concatenate([inp["w_router_group"][0], inp["w_router_expert"][0]], axis=1), f)
    poolw = np.ascontiguousarray(np.transpose(inp["pool_w"][0], (1, 0, 2)).reshape(P, 4 * P), f)
    colv = np.zeros((P, 32), f)
    colv[:, 0:8] = inp["norm_mix"][0].reshape(8, P).T
    cw = inp["conv_w"][0]
    for ch in range(4):
        for tap in range(3):
            colv[:, 8 + ch * 3 + tap] = cw[tap, ch * P:(ch + 1) * P]
    colv[:, 20:24] = inp["norm_conv_out"][0].reshape(4, P).T
    colv[:, 24:28] = inp["pool_scale"][0].reshape(4, P).T
    colv[:, 28:32] = inp["norm_pool_out"][0].reshape(4, P).T
    rowv = np.empty((P, 2 * D), f)
    rowv[:, 0:D] = inp["norm_ffn"][0][None, :]
    rowv[:, D:] = inp["final_norm"][None, :]
    consts = np.zeros((P, 3 * P), f)
    consts[:, 0:P] = np.eye(P, dtype=f)
    consts[:, P:2 * P] = np.triu(np.ones((P, P), f), 1)
    consts[:, 2 * P:] = 1.0
    wg, wu, wd = inp["w_gate"][0], inp["w_up"][0], inp["w_down"][0]
    wall_gu = np.empty((NE, P, 8, 512), f)
    wall_gu[:, :, :, 0:256] = wg.reshape(NE, 8, P, 256).transpose(0, 2, 1, 3)
    wall_gu[:, :, :, 256:512] = wu.reshape(NE, 8, P, 256).transpose(0, 2, 1, 3)
    wall_dn = np.ascontiguousarray(wd.reshape(NE, 2, P, D).transpose(0, 2, 1, 3))
    return dict(w_in=w_in, w_out=w_out, wr=wr, poolw=poolw, colv=colv, rowv=rowv, consts=consts,
                wall_gu=wall_gu.reshape(NE * P, 4096), wall_dn=wall_dn.reshape(NE * P, 2048))


def core_inputs(x, meta, shared, NT):
    B, Sq, _ = x.shape
    nh = Sq // NT
    maps = []
    for c in range(B * nh):
        b, h = divmod(c, nh)
        xm = np.ascontiguousarray(x[b, h * NT:(h + 1) * NT], np.float32)
        xp = np.zeros((P, D), np.float32)
        xp[P - HALO:] = meta if h == 0 else x[b, h * NT - HALO:h * NT]
        m = dict(shared)
        m["x"] = xm
        m["xp"] = xp
        maps.append(m)
    return maps


_NC_CACHE = {}


def kernel(x, meta_tokens, norm_mix, w_in, conv_w, norm_conv_out, pool_w, pool_scale, norm_pool_out, w_out,
           norm_ffn, w_router_group, w_router_expert, w_gate, w_up, w_down, final_norm):
    inp = dict(norm_mix=norm_mix, w_in=w_in, conv_w=conv_w, norm_conv_out=norm_conv_out, pool_w=pool_w,
               pool_scale=pool_scale, norm_pool_out=norm_pool_out, w_out=w_out, norm_ffn=norm_ffn,
               w_router_group=w_router_group, w_router_expert=w_router_expert, w_gate=w_gate, w_up=w_up,
               w_down=w_down, final_norm=final_norm)
    inp = {k: np.asarray(v, np.float32) for k, v in inp.items()}
    x = np.asarray(x, np.float32)
    meta = np.asarray(meta_tokens, np.float32)
    B, Sq, _ = x.shape
    NT = B * Sq // NCORES
    shared = prep_shared(inp)
    maps = core_inputs(x, meta, shared, NT)
    if NT not in _NC_CACHE:
        _NC_CACHE[NT] = build(NT)
    res = run_bass_kernel_spmd(_NC_CACHE[NT], maps, core_ids=list(range(NCORES)))
    out = np.empty((B, Sq, D), np.float32)
    nh = Sq // NT
    for c in range(NCORES):
        b, h = divmod(c, nh)
        out[b, h * NT:(h + 1) * NT] = res.results[c]["out"]
    return out
```

```python
import numpy as np
from contextlib import ExitStack
import concourse.bass as bass
import concourse.mybir as mybir
from concourse.bass_utils import run_bass_kernel_spmd

F32 = mybir.dt.float32
I32 = mybir.dt.int32
BF16 = mybir.dt.bfloat16
ALU = mybir.AluOpType
AF = mybir.ActivationFunctionType
AX = mybir.AxisListType

P = 128
D = 1024
NB = 256
HALO = 16
NE = 32
EPS = 1e-6
NCORES = 8
SEQ_PER_CORE = 4096

ENGS = ("pe", "act", "dve", "pool", "sp")


class Sched:
    def __init__(self, nc, stack):
        self.nc = nc
        self.stack = stack
        self.ops = []
        self.last_w = {}
        self.readers = {}

    def op(self, eng, fn, reads=(), writes=(), dma=None):
        i = len(self.ops)
        deps = set()
        for t in tuple(reads) + tuple(writes):
            if t in self.last_w:
                deps.add(self.last_w[t])
        for t in writes:
            r = self.readers.get(t)
            if r:
                deps.update(r[0].values())
                deps.update(r[1])
        deps.discard(i)
        for t in reads:
            r = self.readers.setdefault(t, ({}, []))
            if dma is None:
                r[0][eng] = i
            else:
                r[1].append(i)
        for t in writes:
            self.last_w[t] = i
            self.readers[t] = ({}, [])
        self.ops.append(dict(eng=eng, fn=fn, deps=deps, dma=dma, sig=False))
        return i

    def emit(self):
        nc, ops = self.nc, self.ops
        for o in ops:
            for d in o["deps"]:
                p = ops[d]
                if p["dma"] is None and o["dma"] is None and p["eng"] == "pe" and o["eng"] == "pe":
                    continue
                p["sig"] = True
        eng_sem = {e: self.stack.enter_context(nc.semaphore("prog_" + e)) for e in ENGS}
        cnt = {e: 0 for e in ENGS}
        dma_sems, dcnt = {}, {}
        for o in ops:
            if o["dma"] is not None:
                k = o["dma"]
                if k not in dma_sems:
                    dma_sems[k] = self.stack.enter_context(nc.semaphore("dma_" + k))
                    dcnt[k] = 0
                dcnt[k] += 16
                o["signal"] = (dma_sems[k], dcnt[k], "D" + k)
            elif o["sig"]:
                cnt[o["eng"]] += 1
                o["signal"] = (eng_sem[o["eng"]], cnt[o["eng"]], "E" + o["eng"])
        per_eng = {e: [] for e in ENGS}
        for o in ops:
            per_eng[o["eng"]].append(o)
        block = self.stack.enter_context(nc.Block())

        def run(engname):
            def body(eng):
                w = {}
                for o in per_eng[engname]:
                    need = {}
                    for d in o["deps"]:
                        p = ops[d]
                        if "signal" not in p:
                            continue
                        sem, val, key = p["signal"]
                        if need.get(key, (None, 0))[1] < val:
                            need[key] = (sem, val)
                    for key, (sem, val) in need.items():
                        if w.get(key, 0) >= val:
                            continue
                        eng.wait_ge(sem, val)
                        w[key] = val
                    ins = o["fn"](eng)
                    if "signal" in o:
                        ins.then_inc(o["signal"][0], 16 if o["dma"] is not None else 1)
                if engname == "sp":
                    for k, sem in dma_sems.items():
                        eng.wait_ge(sem, dcnt[k])
            return body

        block.tensor(run("pe"))
        block.scalar(run("act"))
        block.vector(run("dve"))
        block.gpsimd(run("pool"))
        block.sync(run("sp"))


def build(NT, MM_DT=F32):
    NTT = NT // P
    NBLK = NT // NB
    NST = (2 * NT + NE * (P - 1)) // P
    SUB = NB // P
    nc = bass.Bass("TRN2", target_bir_lowering=False)

    def din(name, shape, dt=F32):
        return nc.dram_tensor(name, shape, dt, kind="ExternalInput").ap()

    x_d = din("x", [NT, D])
    xp_d = din("xp", [P, D])
    w_in_d = din("w_in", [D, 2048])
    w_out_d = din("w_out", [D, D])
    wr_d = din("wr", [D, 36])
    poolw_d = din("poolw", [P, 4 * P])
    colv_d = din("colv", [P, 32])
    rowv_d = din("rowv", [P, 2 * D])
    consts_d = din("consts", [P, 3 * P])
    wgu_d = din("wall_gu", [NE * P, 4096])
    wdn_d = din("wall_dn", [NE * P, 2048])
    out_d = nc.dram_tensor("out", [NT, D], F32, kind="ExternalOutput").ap()
    hf_d = nc.dram_tensor("hf_scr", [NT, D], F32, kind="Internal").ap()
    h1_d = nc.dram_tensor("h1_scr", [NT, D], F32, kind="Internal").ap()
    y_d = nc.dram_tensor("y_scr", [NST * P, D], F32, kind="Internal").ap()

    with ExitStack() as st:
        def sb(name, shape, dt=F32):
            return st.enter_context(nc.sbuf_tensor("s_" + name, shape, dt))

        def psum(name, shape):
            return st.enter_context(nc.psum_tensor("p_" + name, shape, F32))

        S = Sched(nc, st)
        big = sb("big", [P, 16384])
        w_in = big[:, :].rearrange("p (k c) -> p k c", k=8)
        w_out = sb("w_out", [P, 8, D])
        wr = sb("wr_sb", [P, 8, 36])
        poolw = sb("poolw_sb", [P, 4, P])
        poolw_s = sb("poolw_s", [P, 4, P])
        poolw_n = sb("poolw_n", [P, 4, P])
        colv = sb("colv_sb", [P, 32])
        rowv = sb("rowv_sb", [P, D])
        consts = sb("consts_sb", [P, 3 * P])
        ident, utri, ones = consts[:, 0:P], consts[:, P:2 * P], consts[:, 2 * P:3 * P]
        xt = [sb("xt0", [P, D]), sb("xt1", [P, D])]
        x2 = [sb("x20", [P, D]), sb("x21", [P, D])]
        hft = [sb("hft0", [P, D]), sb("hft1", [P, D])]
        junk = sb("junk", [P, D], BF16)
        hnT = sb("hnT", [P, 8, NB])
        bS = sb("bS", [P, 4, NB])
        cS = sb("cS", [P, 4, NB])
        vS = sb("vS", [P, 4, NB])
        uS = sb("uS", [P, 4, NB + HALO])
        vpS = sb("vpS", [P, 4, NB + HALO])
        acc = [sb("acc0", [P, NB]), sb("acc1", [P, NB])]
        sq4 = sb("sq4", [P, 4, NB])
        pooled4 = sb("pooled4", [P, 4, NB])
        tA = sb("tA", [P, NB + HALO])
        tB = sb("tB", [P, NB + HALO])
        rsn = sb("rsn", [P, 2, NB])
        yT = sb("yT", [P, 8, NB])
        hfT = sb("hfT", [P, 8, P])
        sm2 = sb("sm", [P, 2, 256])
        lgs = sb("lgs", [P, 2, 36])
        m12a = sb("m12", [P, 2, NE])
        srun = sb("srun", [P, NE])
        M1all = sb("M1all", [P, NTT, NE], BF16)
        M2all = sb("M2all", [P, NTT, NE], BF16)
        RT = sb("RT", [P, NTT, 4])
        ssb = sb("ssb", [P, 16])
        fin = hft[1]
        yidx = sb("yidx", [P, NTT, 2], I32)
        toki = sb("toki", [P, NST], I32)
        bar = sb("bar", [P, 2])
        idxw = sb("idxw", [P, NST], I32)
        sgS = sb("sgS", [P, 256])
        aS = sb("aS", [P, 256])
        aT = sb("aT", [P, 2, P])

        pin = [psum("pin%d" % i, [P, 512]) for i in range(3)]
        pm = [psum("pm%d" % i, [P, 512]) for i in range(3)]
        po = psum("po", [P, 1024])
        mctr = [0]

        def mbank():
            i = mctr[0] % 3
            mctr[0] += 1
            return i

        ssc = [0]

        def sscol():
            i = ssc[0] % 16
            ssc[0] += 1
            return i

        S.op("sp", lambda e: e.dma_start(out=consts[:, :], in_=consts_d[:, :]), writes=["consts"], dma="c0")
        S.op("sp", lambda e: e.dma_start(out=colv[:, :], in_=colv_d[:, :]), writes=["colv"], dma="c1")
        def setup_weights():
            for k in range(8):
                S.op("sp", lambda e, k=k: e.dma_start(out=w_in[:, k, :], in_=w_in_d[k * P:(k + 1) * P, :]),
                     writes=["w_in%d" % k], dma="win%d" % k)
                if k % 2:
                    S.op("act", lambda e, k=k: e.mul(out=w_in[:, k, :], in_=w_in[:, k, :], mul=colv[:, k:k + 1]),
                         reads=["colv"], writes=["w_in%d" % k])
                else:
                    S.op("dve", lambda e, k=k: e.tensor_scalar(out=w_in[:, k, :], in0=w_in[:, k, :],
                                                              scalar1=colv[:, k:k + 1], scalar2=None, op0=ALU.mult),
                         reads=["colv"], writes=["w_in%d" % k])
            S.op("sp", lambda e: e.dma_start(out=poolw[:, :, :], in_=poolw_d[:, :].rearrange("p (g d) -> p g d", g=4)),
                 writes=["poolw"], dma="c2")
            for g in range(4):
                S.op("dve", lambda e, g=g: e.tensor_scalar(out=poolw_s[:, g, :], in0=poolw[:, g, :], scalar1=1.0 / (2 << g),
                                                          scalar2=None, op0=ALU.mult), reads=["poolw"], writes=["poolw_s"])
            S.op("dve", lambda e: e.tensor_scalar(out=poolw_n[:, :, :], in0=poolw[:, :, :], scalar1=-1.0, scalar2=None,
                                                  op0=ALU.mult), reads=["poolw"], writes=["poolw_n"])
            S.op("sp", lambda e: e.dma_start(out=wr[:, :, :], in_=wr_d[:, :].rearrange("(k p) n -> p k n", p=P)),
                 writes=["wr"], dma="c3")
            S.op("sp", lambda e: e.dma_start(out=rowv[:, :], in_=rowv_d[:, 0:D]), writes=["rowv"], dma="c4")
            S.op("dve", lambda e: e.memset(srun[:, :], 0.0), writes=["srun"])
        WIN = ["w_in%d" % k for k in range(8)]

        def rstd_ops(src_ap, dst_ap, n, rtok, wtok):
            S.op("act", lambda e: e.activation(out=dst_ap, in_=src_ap, func=AF.Ln, bias=EPS, scale=1.0 / n),
                 reads=rtok, writes=wtok)
            S.op("act", lambda e: e.activation(out=dst_ap, in_=dst_ap, func=AF.Exp, scale=-0.5), reads=wtok,
                 writes=wtok)

        def stage_a_act(src_rows, slot):
            xs = xt[slot]
            tk = "xt%d" % slot
            S.op("sp", lambda e: e.dma_start(out=xs[:, :], in_=src_rows), writes=[tk], dma=tk)
            c = sscol()
            sst = "ss%d" % c
            S.op("act", lambda e: e.activation(out=junk[:, :], in_=xs[:, :], func=AF.Square,
                                               accum_out=ssb[:, c:c + 1]), reads=[tk], writes=["junk", sst])
            rstd_ops(ssb[:, c:c + 1], ssb[:, c:c + 1], float(D), [sst], [sst])
            S.op("act", lambda e: e.mul(out=xs[:, :], in_=xs[:, :], mul=ssb[:, c:c + 1]), reads=[sst], writes=[tk])

        def stage_a_tr(slot, col0):
            xs = xt[slot]
            tk = "xt%d" % slot
            for h in range(2):
                b = mbank()
                for kk in range(4):
                    k = h * 4 + kk
                    S.op("pe", lambda e, b=b, kk=kk, k=k: e.transpose(out=pm[b][:, kk * P:(kk + 1) * P],
                                                                      in_=xs[:, k * P:(k + 1) * P], identity=ident),
                         reads=[tk, "consts"], writes=["pm%d" % b])
                S.op("act", lambda e, b=b, h=h: e.copy(out=hnT[:, h * 4:(h + 1) * 4, col0:col0 + P],
                                                       in_=pm[b][:, :].rearrange("p (k t) -> p k t", k=4)),
                     reads=["pm%d" % b], writes=["hnT"])

        def stage_a(src_rows, slot, col0):
            stage_a_act(src_rows, slot)
            stage_a_tr(slot, col0)

        pinc = [0]

        def inproj_group(g, ncols, prefix=False):
            b = pinc[0] % 3
            pinc[0] += 1
            for cc in range(2):
                c = 2 * g + cc
                for k in range(8):
                    S.op("pe", lambda e, b=b, cc=cc, c=c, k=k: e.matmul(
                        pin[b][:, cc * ncols:(cc + 1) * ncols], lhsT=w_in[:, k, c * P:(c + 1) * P],
                        rhs=hnT[:, k, 0:ncols], start=(k == 0), stop=(k == 7)),
                        reads=["hnT", WIN[k]], writes=["pin%d" % b])
            src = pin[b][:, 0:2 * ncols].rearrange("p (c t) -> p c t", c=2)
            j = 2 * (g % 2)
            if not prefix:
                if g < 2:
                    dst, tok = bS[:, j:j + 2, :], "bS"
                elif g < 4:
                    dst, tok = cS[:, j:j + 2, :], "cS"
                elif g < 6:
                    dst, tok = vS[:, j:j + 2, :], "vS"
                else:
                    dst, tok = vpS[:, j:j + 2, HALO:HALO + NB], "vpm"
                S.op("act", lambda e: e.copy(out=dst, in_=src), reads=["pin%d" % b], writes=[tok])
            else:
                srch = src[:, :, ncols - HALO:ncols]
                if g in (2, 3):
                    S.op("act", lambda e: e.copy(out=cS[:, j:j + 2, 0:HALO], in_=srch), reads=["pin%d" % b], writes=["cS"])
                elif g in (4, 5):
                    S.op("dve", lambda e: e.tensor_tensor(out=uS[:, j:j + 2, 0:HALO], in0=cS[:, j:j + 2, 0:HALO],
                                                          in1=srch, op=ALU.mult),
                         reads=["pin%d" % b, "cS"], writes=["uh"])
                else:
                    S.op("act", lambda e: e.copy(out=vpS[:, j:j + 2, 0:HALO], in_=srch), reads=["pin%d" % b], writes=["vph"])

        def mix_part1():
            S.op("dve", lambda e: e.tensor_tensor(out=uS[:, :, HALO:HALO + NB], in0=cS[:, :, :], in1=vS[:, :, :],
                                                  op=ALU.mult), reads=["cS", "vS"], writes=["um"])
            for ch in range(4):
                a = acc[ch % 2]
                at = "acc%d" % (ch % 2)
                S.op("dve", lambda e, a=a, ch=ch: e.tensor_scalar(out=a[:, :], in0=uS[:, ch, HALO - 2:HALO - 2 + NB],
                                                                 scalar1=colv[:, 8 + ch * 3:9 + ch * 3], scalar2=None,
                                                                 op0=ALU.mult), reads=["um", "uh", "colv"], writes=[at])
                for tap in (1, 2):
                    S.op("dve", lambda e, a=a, ch=ch, tap=tap: e.scalar_tensor_tensor(
                        out=a[:, :], in0=uS[:, ch, HALO - 2 + tap:HALO - 2 + tap + NB],
                        scalar=colv[:, 8 + ch * 3 + tap:9 + ch * 3 + tap], in1=a[:, :], op0=ALU.mult, op1=ALU.add),
                        reads=["um", "uh", at], writes=[at])
                S.op("dve", lambda e, a=a, ch=ch: e.tensor_tensor(out=yT[:, ch, :], in0=bS[:, ch, :], in1=a[:, :],
                                                                 op=ALU.mult), reads=["bS", at], writes=["yTc%d" % ch])
            S.op("dve", lambda e: e.tensor_copy(out=uS[:, :, HALO - 2:HALO], in_=uS[:, :, NB + HALO - 2:NB + HALO]),
                 reads=["um"], writes=["uh"])
            W = NB + HALO
            for g in range(4):
                w = 2 << g
                src = vpS[:, g, :]
                cur, curtok = src, None
                lo_needed = HALO
                lvl = 1
                bufs = [tA, tB]
                bi = 0
                while lvl < w:
                    nl = lvl * 2
                    lo = HALO - (w - nl)
                    last = nl == w
                    rt = ["vpm", "vph"] if curtok is None else [curtok]
                    if last:
                        S.op("pool", lambda e, cur=cur, lvl=lvl, g=g: e.tensor_tensor(
                            out=pooled4[:, g, :], in0=cur[:, HALO:W], in1=cur[:, HALO - lvl:W - lvl], op=ALU.add),
                            reads=rt, writes=["pooled%d" % g])
                    else:
                        dstb = bufs[bi]
                        dtok = "tA" if bi == 0 else "tB"
                        S.op("pool", lambda e, dstb=dstb, cur=cur, lo=lo, lvl=lvl: e.tensor_tensor(
                            out=dstb[:, lo:W], in0=cur[:, lo:W], in1=cur[:, lo - lvl:W - lvl], op=ALU.add),
                            reads=rt, writes=[dtok])
                        cur, curtok = dstb, dtok
                        bi ^= 1
                    lvl = nl
            S.op("pool", lambda e: e.tensor_copy(out=vpS[:, :, 0:HALO], in_=vpS[:, :, NB:NB + HALO]),
                 reads=["vpm"], writes=["vph"])

        def mix_part1b():
            for g in range(4):
                pb = pooled4[:, g, :]
                pt = "pooled%d" % g
                if g % 2 == 0:
                    mix_part1.bank[g // 2] = mbank()
                mb_ = mix_part1.bank[g // 2]
                S.op("pe", lambda e, mb_=mb_, g=g, pb=pb: e.matmul(pm[mb_][:, (g % 2) * NB:(g % 2 + 1) * NB],
                                                                   lhsT=poolw_s[:, g, :], rhs=pb, start=True, stop=False),
                     reads=[pt, "poolw_s"], writes=["pm%d" % mb_])
                S.op("pe", lambda e, mb_=mb_, g=g: e.matmul(pm[mb_][:, (g % 2) * NB:(g % 2 + 1) * NB],
                                                            lhsT=poolw_n[:, g, :], rhs=vpS[:, g, HALO:HALO + NB],
                                                            start=False, stop=True),
                     reads=["vpm", "poolw_n"], writes=["pm%d" % mb_])
            for g in range(4):
                mb_ = mix_part1.bank[g // 2]
                S.op("dve", lambda e, mb_=mb_, g=g: e.tensor_scalar(out=yT[:, 4 + g, :],
                                                                     in0=pm[mb_][:, (g % 2) * NB:(g % 2 + 1) * NB],
                                                                     scalar1=colv[:, 24 + g:25 + g], scalar2=None,
                                                                     op0=ALU.mult),
                     reads=["pm%d" % mb_, "colv"], writes=["yTp%d" % g])

        mix_part1.bank = [0, 0]

        mix2_bank = [0]

        def mix2_pre(half):
            pre = "yTc" if half == 0 else "yTp"
            S.op("act", lambda e: e.activation(out=sq4[:, :, :], in_=yT[:, half * 4:half * 4 + 4, :], func=AF.Square),
                 reads=[pre + "%d" % i for i in range(4)], writes=["sq4"])
            ab = acc[half]
            S.op("dve", lambda e: e.tensor_reduce(out=ab[:, :], in_=sq4[:, :, :].rearrange("p c t -> p t c"),
                                                  axis=AX.X, op=ALU.add), reads=["sq4"], writes=["acc%d" % half])

        def mix2_mm():
            sb_ = mbank()
            for half in range(2):
                S.op("pe", lambda e, half=half: e.matmul(pm[sb_][:, half * NB:(half + 1) * NB], lhsT=ones,
                                                         rhs=acc[half][:, :], start=True, stop=True),
                     reads=["acc%d" % half, "consts"], writes=["pm%d" % sb_])
            for half in range(2):
                rstd_ops(pm[sb_][:, half * NB:(half + 1) * NB], rsn[:, half, :], 512.0, ["pm%d" % sb_], ["rsn%d" % half])

        def mix_part3():
            for half, pre, gcol in ((0, "yTc", 20), (1, "yTp", 28)):
                for ch in range(4):
                    S.op("dve", lambda e, half=half, ch=ch, gcol=gcol: e.scalar_tensor_tensor(
                        out=yT[:, half * 4 + ch, :], in0=yT[:, half * 4 + ch, :], scalar=colv[:, gcol + ch:gcol + ch + 1],
                        in1=rsn[:, half, :], op0=ALU.mult, op1=ALU.mult),
                        reads=[pre + "%d" % ch, "rsn%d" % half, "colv"], writes=[pre + "%d" % ch])

        YT_ALL = ["yTc%d" % i for i in range(4)] + ["yTp%d" % i for i in range(4)]

        def stage_d(tt, s):
            r0 = tt * P
            slot = tt % 2
            xb, xtok = x2[slot], "x2%d" % slot
            hb, htok = hft[slot], "hft%d" % slot
            S.op("sp", lambda e: e.dma_start(out=xb[:, :], in_=x_d[r0:r0 + P, :]), writes=[xtok], dma=xtok)
            for half in range(2):
                for k in range(8):
                    S.op("pe", lambda e, half=half, k=k: e.matmul(
                        po[:, half * 512:(half + 1) * 512], lhsT=yT[:, k, s * P:(s + 1) * P],
                        rhs=w_out[:, k, half * 512:(half + 1) * 512], start=(k == 0), stop=(k == 7)),
                        reads=YT_ALL + ["w_out"], writes=["po"])
            S.op("dve", lambda e: e.tensor_tensor(out=xb[:, :], in0=xb[:, :], in1=po[:, :], op=ALU.add),
                 reads=["po", xtok], writes=[xtok])
            c = sscol()
            sst = "ss%d" % c
            S.op("act", lambda e: e.activation(out=junk[:, :], in_=xb[:, :], func=AF.Square,
                                               accum_out=ssb[:, c:c + 1]), reads=[xtok], writes=["junk", sst])
            rstd_ops(ssb[:, c:c + 1], ssb[:, c:c + 1], float(D), [sst], [sst])
            S.op("dve", lambda e: e.scalar_tensor_tensor(out=hb[:, :], in0=xb[:, :], scalar=ssb[:, c:c + 1],
                                                         in1=rowv[:, :], op0=ALU.mult, op1=ALU.mult),
                 reads=[xtok, sst, "rowv"], writes=[htok])
            S.op("act", lambda e: e.dma_start(out=h1_d[r0:r0 + P, :], in_=xb[:, :]), reads=[xtok, "h1_d"],
                 dma="h1s%d" % slot)
            S.op("act", lambda e: e.dma_start(out=hf_d[r0:r0 + P, :], in_=hb[:, :]), reads=[htok, "hf_d"],
                 dma="hfs%d" % slot)

        def stage_dtr(tt, s):
            slot = tt % 2
            hb, htok = hft[slot], "hft%d" % slot
            for h in range(2):
                b = mbank()
                for kk in range(4):
                    k = h * 4 + kk
                    S.op("pe", lambda e, b=b, kk=kk, k=k: e.transpose(out=pm[b][:, kk * P:(kk + 1) * P],
                                                                      in_=hb[:, k * P:(k + 1) * P], identity=ident),
                         reads=[htok, "consts"], writes=["pm%d" % b])
                S.op("act", lambda e, b=b, h=h: e.copy(out=hfT[:, h * 4:(h + 1) * 4, :],
                                                       in_=pm[b][:, :].rearrange("p (k t) -> p k t", k=4)),
                     reads=["pm%d" % b], writes=["hfT"])

        def stage_drt(tt, s):
            rb = mbank()
            rbt = "pm%d" % rb
            for k in range(8):
                S.op("pe", lambda e, k=k: e.matmul(pm[rb][:, 0:36], lhsT=hfT[:, k, :], rhs=wr[:, k, :], start=(k == 0),
                                                   stop=(k == 7)), reads=["hfT", "wr"], writes=[rbt])
            S.op("dve", lambda e: e.tensor_copy(out=lgs[:, s, :], in_=pm[rb][:, 0:36]), reads=[rbt],
                 writes=["lg%d" % s])

        def stage_d2(tt, s):
            lg = lgs[:, s, :]
            sm = sm2[:, s, :]
            m12 = m12a[:, s, :]
            M12T = "m12_%d" % s
            gmax, ngmax, sume, g1 = sm[:, 40:41], sm[:, 41:42], sm[:, 42:43], sm[:, 43:44]
            goh = sm[:, 44:48]
            eg = sm[:, 48:52]
            sel = sm[:, 56:64]
            top8 = sm[:, 64:72]
            mask2 = sm[:, 72:80]
            mask1 = sm[:, 80:88]
            dd, ed = sm[:, 88:89], sm[:, 89:90]
            prod = sm[:, 96:128]
            m1f = sm[:, 128:160]
            m2f = sm[:, 160:192]
            T = "sm%d" % s
            S.op("dve", lambda e: e.tensor_reduce(out=gmax, in_=lg[:, 0:4], axis=AX.X, op=ALU.max),
                 reads=[T, "lg%d" % s], writes=[T])
            S.op("dve", lambda e: e.tensor_scalar(out=goh, in0=lg[:, 0:4], scalar1=gmax, scalar2=None, op0=ALU.is_ge),
                 reads=[T], writes=[T])
            S.op("dve", lambda e: e.tensor_scalar(out=ngmax, in0=gmax, scalar1=-1.0, scalar2=None, op0=ALU.mult),
                 reads=[T], writes=[T])
            S.op("act", lambda e: e.activation(out=eg, in_=lg[:, 0:4], func=AF.Exp, bias=ngmax, accum_out=sume),
                 reads=[T], writes=[T])
            S.op("dve", lambda e: e.reciprocal(out=g1, in_=sume), reads=[T], writes=[T])
            S.op("dve", lambda e: e.tensor_scalar(out=sel, in0=lg[:, 4:12], scalar1=goh[:, 0:1], scalar2=None,
                                                  op0=ALU.mult), reads=[T], writes=[T])
            for g in range(1, 4):
                S.op("dve", lambda e, g=g: e.scalar_tensor_tensor(out=sel, in0=lg[:, 4 + 8 * g:12 + 8 * g],
                                                                  scalar=goh[:, g:g + 1], in1=sel, op0=ALU.mult,
                                                                  op1=ALU.add), reads=[T], writes=[T])
            S.op("dve", lambda e: e.max(out=top8, in_=sel), reads=[T], writes=[T])
            S.op("dve", lambda e: e.tensor_scalar(out=mask2, in0=sel, scalar1=top8[:, 1:2], scalar2=None, op0=ALU.is_ge),
                 reads=[T], writes=[T])
            S.op("dve", lambda e: e.tensor_scalar(out=mask1, in0=sel, scalar1=top8[:, 0:1], scalar2=None, op0=ALU.is_ge),
                 reads=[T], writes=[T])
            S.op("dve", lambda e: e.tensor_tensor(out=dd, in0=top8[:, 1:2], in1=top8[:, 0:1], op=ALU.subtract),
                 reads=[T], writes=[T])
            S.op("act", lambda e: e.activation(out=ed, in_=dd, func=AF.Exp), reads=[T], writes=[T])
            S.op("dve", lambda e: e.tensor_scalar(out=ed, in0=ed, scalar1=1.0, scalar2=None, op0=ALU.add),
                 reads=[T], writes=[T])
            S.op("dve", lambda e: e.reciprocal(out=ed, in_=ed), reads=[T], writes=[T])
            S.op("dve", lambda e: e.tensor_tensor(out=RT[:, tt, 2:3], in0=g1, in1=ed, op=ALU.mult), reads=[T],
                 writes=["RT"])
            S.op("dve", lambda e: e.tensor_tensor(out=RT[:, tt, 3:4], in0=g1, in1=RT[:, tt, 2:3], op=ALU.subtract),
                 reads=[T, "RT"], writes=["RT"])
            for g in range(4):
                S.op("dve", lambda e, g=g: e.tensor_scalar(out=m12[:, 8 * g:8 * g + 8], in0=mask2, scalar1=goh[:, g:g + 1],
                                                           scalar2=None, op0=ALU.mult), reads=[T], writes=[M12T])
                S.op("dve", lambda e, g=g: e.tensor_scalar(out=m1f[:, 8 * g:8 * g + 8], in0=mask1, scalar1=goh[:, g:g + 1],
                                                           scalar2=None, op0=ALU.mult), reads=[T], writes=[T])
            S.op("dve", lambda e: e.tensor_tensor(out=m2f, in0=m12, in1=m1f, op=ALU.subtract),
                 reads=[T, M12T], writes=[T])
            S.op("dve", lambda e: e.tensor_copy(out=M1all[:, tt, :], in_=m1f), reads=[T], writes=["Mall"])
            S.op("dve", lambda e: e.tensor_copy(out=M2all[:, tt, :], in_=m2f), reads=[T], writes=["Mall"])

        def stage_d2b(tt, s):
            lg = lgs[:, s, :]
            sm = sm2[:, s, :]
            m12 = m12a[:, s, :]
            M12T = "m12_%d" % s
            gmax, ngmax, sume, g1 = sm[:, 40:41], sm[:, 41:42], sm[:, 42:43], sm[:, 43:44]
            goh = sm[:, 44:48]
            eg = sm[:, 48:52]
            sel = sm[:, 56:64]
            top8 = sm[:, 64:72]
            mask2 = sm[:, 72:80]
            mask1 = sm[:, 80:88]
            dd, ed = sm[:, 88:89], sm[:, 89:90]
            prod = sm[:, 96:128]
            m1f = sm[:, 128:160]
            m2f = sm[:, 160:192]
            T = "sm%d" % s
            kb = mbank()
            kbt = "pm%d" % kb
            S.op("pe", lambda e: e.matmul(pm[kb][:, 0:NE], lhsT=utri, rhs=m12, start=True, stop=False),
                 reads=[M12T, "consts"], writes=[kbt])
            S.op("pe", lambda e: e.matmul(pm[kb][:, 0:NE], lhsT=ones, rhs=srun[:, :], start=False, stop=True),
                 reads=["srun", "consts"], writes=[kbt])
            for kk, mf in ((0, m1f), (1, m2f)):
                S.op("dve", lambda e, mf=mf: e.tensor_tensor(out=prod, in0=mf, in1=pm[kb][:, 0:NE], op=ALU.mult),
                     reads=[T, kbt], writes=[T])
                S.op("dve", lambda e, kk=kk: e.tensor_reduce(out=RT[:, tt, kk:kk + 1], in_=prod, axis=AX.X, op=ALU.add),
                     reads=[T], writes=["RT"])
            S.op("dve", lambda e: e.tensor_tensor(out=srun[:, :], in0=srun[:, :], in1=m12, op=ALU.add),
                 reads=[M12T, "srun"], writes=["srun"])

        stage_a_act(xp_d[:, :], 0)
        setup_weights()
        stage_a_tr(0, 0)
        for g in range(2, 8):
            inproj_group(g, P, prefix=True)
        for s in range(SUB):
            stage_a(x_d[s * P:(s + 1) * P, :], s % 2, s * P)
        for g in range(8):
            inproj_group(g, NB)
        for k in range(8):
            S.op("sp", lambda e, k=k: e.dma_start(out=w_out[:, k, :], in_=w_out_d[k * P:(k + 1) * P, :]),
                 writes=["w_out"], dma="wout%d" % k)
        def a_act_block(bi):
            for s in range(SUB):
                r0 = bi * NB + s * P
                stage_a_act(x_d[r0:r0 + P, :], s % 2)

        if NBLK > 1:
            a_act_block(1)
        pending = [None]
        for i in range(NBLK):
            nxt = i + 1 < NBLK
            if nxt:
                for s in range(SUB):
                    stage_a_tr(s % 2, s * P)
            if pending[0] is not None:
                stage_drt(*pending[0])
                pending[0] = None
            mix_part1()
            mix2_pre(0)
            if i > 0:
                stage_d2((i - 1) * SUB, 0)
            if nxt:
                inproj_group(0, NB)
                inproj_group(1, NB)
            mix_part1b()
            mix2_pre(1)
            if i > 0:
                stage_d2((i - 1) * SUB + 1, 1)
            if nxt:
                inproj_group(2, NB)
                inproj_group(3, NB)
            mix2_mm()
            if i + 2 < NBLK:
                a_act_block(i + 2)
            if nxt:
                inproj_group(4, NB)
            mix_part3()
            if i > 0:
                stage_d2b((i - 1) * SUB, 0)
            if nxt:
                inproj_group(5, NB)
            if i > 0:
                stage_d2b((i - 1) * SUB + 1, 1)
            assert SUB == 2
            stage_d(i * SUB, 0)
            if nxt:
                inproj_group(6, NB)
            stage_dtr(i * SUB, 0)
            stage_d(i * SUB + 1, 1)
            stage_drt(i * SUB, 0)
            if nxt:
                inproj_group(7, NB)
            stage_dtr(i * SUB + 1, 1)
            pending[0] = (i * SUB + 1, 1)
        stage_drt(*pending[0])
        for s in range(SUB):
            stage_d2((NBLK - 1) * SUB + s, s)
        for s in range(SUB):
            stage_d2b((NBLK - 1) * SUB + s, s)

        cb = mbank()
        cbt = "pm%d" % cb
        S.op("pe", lambda e: e.matmul(pm[cb][:, 0:NE], lhsT=ones, rhs=srun[:, :], start=True, stop=True),
             reads=["srun", "consts"], writes=[cbt])
        F = "hft1"
        cnt = fin[:, 0:32]
        tl = fin[:, 32:64]
        ca = fin[:, 64:96]
        cb2 = fin[:, 96:128]
        tstart = fin[:, 128:160]
        S.op("dve", lambda e: e.tensor_copy(out=cnt, in_=pm[cb][:, 0:NE]), reads=[cbt], writes=[F])
        thr32 = fin[:, 904:936]
        S.op("pool", lambda e: e.iota(thr32, pattern=[[P, 32]], base=0, channel_multiplier=0,
                                      allow_small_or_imprecise_dtypes=True), writes=[F])
        cmpA = hft[0][:, 0:NE * NTT].rearrange("p (e m) -> p e m", m=NTT)
        S.op("dve", lambda e: e.tensor_tensor(out=cmpA, in0=cnt.unsqueeze(2).broadcast_to([P, NE, NTT]),
                                              in1=thr32[:, 0:NTT].unsqueeze(1).broadcast_to([P, NE, NTT]), op=ALU.is_gt),
             reads=[F, "hft0"], writes=["hft0"])
        S.op("dve", lambda e: e.tensor_reduce(out=tl, in_=cmpA, axis=AX.X, op=ALU.add), reads=["hft0"], writes=[F])
        S.op("dve", lambda e: e.tensor_copy(out=ca, in_=tl), reads=[F], writes=[F])
        cur, oth = ca, cb2
        sh = 1
        while sh < NE:
            S.op("dve", lambda e, cur=cur, oth=oth, sh=sh: e.tensor_copy(out=oth[:, 0:sh], in_=cur[:, 0:sh]), reads=[F],
                 writes=[F])
            S.op("dve", lambda e, cur=cur, oth=oth, sh=sh: e.tensor_tensor(out=oth[:, sh:NE], in0=cur[:, sh:NE],
                                                                           in1=cur[:, 0:NE - sh], op=ALU.add),
                 reads=[F], writes=[F])
            cur, oth = oth, cur
            sh *= 2
        tend = cur
        S.op("dve", lambda e: e.tensor_tensor(out=tstart, in0=tend, in1=tl, op=ALU.subtract), reads=[F], writes=[F])
        tsl = fin[:, 160:160 + 2 * NTT].rearrange("p (t k) -> p t k", k=2)
        flr = fin[:, 256:256 + 2 * NTT].rearrange("p (t k) -> p t k", k=2)
        sres = fin[:, 384:384 + 2 * NTT].rearrange("p (t k) -> p t k", k=2)
        tmp = fin[:, 512:512 + 2 * NTT].rearrange("p (t k) -> p t k", k=2)
        prodA = hft[0][:, 0:NTT * NE].rearrange("p (t e) -> p t e", e=NE)
        tsb = tstart.unsqueeze(1).broadcast_to([P, NTT, NE])
        for kk, Mx in ((0, M1all), (1, M2all)):
            S.op("dve", lambda e, Mx=Mx: e.tensor_tensor(out=prodA, in0=Mx[:, :, :], in1=tsb, op=ALU.mult),
                 reads=[F, "Mall", "hft0"], writes=["hft0"])
            S.op("dve", lambda e, kk=kk: e.tensor_reduce(out=tsl[:, :, kk], in_=prodA, axis=AX.X, op=ALU.add),
                 reads=["hft0"], writes=[F])
        rank = RT[:, :, 0:2]
        hflat0 = hnT[:, :, :].rearrange("p k t -> p (k t)")
        cmpF = hflat0[:, 0:2 * NTT * (NTT - 1)].rearrange("p (t k m) -> p t k m", k=2, m=NTT - 1)
        S.op("dve", lambda e: e.tensor_tensor(
            out=cmpF, in0=rank.unsqueeze(3).broadcast_to([P, NTT, 2, NTT - 1]),
            in1=thr32[:, 1:NTT].unsqueeze(1).unsqueeze(1).broadcast_to([P, NTT, 2, NTT - 1]), op=ALU.is_ge),
            reads=["RT", F, "hnT"], writes=["hnT"])
        S.op("dve", lambda e: e.tensor_reduce(out=flr, in_=cmpF, axis=AX.X, op=ALU.add), reads=["hnT"], writes=[F])
        S.op("dve", lambda e: e.tensor_tensor(out=tsl, in0=tsl, in1=flr, op=ALU.add), reads=[F], writes=[F])
        S.op("dve", lambda e: e.scalar_tensor_tensor(out=sres, in0=flr, scalar=-float(P), in1=rank, op0=ALU.mult,
                                                     op1=ALU.add), reads=[F, "RT"], writes=[F])
        S.op("dve", lambda e: e.scalar_tensor_tensor(out=tmp, in0=tsl, scalar=float(P), in1=sres, op0=ALU.mult,
                                                     op1=ALU.add), reads=[F], writes=[F])
        S.op("dve", lambda e: e.tensor_copy(out=yidx[:, :, :], in_=tmp), reads=[F], writes=["yidx"])
        jio = fin[:, 640:640 + NST]
        ej = fin[:, 768:768 + NST]
        pio = fin[:, 900:901]
        S.op("pool", lambda e: e.iota(jio, pattern=[[1, NST]], base=0, channel_multiplier=0,
                                      allow_small_or_imprecise_dtypes=True), writes=[F])
        S.op("pool", lambda e: e.iota(pio, pattern=[[0, 1]], base=0, channel_multiplier=1,
                                      allow_small_or_imprecise_dtypes=True), writes=[F])
        for c0 in range(0, NST, 32):
            nj = min(32, NST - c0)
            cmpE = hft[0][:, 0:nj * NE].rearrange("p (j e) -> p j e", e=NE)
            S.op("dve", lambda e, c0=c0, nj=nj, cmpE=cmpE: e.tensor_tensor(
                out=cmpE, in0=jio[:, c0:c0 + nj].unsqueeze(2).broadcast_to([P, nj, NE]),
                in1=tend.unsqueeze(1).broadcast_to([P, nj, NE]), op=ALU.is_ge), reads=[F, "hft0"], writes=["hft0"])
            S.op("dve", lambda e, c0=c0, nj=nj, cmpE=cmpE: e.tensor_reduce(out=ej[:, c0:c0 + nj], in_=cmpE, axis=AX.X,
                                                                           op=ALU.add), reads=["hft0"], writes=[F])
        S.op("dve", lambda e: e.tensor_scalar(out=ej, in0=ej, scalar1=float(NE - 1), scalar2=float(P), op0=ALU.min,
                                              op1=ALU.mult), reads=[F], writes=[F])
        S.op("dve", lambda e: e.tensor_scalar(out=ej, in0=ej, scalar1=pio, scalar2=None, op0=ALU.add),
             reads=[F], writes=[F])
        S.op("dve", lambda e: e.tensor_copy(out=idxw[:, :], in_=ej), reads=[F], writes=["idxw"])
        hflat = hnT[:, :, :].rearrange("p k t -> p (k t)")
        OS = [hflat[:, 0:P], hflat[:, P:2 * P]]
        OT = [hflat[:, 256:256 + NST], hflat[:, 256 + NST:256 + 2 * NST]]
        io128 = hflat[:, 512:512 + P]
        tokf = hflat[:, 640:640 + NTT]
        S.op("pool", lambda e: e.iota(io128, pattern=[[1, P]], base=0, channel_multiplier=0,
                                      allow_small_or_imprecise_dtypes=True), writes=["hnT"])
        S.op("pool", lambda e: e.iota(tokf, pattern=[[P, NTT]], base=0, channel_multiplier=1,
                                      allow_small_or_imprecise_dtypes=True), writes=["hnT"])
        tb = mbank()
        tbt = "pm%d" % tb
        n = 0
        for tt in range(NTT):
            for kk in range(2):
                sl = n % 2
                S.op("dve", lambda e, sl=sl, tt=tt, kk=kk: e.tensor_scalar(
                    out=OS[sl], in0=io128, scalar1=sres[:, tt, kk:kk + 1], scalar2=None, op0=ALU.is_equal),
                    reads=[F, "hnT"], writes=["OS%d" % sl])
                S.op("dve", lambda e, sl=sl, tt=tt, kk=kk: e.tensor_scalar(
                    out=OT[sl], in0=jio, scalar1=tsl[:, tt, kk:kk + 1], scalar2=tokf[:, tt:tt + 1], op0=ALU.is_equal,
                    op1=ALU.mult), reads=[F, "hnT"], writes=["OT%d" % sl])
                S.op("pe", lambda e, sl=sl, n=n: e.matmul(pm[tb][:, 0:NST], lhsT=OS[sl], rhs=OT[sl], start=(n == 0),
                                                          stop=(n == 2 * NTT - 1)),
                     reads=["OS%d" % sl, "OT%d" % sl], writes=[tbt])
                n += 1
        S.op("dve", lambda e: e.tensor_copy(out=toki[:, :], in_=pm[tb][:, 0:NST]), reads=[tbt],
             writes=["toki", "hf_d", "h1_d"])

        NSL = 4
        wof = w_out[:, :, :].rearrange("p k d -> p (k d)")
        wgu = [big[:, 0:4096], big[:, 4096:8192], big[:, 8192:12288], wof[:, 4096:8192]]
        wdn = [big[:, 12288:14336], big[:, 14336:16384], wof[:, 0:2048], wof[:, 2048:4096]]
        xg = [xt[0], xt[1], hft[0]]
        xgt = ["xt0", "xt1", "hft0"]
        NXS = 3

        def gather_x(j):
            sl = j % NXS
            S.op("pool", lambda e: e.indirect_dma_start(
                out=xg[sl][:, :], out_offset=None, in_=hf_d[:, :],
                in_offset=bass.IndirectOffsetOnAxis(ap=toki[:, j:j + 1], axis=0)),
                reads=["toki", "hf_d"], writes=[xgt[sl]], dma="xg%d" % sl)

        def gather_gu(j):
            sl = j % NSL
            first = []
            S.op("pool", lambda e: e.indirect_dma_start(
                out=wgu[sl], out_offset=None, in_=wgu_d[:, :],
                in_offset=bass.IndirectOffsetOnAxis(ap=idxw[:, j:j + 1], axis=0)),
                reads=["idxw"], writes=["wgu%d" % sl] + first, dma="wgu%d" % sl)

        def gather_dn(j):
            sl = j % NSL
            first = []
            S.op("pool", lambda e: e.indirect_dma_start(
                out=wdn[sl], out_offset=None, in_=wdn_d[:, :],
                in_offset=bass.IndirectOffsetOnAxis(ap=idxw[:, j:j + 1], axis=0)),
                reads=["idxw"], writes=["wdn%d" % sl] + first, dma="wdn%d" % sl)

        def xt_b(j):
            sl = j % NXS
            for h in range(2):
                b = mbank()
                for kk in range(4):
                    k = h * 4 + kk
                    S.op("pe", lambda e, b=b, kk=kk, k=k: e.transpose(out=pm[b][:, kk * P:(kk + 1) * P],
                                                                      in_=xg[sl][:, k * P:(k + 1) * P], identity=ident),
                         reads=[xgt[sl], "consts"], writes=["pm%d" % b])
                S.op("act" if h == 0 else "dve", lambda e, b=b, h=h: (e.copy if h == 0 else e.tensor_copy)(
                    out=hfT[:, h * 4:(h + 1) * 4, :], in_=pm[b][:, :].rearrange("p (k t) -> p k t", k=4)),
                    reads=["pm%d" % b], writes=["hfT"])

        def gu_b(j):
            sl = j % NSL
            for k in range(8):
                S.op("pe", lambda e, k=k: e.matmul(pin[0][:, :], lhsT=hfT[:, k, :], rhs=wgu[sl][:, k * 512:(k + 1) * 512],
                                                   start=(k == 0), stop=(k == 7)),
                     reads=["hfT", "wgu%d" % sl], writes=["pin0"])
            S.op("act", lambda e: e.activation(out=sgS[:, :], in_=pin[0][:, 0:256], func=AF.Silu), reads=["pin0"],
                 writes=["sgS"])
            S.op("dve", lambda e: e.tensor_tensor(out=aS[:, :], in0=sgS[:, :], in1=pin[0][:, 256:512], op=ALU.mult),
                 reads=["sgS", "pin0"], writes=["aS"])

        def at_b(j):
            for c in range(2):
                S.op("pe", lambda e, c=c: e.transpose(out=pin[1][:, c * P:(c + 1) * P], in_=aS[:, c * P:(c + 1) * P],
                                                      identity=ident), reads=["aS", "consts"], writes=["pin1"])
            S.op("act", lambda e: e.copy(out=aT[:, :, :], in_=pin[1][:, 0:2 * P].rearrange("p (c t) -> p c t", c=2)),
                 reads=["pin1"], writes=["aT"])

        def down_b(j):
            sl = j % NSL
            for half in range(2):
                for c in range(2):
                    S.op("pe", lambda e, half=half, c=c: e.matmul(
                        po[:, half * 512:(half + 1) * 512], lhsT=aT[:, c, :],
                        rhs=wdn[sl][:, c * 1024 + half * 512:c * 1024 + (half + 1) * 512],
                        start=(c == 0), stop=(c == 1)), reads=["aT", "wdn%d" % sl], writes=["po"])
            ys = j % 2
            yb, ytok = x2[ys], "x2%d" % ys
            S.op("act", lambda e: e.copy(out=yb[:, 0:512], in_=po[:, 0:512]), reads=["po"], writes=[ytok])
            S.op("dve", lambda e: e.tensor_copy(out=yb[:, 512:1024], in_=po[:, 512:1024]), reads=["po"], writes=[ytok])
            S.op("sp", lambda e: e.dma_start(out=y_d[j * P:(j + 1) * P, :], in_=yb[:, :]), reads=[ytok, "y_d"],
                 dma="ys%d" % ys)

        S.op("pool", lambda e: e.memset(bar[:, 0:1], 0.0), writes=WIN + ["w_out", "bar"])
        for j in range(NXS):
            gather_x(j)
        for j in range(NSL):
            gather_gu(j)
            gather_dn(j)
        xt_b(0)
        gu_b(0)
        if NSL < NST:
            gather_gu(NSL)
        for j in range(NST):
            if j + 1 < NST:
                xt_b(j + 1)
            if j >= 1:
                down_b(j - 1)
                if j - 1 + NSL < NST:
                    gather_dn(j - 1 + NSL)
            at_b(j)
            if j + 1 < NST:
                gu_b(j + 1)
                if j + 1 + NSL < NST:
                    gather_gu(j + 1 + NSL)
            if j + NXS < NST:
                gather_x(j + NXS)
        down_b(NST - 1)

        fn_row = yT[:, :, :].rearrange("p k t -> p (k t)")[:, 0:D]
        S.op("sp", lambda e: e.dma_start(out=fn_row, in_=rowv_d[:, D:2 * D]), writes=YT_ALL + ["y_d"], dma="c5")
        wo = [w_out[:, k, :] for k in range(8)]
        NCS = 4
        cy1 = [xt[0][:, :], xt[1][:, :], wo[0], wo[1]]
        cy2 = [x2[0][:, :], x2[1][:, :], wo[2], wo[3]]
        chb = [hft[0][:, :], hft[1][:, :], wo[4], wo[5]]
        cout = [wo[6], wo[7]]
        first_c = set()

        def c_loads(tt):
            sl = tt % NCS
            r0 = tt * P
            extra = []
            first_c.add(sl)
            S.op("pool", lambda e: e.indirect_dma_start(
                out=cy1[sl], out_offset=None, in_=y_d[:, :],
                in_offset=bass.IndirectOffsetOnAxis(ap=yidx[:, tt, 0:1], axis=0)),
                reads=["yidx", "y_d"], writes=["cy1%d" % sl] + (["xt%d" % sl] if sl < 2 else extra), dma="cy1%d" % sl)
            S.op("pool", lambda e: e.indirect_dma_start(
                out=cy2[sl], out_offset=None, in_=y_d[:, :],
                in_offset=bass.IndirectOffsetOnAxis(ap=yidx[:, tt, 1:2], axis=0)),
                reads=["yidx", "y_d"], writes=["cy2%d" % sl] + (["x2%d" % sl] if sl < 2 else extra), dma="cy2%d" % sl)
            S.op("sp", lambda e: e.dma_start(out=chb[sl], in_=h1_d[r0:r0 + P, :]), reads=["h1_d", "barc"],
                 writes=["chb%d" % sl] + (["hft%d" % sl] if sl < 2 else extra), dma="ch%d" % sl)

        S.op("pool", lambda e: e.memset(bar[:, 1:2], 0.0),
             writes=["wgu%d" % i for i in range(4)] + ["wdn%d" % i for i in range(4)] + ["barc"])
        c_loads(0)
        c_loads(1)
        for tt in range(NTT):
            sl = tt % NCS
            r0 = tt * P
            y1, y1t = cy1[sl], "cy1%d" % sl
            y2, y2t = cy2[sl], "cy2%d" % sl
            hb, hbt = chb[sl], "chb%d" % sl
            ob, obt = cout[tt % 2], "cout%d" % (tt % 2)
            if tt + 2 < NTT:
                c_loads(tt + 2)
            S.op("dve", lambda e, tt=tt, y1=y1, hb=hb: e.scalar_tensor_tensor(
                out=hb, in0=y1, scalar=RT[:, tt, 2:3], in1=hb, op0=ALU.mult, op1=ALU.add),
                reads=[y1t, hbt, "RT"], writes=[hbt])
            S.op("dve", lambda e, tt=tt, y2=y2, hb=hb: e.scalar_tensor_tensor(
                out=hb, in0=y2, scalar=RT[:, tt, 3:4], in1=hb, op0=ALU.mult, op1=ALU.add),
                reads=[y2t, hbt, "RT"], writes=[hbt])
            c = sscol()
            sst = "ss%d" % c
            S.op("act", lambda e, hb=hb, c=c: e.activation(out=junk[:, :], in_=hb, func=AF.Square,
                                                           accum_out=ssb[:, c:c + 1]), reads=[hbt],
                 writes=["junk", sst])
            rstd_ops(ssb[:, c:c + 1], ssb[:, c:c + 1], float(D), [sst], [sst])
            first_o = []
            S.op("dve", lambda e, hb=hb, c=c, ob=ob: e.scalar_tensor_tensor(
                out=ob, in0=hb, scalar=ssb[:, c:c + 1], in1=fn_row, op0=ALU.mult, op1=ALU.mult),
                reads=[hbt, sst, "barc"] + YT_ALL, writes=[obt] + first_o)
            S.op("act", lambda e, ob=ob, r0=r0: e.dma_start(out=out_d[r0:r0 + P, :], in_=ob), reads=[obt],
                 dma="os%d" % (tt % 2))
        S.emit()
    return nc


def prep_shared(inp):
    f = np.float32
    w_in = np.ascontiguousarray(inp["w_in"][0], f)
    w_out = np.ascontiguousarray(inp["w_out"][0], f)
    wr = np.ascontiguousarray(np.concatenate([inp["w_router_group"][0], inp["w_router_expert"][0]], axis=1), f)
    poolw = np.ascontiguousarray(np.transpose(inp["pool_w"][0], (1, 0, 2)).reshape(P, 4 * P), f)
    colv = np.zeros((P, 32), f)
    colv[:, 0:8] = inp["norm_mix"][0].reshape(8, P).T
    cw = inp["conv_w"][0]
    for ch in range(4):
        for tap in range(3):
            colv[:, 8 + ch * 3 + tap] = cw[tap, ch * P:(ch + 1) * P]
    colv[:, 20:24] = inp["norm_conv_out"][0].reshape(4, P).T
    colv[:, 24:28] = inp["pool_scale"][0].reshape(4, P).T
    colv[:, 28:32] = inp["norm_pool_out"][0].reshape(4, P).T
    rowv = np.empty((P, 2 * D), f)
    rowv[:, 0:D] = inp["norm_ffn"][0][None, :]
    rowv[:, D:] = inp["final_norm"][None, :]
    consts = np.zeros((P, 3 * P), f)
    consts[:, 0:P] = np.eye(P, dtype=f)
    consts[:, P:2 * P] = np.triu(np.ones((P, P), f), 1)
    consts[:, 2 * P:] = 1.0
    wg, wu, wd = inp["w_gate"][0], inp["w_up"][0], inp["w_down"][0]
    wall_gu = np.empty((NE, P, 8, 512), f)
    wall_gu[:, :, :, 0:256] = wg.reshape(NE, 8, P, 256).transpose(0, 2, 1, 3)
    wall_gu[:, :, :, 256:512] = wu.reshape(NE, 8, P, 256).transpose(0, 2, 1, 3)
    wall_dn = np.ascontiguousarray(wd.reshape(NE, 2, P, D).transpose(0, 2, 1, 3))
    return dict(w_in=w_in, w_out=w_out, wr=wr, poolw=poolw, colv=colv, rowv=rowv, consts=consts,
                wall_gu=wall_gu.reshape(NE * P, 4096), wall_dn=wall_dn.reshape(NE * P, 2048))


def core_inputs(x, meta, shared, NT):
    B, Sq, _ = x.shape
    nh = Sq // NT
    maps = []
    for c in range(B * nh):
        b, h = divmod(c, nh)
        xm = np.ascontiguousarray(x[b, h * NT:(h + 1) * NT], np.float32)
        xp = np.zeros((P, D), np.float32)
        xp[P - HALO:] = meta if h == 0 else x[b, h * NT - HALO:h * NT]
        m = dict(shared)
        m["x"] = xm
        m["xp"] = xp
        maps.append(m)
    return maps


_NC_CACHE = {}


def kernel(x, meta_tokens, norm_mix, w_in, conv_w, norm_conv_out, pool_w, pool_scale, norm_pool_out, w_out,
           norm_ffn, w_router_group, w_router_expert, w_gate, w_up, w_down, final_norm):
    inp = dict(norm_mix=norm_mix, w_in=w_in, conv_w=conv_w, norm_conv_out=norm_conv_out, pool_w=pool_w,
               pool_scale=pool_scale, norm_pool_out=norm_pool_out, w_out=w_out, norm_ffn=norm_ffn,
               w_router_group=w_router_group, w_router_expert=w_router_expert, w_gate=w_gate, w_up=w_up,
               w_down=w_down, final_norm=final_norm)
    inp = {k: np.asarray(v, np.float32) for k, v in inp.items()}
    x = np.asarray(x, np.float32)
    meta = np.asarray(meta_tokens, np.float32)
    B, Sq, _ = x.shape
    NT = B * Sq // NCORES
    shared = prep_shared(inp)
    maps = core_inputs(x, meta, shared, NT)
    if NT not in _NC_CACHE:
        _NC_CACHE[NT] = build(NT)
    res = run_bass_kernel_spmd(_NC_CACHE[NT], maps, core_ids=list(range(NCORES)))
    out = np.empty((B, Sq, D), np.float32)
    nh = Sq // NT
    for c in range(NCORES):
        b, h = divmod(c, nh)
        out[b, h * NT:(h + 1) * NT] = res.results[c]["out"]
    return out
```

```python
import numpy as np
from contextlib import ExitStack
import concourse.bass as bass
import concourse.mybir as mybir
from concourse.bass_utils import run_bass_kernel_spmd

F32 = mybir.dt.float32
I32 = mybir.dt.int32
BF16 = mybir.dt.bfloat16
ALU = mybir.AluOpType
AF = mybir.ActivationFunctionType
AX = mybir.AxisListType

P = 128
D = 1024
NB = 256
HALO = 16
NE = 32
EPS = 1e-6
NCORES = 8
SEQ_PER_CORE = 4096

ENGS = ("pe", "act", "dve", "pool", "sp")


class Sched:
    def __init__(self, nc, stack):
        self.nc = nc
        self.stack = stack
        self.ops = []
        self.last_w = {}
        self.readers = {}

    def op(self, eng, fn, reads=(), writes=(), dma=None):
        i = len(self.ops)
        deps = set()
        for t in tuple(reads) + tuple(writes):
            if t in self.last_w:
                deps.add(self.last_w[t])
        for t in writes:
            r = self.readers.get(t)
            if r:
                deps.update(r[0].values())
                deps.update(r[1])
        deps.discard(i)
        for t in reads:
            r = self.readers.setdefault(t, ({}, []))
            if dma is None:
                r[0][eng] = i
            else:
                r[1].append(i)
        for t in writes:
            self.last_w[t] = i
            self.readers[t] = ({}, [])
        self.ops.append(dict(eng=eng, fn=fn, deps=deps, dma=dma, sig=False))
        return i

    def emit(self):
        nc, ops = self.nc, self.ops
        for o in ops:
            for d in o["deps"]:
                p = ops[d]
                if p["dma"] is None and o["dma"] is None and p["eng"] == "pe" and o["eng"] == "pe":
                    continue
                p["sig"] = True
        eng_sem = {e: self.stack.enter_context(nc.semaphore("prog_" + e)) for e in ENGS}
        cnt = {e: 0 for e in ENGS}
        dma_sems, dcnt = {}, {}
        for o in ops:
            if o["dma"] is not None:
                k = o["dma"]
                if k not in dma_sems:
                    dma_sems[k] = self.stack.enter_context(nc.semaphore("dma_" + k))
                    dcnt[k] = 0
                dcnt[k] += 16
                o["signal"] = (dma_sems[k], dcnt[k], "D" + k)
            elif o["sig"]:
                cnt[o["eng"]] += 1
                o["signal"] = (eng_sem[o["eng"]], cnt[o["eng"]], "E" + o["eng"])
        per_eng = {e: [] for e in ENGS}
        for o in ops:
            per_eng[o["eng"]].append(o)
        block = self.stack.enter_context(nc.Block())

        def run(engname):
            def body(eng):
                w = {}
                for o in per_eng[engname]:
                    need = {}
                    for d in o["deps"]:
                        p = ops[d]
                        if "signal" not in p:
                            continue
                        sem, val, key = p["signal"]
                        if need.get(key, (None, 0))[1] < val:
                            need[key] = (sem, val)
                    for key, (sem, val) in need.items():
                        if w.get(key, 0) >= val:
                            continue
                        eng.wait_ge(sem, val)
                        w[key] = val
                    ins = o["fn"](eng)
                    if "signal" in o:
                        ins.then_inc(o["signal"][0], 16 if o["dma"] is not None else 1)
                if engname == "sp":
                    for k, sem in dma_sems.items():
                        eng.wait_ge(sem, dcnt[k])
            return body

        block.tensor(run("pe"))
        block.scalar(run("act"))
        block.vector(run("dve"))
        block.gpsimd(run("pool"))
        block.sync(run("sp"))


def build(NT, MM_DT=F32):
    NTT = NT // P
    NBLK = NT // NB
    NST = (2 * NT + NE * (P - 1)) // P
    SUB = NB // P
    nc = bass.Bass("TRN2", target_bir_lowering=False)

    def din(name, shape, dt=F32):
        return nc.dram_tensor(name, shape, dt, kind="ExternalInput").ap()

    x_d = din("x", [NT, D])
    xp_d = din("xp", [P, D])
    w_in_d = din("w_in", [D, 2048])
    w_out_d = din("w_out", [D, D])
    wr_d = din("wr", [D, 36])
    poolw_d = din("poolw", [P, 4 * P])
    colv_d = din("colv", [P, 32])
    rowv_d = din("rowv", [P, 2 * D])
    consts_d = din("consts", [P, 3 * P])
    wgu_d = din("wall_gu", [NE * P, 4096])
    wdn_d = din("wall_dn", [NE * P, 2048])
    out_d = nc.dram_tensor("out", [NT, D], F32, kind="ExternalOutput").ap()
    hf_d = nc.dram_tensor("hf_scr", [NT, D], F32, kind="Internal").ap()
    h1_d = nc.dram_tensor("h1_scr", [NT, D], F32, kind="Internal").ap()
    y_d = nc.dram_tensor("y_scr", [NST * P, D], F32, kind="Internal").ap()

    with ExitStack() as st:
        def sb(name, shape, dt=F32):
            return st.enter_context(nc.sbuf_tensor("s_" + name, shape, dt))

        def psum(name, shape):
            return st.enter_context(nc.psum_tensor("p_" + name, shape, F32))

        S = Sched(nc, st)
        big = sb("big", [P, 16384])
        w_in = big[:, :].rearrange("p (k c) -> p k c", k=8)
        w_out = sb("w_out", [P, 8, D])
        wr = sb("wr_sb", [P, 8, 36])
        poolw = sb("poolw_sb", [P, 4, P])
        poolw_s = sb("poolw_s", [P, 4, P])
        poolw_n = sb("poolw_n", [P, 4, P])
        colv = sb("colv_sb", [P, 32])
        rowv = sb("rowv_sb", [P, D])
        consts = sb("consts_sb", [P, 3 * P])
        ident, utri, ones = consts[:, 0:P], consts[:, P:2 * P], consts[:, 2 * P:3 * P]
        xt = [sb("xt0", [P, D]), sb("xt1", [P, D])]
        x2 = [sb("x20", [P, D]), sb("x21", [P, D])]
        hft = [sb("hft0", [P, D]), sb("hft1", [P, D])]
        junk = sb("junk", [P, D], BF16)
        hnT = sb("hnT", [P, 8, NB])
        bS = sb("bS", [P, 4, NB])
        cS = sb("cS", [P, 4, NB])
        vS = sb("vS", [P, 4, NB])
        uS = sb("uS", [P, 4, NB + HALO])
        vpS = sb("vpS", [P, 4, NB + HALO])
        acc = [sb("acc0", [P, NB]), sb("acc1", [P, NB])]
        sq4 = sb("sq4", [P, 4, NB])
        pooled4 = sb("pooled4", [P, 4, NB])
        tA = sb("tA", [P, NB + HALO])
        tB = sb("tB", [P, NB + HALO])
        rsn = sb("rsn", [P, 2, NB])
        yT = sb("yT", [P, 8, NB])
        hfT = sb("hfT", [P, 8, P])
        sm2 = sb("sm", [P, 2, 256])
        lgs = sb("lgs", [P, 2, 36])
        m12a = sb("m12", [P, 2, NE])
        srun = sb("srun", [P, NE])
        M1all = sb("M1all", [P, NTT, NE], BF16)
        M2all = sb("M2all", [P, NTT, NE], BF16)
        RT = sb("RT", [P, NTT, 4])
        ssb = sb("ssb", [P, 16])
        fin = hft[1]
        yidx = sb("yidx", [P, NTT, 2], I32)
        toki = sb("toki", [P, NST], I32)
        bar = sb("bar", [P, 2])
        idxw = sb("idxw", [P, NST], I32)
        sgS = sb("sgS", [P, 256])
        aS = sb("aS", [P, 256])
        aT = sb("aT", [P, 2, P])

        pin = [psum("pin%d" % i, [P, 512]) for i in range(3)]
        pm = [psum("pm%d" % i, [P, 512]) for i in range(3)]
        po = psum("po", [P, 1024])
        mctr = [0]

        def mbank():
            i = mctr[0] % 3
            mctr[0] += 1
            return i

        ssc = [0]

        def sscol():
            i = ssc[0] % 16
            ssc[0] += 1
            return i

        S.op("sp", lambda e: e.dma_start(out=consts[:, :], in_=consts_d[:, :]), writes=["consts"], dma="c0")
        S.op("sp", lambda e: e.dma_start(out=colv[:, :], in_=colv_d[:, :]), writes=["colv"], dma="c1")
        def setup_weights():
            for k in range(8):
                S.op("sp", lambda e, k=k: e.dma_start(out=w_in[:, k, :], in_=w_in_d[k * P:(k + 1) * P, :]),
                     writes=["w_in%d" % k], dma="win%d" % k)
                if k % 2:
                    S.op("act", lambda e, k=k: e.mul(out=w_in[:, k, :], in_=w_in[:, k, :], mul=colv[:, k:k + 1]),
                         reads=["colv"], writes=["w_in%d" % k])
                else:
                    S.op("dve", lambda e, k=k: e.tensor_scalar(out=w_in[:, k, :], in0=w_in[:, k, :],
                                                              scalar1=colv[:, k:k + 1], scalar2=None, op0=ALU.mult),
                         reads=["colv"], writes=["w_in%d" % k])
            S.op("sp", lambda e: e.dma_start(out=poolw[:, :, :], in_=poolw_d[:, :].rearrange("p (g d) -> p g d", g=4)),
                 writes=["poolw"], dma="c2")
            for g in range(4):
                S.op("dve", lambda e, g=g: e.tensor_scalar(out=poolw_s[:, g, :], in0=poolw[:, g, :], scalar1=1.0 / (2 << g),
                                                          scalar2=None, op0=ALU.mult), reads=["poolw"], writes=["poolw_s"])
            S.op("dve", lambda e: e.tensor_scalar(out=poolw_n[:, :, :], in0=poolw[:, :, :], scalar1=-1.0, scalar2=None,
                                                  op0=ALU.mult), reads=["poolw"], writes=["poolw_n"])
            S.op("sp", lambda e: e.dma_start(out=wr[:, :, :], in_=wr_d[:, :].rearrange("(k p) n -> p k n", p=P)),
                 writes=["wr"], dma="c3")
            S.op("sp", lambda e: e.dma_start(out=rowv[:, :], in_=rowv_d[:, 0:D]), writes=["rowv"], dma="c4")
            S.op("dve", lambda e: e.memset(srun[:, :], 0.0), writes=["srun"])
        WIN = ["w_in%d" % k for k in range(8)]

        def rstd_ops(src_ap, dst_ap, n, rtok, wtok):
            S.op("act", lambda e: e.activation(out=dst_ap, in_=src_ap, func=AF.Ln, bias=EPS, scale=1.0 / n),
                 reads=rtok, writes=wtok)
            S.op("act", lambda e: e.activation(out=dst_ap, in_=dst_ap, func=AF.Exp, scale=-0.5), reads=wtok,
                 writes=wtok)

        def stage_a_act(src_rows, slot):
            xs = xt[slot]
            tk = "xt%d" % slot
            S.op("sp", lambda e: e.dma_start(out=xs[:, :], in_=src_rows), writes=[tk], dma=tk)
            c = sscol()
            sst = "ss%d" % c
            S.op("act", lambda e: e.activation(out=junk[:, :], in_=xs[:, :], func=AF.Square,
                                               accum_out=ssb[:, c:c + 1]), reads=[tk], writes=["junk", sst])
            rstd_ops(ssb[:, c:c + 1], ssb[:, c:c + 1], float(D), [sst], [sst])
            S.op("act", lambda e: e.mul(out=xs[:, :], in_=xs[:, :], mul=ssb[:, c:c + 1]), reads=[sst], writes=[tk])

        def stage_a_tr(slot, col0):
            xs = xt[slot]
            tk = "xt%d" % slot
            for h in range(2):
                b = mbank()
                for kk in range(4):
                    k = h * 4 + kk
                    S.op("pe", lambda e, b=b, kk=kk, k=k: e.transpose(out=pm[b][:, kk * P:(kk + 1) * P],
                                                                      in_=xs[:, k * P:(k + 1) * P], identity=ident),
                         reads=[tk, "consts"], writes=["pm%d" % b])
                S.op("act", lambda e, b=b, h=h: e.copy(out=hnT[:, h * 4:(h + 1) * 4, col0:col0 + P],
                                                       in_=pm[b][:, :].rearrange("p (k t) -> p k t", k=4)),
                     reads=["pm%d" % b], writes=["hnT"])

        def stage_a(src_rows, slot, col0):
            stage_a_act(src_rows, slot)
            stage_a_tr(slot, col0)

        pinc = [0]

        def inproj_group(g, ncols, prefix=False):
            b = pinc[0] % 3
            pinc[0] += 1
            for cc in range(2):
                c = 2 * g + cc
                for k in range(8):
                    S.op("pe", lambda e, b=b, cc=cc, c=c, k=k: e.matmul(
                        pin[b][:, cc * ncols:(cc + 1) * ncols], lhsT=w_in[:, k, c * P:(c + 1) * P],
                        rhs=hnT[:, k, 0:ncols], start=(k == 0), stop=(k == 7)),
                        reads=["hnT", WIN[k]], writes=["pin%d" % b])
            src = pin[b][:, 0:2 * ncols].rearrange("p (c t) -> p c t", c=2)
            j = 2 * (g % 2)
            if not prefix:
                if g < 2:
                    dst, tok = bS[:, j:j + 2, :], "bS"
                elif g < 4:
                    dst, tok = cS[:, j:j + 2, :], "cS"
                elif g < 6:
                    dst, tok = vS[:, j:j + 2, :], "vS"
                else:
                    dst, tok = vpS[:, j:j + 2, HALO:HALO + NB], "vpm"
                S.op("act", lambda e: e.copy(out=dst, in_=src), reads=["pin%d" % b], writes=[tok])
            else:
                srch = src[:, :, ncols - HALO:ncols]
                if g in (2, 3):
                    S.op("act", lambda e: e.copy(out=cS[:, j:j + 2, 0:HALO], in_=srch), reads=["pin%d" % b], writes=["cS"])
                elif g in (4, 5):
                    S.op("dve", lambda e: e.tensor_tensor(out=uS[:, j:j + 2, 0:HALO], in0=cS[:, j:j + 2, 0:HALO],
                                                          in1=srch, op=ALU.mult),
                         reads=["pin%d" % b, "cS"], writes=["uh"])
                else:
                    S.op("act", lambda e: e.copy(out=vpS[:, j:j + 2, 0:HALO], in_=srch), reads=["pin%d" % b], writes=["vph"])

        def mix_part1():
            S.op("dve", lambda e: e.tensor_tensor(out=uS[:, :, HALO:HALO + NB], in0=cS[:, :, :], in1=vS[:, :, :],
                                                  op=ALU.mult), reads=["cS", "vS"], writes=["um"])
            for ch in range(4):
                a = acc[ch % 2]
                at = "acc%d" % (ch % 2)
                S.op("dve", lambda e, a=a, ch=ch: e.tensor_scalar(out=a[:, :], in0=uS[:, ch, HALO - 2:HALO - 2 + NB],
                                                                 scalar1=colv[:, 8 + ch * 3:9 + ch * 3], scalar2=None,
                                                                 op0=ALU.mult), reads=["um", "uh", "colv"], writes=[at])
                for tap in (1, 2):
                    S.op("dve", lambda e, a=a, ch=ch, tap=tap: e.scalar_tensor_tensor(
                        out=a[:, :], in0=uS[:, ch, HALO - 2 + tap:HALO - 2 + tap + NB],
                        scalar=colv[:, 8 + ch * 3 + tap:9 + ch * 3 + tap], in1=a[:, :], op0=ALU.mult, op1=ALU.add),
                        reads=["um", "uh", at], writes=[at])
                S.op("dve", lambda e, a=a, ch=ch: e.tensor_tensor(out=yT[:, ch, :], in0=bS[:, ch, :], in1=a[:, :],
                                                                 op=ALU.mult), reads=["bS", at], writes=["yTc%d" % ch])
            S.op("dve", lambda e: e.tensor_copy(out=uS[:, :, HALO - 2:HALO], in_=uS[:, :, NB + HALO - 2:NB + HALO]),
                 reads=["um"], writes=["uh"])
            W = NB + HALO
            for g in range(4):
                w = 2 << g
                src = vpS[:, g, :]
                cur, curtok = src, None
                lo_needed = HALO
                lvl = 1
                bufs = [tA, tB]
                bi = 0
                while lvl < w:
                    nl = lvl * 2
                    lo = HALO - (w - nl)
                    last = nl == w
                    rt = ["vpm", "vph"] if curtok is None else [curtok]
                    if last:
                        S.op("pool", lambda e, cur=cur, lvl=lvl, g=g: e.tensor_tensor(
                            out=pooled4[:, g, :], in0=cur[:, HALO:W], in1=cur[:, HALO - lvl:W - lvl], op=ALU.add),
                            reads=rt, writes=["pooled%d" % g])
                    else:
                        dstb = bufs[bi]
                        dtok = "tA" if bi == 0 else "tB"
                        S.op("pool", lambda e, dstb=dstb, cur=cur, lo=lo, lvl=lvl: e.tensor_tensor(
                            out=dstb[:, lo:W], in0=cur[:, lo:W], in1=cur[:, lo - lvl:W - lvl], op=ALU.add),
                            reads=rt, writes=[dtok])
                        cur, curtok = dstb, dtok
                        bi ^= 1
                    lvl = nl
            S.op("pool", lambda e: e.tensor_copy(out=vpS[:, :, 0:HALO], in_=vpS[:, :, NB:NB + HALO]),
                 reads=["vpm"], writes=["vph"])

        def mix_part1b():
            for g in range(4):
                pb = pooled4[:, g, :]
                pt = "pooled%d" % g
                if g % 2 == 0:
                    mix_part1.bank[g // 2] = mbank()
                mb_ = mix_part1.bank[g // 2]
                S.op("pe", lambda e, mb_=mb_, g=g, pb=pb: e.matmul(pm[mb_][:, (g % 2) * NB:(g % 2 + 1) * NB],
                                                                   lhsT=poolw_s[:, g, :], rhs=pb, start=True, stop=False),
                     reads=[pt, "poolw_s"], writes=["pm%d" % mb_])
                S.op("pe", lambda e, mb_=mb_, g=g: e.matmul(pm[mb_][:, (g % 2) * NB:(g % 2 + 1) * NB],
                                                            lhsT=poolw_n[:, g, :], rhs=vpS[:, g, HALO:HALO + NB],
                                                            start=False, stop=True),
                     reads=["vpm", "poolw_n"], writes=["pm%d" % mb_])
            for g in range(4):
                mb_ = mix_part1.bank[g // 2]
                S.op("dve", lambda e, mb_=mb_, g=g: e.tensor_scalar(out=yT[:, 4 + g, :],
                                                                     in0=pm[mb_][:, (g % 2) * NB:(g % 2 + 1) * NB],
                                                                     scalar1=colv[:, 24 + g:25 + g], scalar2=None,
                                                                     op0=ALU.mult),
                     reads=["pm%d" % mb_, "colv"], writes=["yTp%d" % g])

        mix_part1.bank = [0, 0]

        mix2_bank = [0]

        def mix2_pre(half):
            pre = "yTc" if half == 0 else "yTp"
            S.op("act", lambda e: e.activation(out=sq4[:, :, :], in_=yT[:, half * 4:half * 4 + 4, :], func=AF.Square),
                 reads=[pre + "%d" % i for i in range(4)], writes=["sq4"])
            ab = acc[half]
            S.op("dve", lambda e: e.tensor_reduce(out=ab[:, :], in_=sq4[:, :, :].rearrange("p c t -> p t c"),
                                                  axis=AX.X, op=ALU.add), reads=["sq4"], writes=["acc%d" % half])

        def mix2_mm():
            sb_ = mbank()
            for half in range(2):
                S.op("pe", lambda e, half=half: e.matmul(pm[sb_][:, half * NB:(half + 1) * NB], lhsT=ones,
                                                         rhs=acc[half][:, :], start=True, stop=True),
                     reads=["acc%d" % half, "consts"], writes=["pm%d" % sb_])
            for half in range(2):
                rstd_ops(pm[sb_][:, half * NB:(half + 1) * NB], rsn[:, half, :], 512.0, ["pm%d" % sb_], ["rsn%d" % half])

        def mix_part3():
            for half, pre, gcol in ((0, "yTc", 20), (1, "yTp", 28)):
                for ch in range(4):
                    S.op("dve", lambda e, half=half, ch=ch, gcol=gcol: e.scalar_tensor_tensor(
                        out=yT[:, half * 4 + ch, :], in0=yT[:, half * 4 + ch, :], scalar=colv[:, gcol + ch:gcol + ch + 1],
                        in1=rsn[:, half, :], op0=ALU.mult, op1=ALU.mult),
                        reads=[pre + "%d" % ch, "rsn%d" % half, "colv"], writes=[pre + "%d" % ch])

        YT_ALL = ["yTc%d" % i for i in range(4)] + ["yTp%d" % i for i in range(4)]

        def stage_d(tt, s):
            r0 = tt * P
            slot = tt % 2
            xb, xtok = x2[slot], "x2%d" % slot
            hb, htok = hft[slot], "hft%d" % slot
            S.op("sp", lambda e: e.dma_start(out=xb[:, :], in_=x_d[r0:r0 + P, :]), writes=[xtok], dma=xtok)
            for half in range(2):
                for k in range(8):
                    S.op("pe", lambda e, half=half, k=k: e.matmul(
                        po[:, half * 512:(half + 1) * 512], lhsT=yT[:, k, s * P:(s + 1) * P],
                        rhs=w_out[:, k, half * 512:(half + 1) * 512], start=(k == 0), stop=(k == 7)),
                        reads=YT_ALL + ["w_out"], writes=["po"])
            S.op("dve", lambda e: e.tensor_tensor(out=xb[:, :], in0=xb[:, :], in1=po[:, :], op=ALU.add),
                 reads=["po", xtok], writes=[xtok])
            c = sscol()
            sst = "ss%d" % c
            S.op("act", lambda e: e.activation(out=junk[:, :], in_=xb[:, :], func=AF.Square,
                                               accum_out=ssb[:, c:c + 1]), reads=[xtok], writes=["junk", sst])
            rstd_ops(ssb[:, c:c + 1], ssb[:, c:c + 1], float(D), [sst], [sst])
            S.op("dve", lambda e: e.scalar_tensor_tensor(out=hb[:, :], in0=xb[:, :], scalar=ssb[:, c:c + 1],
                                                         in1=rowv[:, :], op0=ALU.mult, op1=ALU.mult),
                 reads=[xtok, sst, "rowv"], writes=[htok])
            S.op("act", lambda e: e.dma_start(out=h1_d[r0:r0 + P, :], in_=xb[:, :]), reads=[xtok, "h1_d"],
                 dma="h1s%d" % slot)
            S.op("act", lambda e: e.dma_start(out=hf_d[r0:r0 + P, :], in_=hb[:, :]), reads=[htok, "hf_d"],
                 dma="hfs%d" % slot)

        def stage_dtr(tt, s):
            slot = tt % 2
            hb, htok = hft[slot], "hft%d" % slot
            for h in range(2):
                b = mbank()
                for kk in range(4):
                    k = h * 4 + kk
                    S.op("pe", lambda e, b=b, kk=kk, k=k: e.transpose(out=pm[b][:, kk * P:(kk + 1) * P],
                                                                      in_=hb[:, k * P:(k + 1) * P], identity=ident),
                         reads=[htok, "consts"], writes=["pm%d" % b])
                S.op("act", lambda e, b=b, h=h: e.copy(out=hfT[:, h * 4:(h + 1) * 4, :],
                                                       in_=pm[b][:, :].rearrange("p (k t) -> p k t", k=4)),
                     reads=["pm%d" % b], writes=["hfT"])

        def stage_drt(tt, s):
            rb = mbank()
            rbt = "pm%d" % rb
            for k in range(8):
                S.op("pe", lambda e, k=k: e.matmul(pm[rb][:, 0:36], lhsT=hfT[:, k, :], rhs=wr[:, k, :], start=(k == 0),
                                                   stop=(k == 7)), reads=["hfT", "wr"], writes=[rbt])
            S.op("dve", lambda e: e.tensor_copy(out=lgs[:, s, :], in_=pm[rb][:, 0:36]), reads=[rbt],
                 writes=["lg%d" % s])

        def stage_d2(tt, s):
            lg = lgs[:, s, :]
            sm = sm2[:, s, :]
            m12 = m12a[:, s, :]
            M12T = "m12_%d" % s
            gmax, ngmax, sume, g1 = sm[:, 40:41], sm[:, 41:42], sm[:, 42:43], sm[:, 43:44]
            goh = sm[:, 44:48]
            eg = sm[:, 48:52]
            sel = sm[:, 56:64]
            top8 = sm[:, 64:72]
            mask2 = sm[:, 72:80]
            mask1 = sm[:, 80:88]
            dd, ed = sm[:, 88:89], sm[:, 89:90]
            prod = sm[:, 96:128]
            m1f = sm[:, 128:160]
            m2f = sm[:, 160:192]
            T = "sm%d" % s
            S.op("dve", lambda e: e.tensor_reduce(out=gmax, in_=lg[:, 0:4], axis=AX.X, op=ALU.max),
                 reads=[T, "lg%d" % s], writes=[T])
            S.op("dve", lambda e: e.tensor_scalar(out=goh, in0=lg[:, 0:4], scalar1=gmax, scalar2=None, op0=ALU.is_ge),
                 reads=[T], writes=[T])
            S.op("dve", lambda e: e.tensor_scalar(out=ngmax, in0=gmax, scalar1=-1.0, scalar2=None, op0=ALU.mult),
                 reads=[T], writes=[T])
            S.op("act", lambda e: e.activation(out=eg, in_=lg[:, 0:4], func=AF.Exp, bias=ngmax, accum_out=sume),
                 reads=[T], writes=[T])
            S.op("dve", lambda e: e.reciprocal(out=g1, in_=sume), reads=[T], writes=[T])
            S.op("dve", lambda e: e.tensor_scalar(out=sel, in0=lg[:, 4:12], scalar1=goh[:, 0:1], scalar2=None,
                                                  op0=ALU.mult), reads=[T], writes=[T])
            for g in range(1, 4):
                S.op("dve", lambda e, g=g: e.scalar_tensor_tensor(out=sel, in0=lg[:, 4 + 8 * g:12 + 8 * g],
                                                                  scalar=goh[:, g:g + 1], in1=sel, op0=ALU.mult,
                                                                  op1=ALU.add), reads=[T], writes=[T])
            S.op("dve", lambda e: e.max(out=top8, in_=sel), reads=[T], writes=[T])
            S.op("dve", lambda e: e.tensor_scalar(out=mask2, in0=sel, scalar1=top8[:, 1:2], scalar2=None, op0=ALU.is_ge),
                 reads=[T], writes=[T])
            S.op("dve", lambda e: e.tensor_scalar(out=mask1, in0=sel, scalar1=top8[:, 0:1], scalar2=None, op0=ALU.is_ge),
                 reads=[T], writes=[T])
            S.op("dve", lambda e: e.tensor_tensor(out=dd, in0=top8[:, 1:2], in1=top8[:, 0:1], op=ALU.subtract),
                 reads=[T], writes=[T])
            S.op("act", lambda e: e.activation(out=ed, in_=dd, func=AF.Exp), reads=[T], writes=[T])
            S.op("dve", lambda e: e.tensor_scalar(out=ed, in0=ed, scalar1=1.0, scalar2=None, op0=ALU.add),
                 reads=[T], writes=[T])
            S.op("dve", lambda e: e.reciprocal(out=ed, in_=ed), reads=[T], writes=[T])
            S.op("dve", lambda e: e.tensor_tensor(out=RT[:, tt, 2:3], in0=g1, in1=ed, op=ALU.mult), reads=[T],
                 writes=["RT"])
            S.op("dve", lambda e: e.tensor_tensor(out=RT[:, tt, 3:4], in0=g1, in1=RT[:, tt, 2:3], op=ALU.subtract),
                 reads=[T, "RT"], writes=["RT"])
            for g in range(4):
                S.op("dve", lambda e, g=g: e.tensor_scalar(out=m12[:, 8 * g:8 * g + 8], in0=mask2, scalar1=goh[:, g:g + 1],
                                                           scalar2=None, op0=ALU.mult), reads=[T], writes=[M12T])
                S.op("dve", lambda e, g=g: e.tensor_scalar(out=m1f[:, 8 * g:8 * g + 8], in0=mask1, scalar1=goh[:, g:g + 1],
                                                           scalar2=None, op0=ALU.mult), reads=[T], writes=[T])
            S.op("dve", lambda e: e.tensor_tensor(out=m2f, in0=m12, in1=m1f, op=ALU.subtract),
                 reads=[T, M12T], writes=[T])
            S.op("dve", lambda e: e.tensor_copy(out=M1all[:, tt, :], in_=m1f), reads=[T], writes=["Mall"])
            S.op("dve", lambda e: e.tensor_copy(out=M2all[:, tt, :], in_=m2f), reads=[T], writes=["Mall"])

        def stage_d2b(tt, s):
            lg = lgs[:, s, :]
            sm = sm2[:, s, :]
            m12 = m12a[:, s, :]
            M12T = "m12_%d" % s
            gmax, ngmax, sume, g1 = sm[:, 40:41], sm[:, 41:42], sm[:, 42:43], sm[:, 43:44]
            goh = sm[:, 44:48]
            eg = sm[:, 48:52]
            sel = sm[:, 56:64]
            top8 = sm[:, 64:72]
            mask2 = sm[:, 72:80]
            mask1 = sm[:, 80:88]
            dd, ed = sm[:, 88:89], sm[:, 89:90]
            prod = sm[:, 96:128]
            m1f = sm[:, 128:160]
            m2f = sm[:, 160:192]
            T = "sm%d" % s
            kb = mbank()
            kbt = "pm%d" % kb
            S.op("pe", lambda e: e.matmul(pm[kb][:, 0:NE], lhsT=utri, rhs=m12, start=True, stop=False),
                 reads=[M12T, "consts"], writes=[kbt])
            S.op("pe", lambda e: e.matmul(pm[kb][:, 0:NE], lhsT=ones, rhs=srun[:, :], start=False, stop=True),
                 reads=["srun", "consts"], writes=[kbt])
            for kk, mf in ((0, m1f), (1, m2f)):
                S.op("dve", lambda e, mf=mf: e.tensor_tensor(out=prod, in0=mf, in1=pm[kb][:, 0:NE], op=ALU.mult),
                     reads=[T, kbt], writes=[T])
                S.op("dve", lambda e, kk=kk: e.tensor_reduce(out=RT[:, tt, kk:kk + 1], in_=prod, axis=AX.X, op=ALU.add),
                     reads=[T], writes=["RT"])
            S.op("dve", lambda e: e.tensor_tensor(out=srun[:, :], in0=srun[:, :], in1=m12, op=ALU.add),
                 reads=[M12T, "srun"], writes=["srun"])

        stage_a_act(xp_d[:, :], 0)
        setup_weights()
        stage_a_tr(0, 0)
        for g in range(2, 8):
            inproj_group(g, P, prefix=True)
        for s in range(SUB):
            stage_a(x_d[s * P:(s + 1) * P, :], s % 2, s * P)
        for g in range(8):
            inproj_group(g, NB)
        for k in range(8):
            S.op("sp", lambda e, k=k: e.dma_start(out=w_out[:, k, :], in_=w_out_d[k * P:(k + 1) * P, :]),
                 writes=["w_out"], dma="wout%d" % k)
        def a_act_block(bi):
            for s in range(SUB):
                r0 = bi * NB + s * P
                stage_a_act(x_d[r0:r0 + P, :], s % 2)

        if NBLK > 1:
            a_act_block(1)
        pending = [None]
        for i in range(NBLK):
            nxt = i + 1 < NBLK
            if nxt:
                for s in range(SUB):
                    stage_a_tr(s % 2, s * P)
            if pending[0] is not None:
                stage_drt(*pending[0])
                pending[0] = None
            if not nxt and i > 0:
                mix_part1()
                mix2_pre(0)
                mix_part1b()
                mix2_pre(1)
                mix2_mm()
                mix_part3()
                stage_d(i * SUB, 0)
                stage_d2((i - 1) * SUB, 0)
                stage_dtr(i * SUB, 0)
                stage_d(i * SUB + 1, 1)
                stage_d2((i - 1) * SUB + 1, 1)
                stage_drt(i * SUB, 0)
                stage_dtr(i * SUB + 1, 1)
                stage_d2b((i - 1) * SUB, 0)
                stage_d2b((i - 1) * SUB + 1, 1)
                pending[0] = (i * SUB + 1, 1)
                continue
            mix_part1()
            mix2_pre(0)
            if i > 0:
                stage_d2((i - 1) * SUB, 0)
            if nxt:
                inproj_group(0, NB)
                inproj_group(1, NB)
            mix_part1b()
            mix2_pre(1)
            if i > 0:
                stage_d2((i - 1) * SUB + 1, 1)
            if nxt:
                inproj_group(2, NB)
                inproj_group(3, NB)
            mix2_mm()
            if i + 2 < NBLK:
                a_act_block(i + 2)
            if nxt:
                inproj_group(4, NB)
            mix_part3()
            if i > 0:
                stage_d2b((i - 1) * SUB, 0)
            if nxt:
                inproj_group(5, NB)
            if i > 0:
                stage_d2b((i - 1) * SUB + 1, 1)
            assert SUB == 2
            stage_d(i * SUB, 0)
            if nxt:
                inproj_group(6, NB)
            stage_dtr(i * SUB, 0)
            stage_d(i * SUB + 1, 1)
            stage_drt(i * SUB, 0)
            if nxt:
                inproj_group(7, NB)
            stage_dtr(i * SUB + 1, 1)
            pending[0] = (i * SUB + 1, 1)
        stage_drt(*pending[0])
        for s in range(SUB):
            stage_d2((NBLK - 1) * SUB + s, s)
        for s in range(SUB):
            stage_d2b((NBLK - 1) * SUB + s, s)

        cb = mbank()
        cbt = "pm%d" % cb
        S.op("pe", lambda e: e.matmul(pm[cb][:, 0:NE], lhsT=ones, rhs=srun[:, :], start=True, stop=True),
             reads=["srun", "consts"], writes=[cbt])
        F = "hft1"
        cnt = fin[:, 0:32]
        tl = fin[:, 32:64]
        ca = fin[:, 64:96]
        cb2 = fin[:, 96:128]
        tstart = fin[:, 128:160]
        S.op("dve", lambda e: e.tensor_copy(out=cnt, in_=pm[cb][:, 0:NE]), reads=[cbt], writes=[F])
        thr32 = fin[:, 904:936]
        S.op("pool", lambda e: e.iota(thr32, pattern=[[P, 32]], base=0, channel_multiplier=0,
                                      allow_small_or_imprecise_dtypes=True), writes=[F])
        cmpA = hft[0][:, 0:NE * NTT].rearrange("p (e m) -> p e m", m=NTT)
        S.op("dve", lambda e: e.tensor_tensor(out=cmpA, in0=cnt.unsqueeze(2).broadcast_to([P, NE, NTT]),
                                              in1=thr32[:, 0:NTT].unsqueeze(1).broadcast_to([P, NE, NTT]), op=ALU.is_gt),
             reads=[F, "hft0"], writes=["hft0"])
        S.op("dve", lambda e: e.tensor_reduce(out=tl, in_=cmpA, axis=AX.X, op=ALU.add), reads=["hft0"], writes=[F])
        S.op("dve", lambda e: e.tensor_copy(out=ca, in_=tl), reads=[F], writes=[F])
        cur, oth = ca, cb2
        sh = 1
        while sh < NE:
            S.op("dve", lambda e, cur=cur, oth=oth, sh=sh: e.tensor_copy(out=oth[:, 0:sh], in_=cur[:, 0:sh]), reads=[F],
                 writes=[F])
            S.op("dve", lambda e, cur=cur, oth=oth, sh=sh: e.tensor_tensor(out=oth[:, sh:NE], in0=cur[:, sh:NE],
                                                                           in1=cur[:, 0:NE - sh], op=ALU.add),
                 reads=[F], writes=[F])
            cur, oth = oth, cur
            sh *= 2
        tend = cur
        S.op("dve", lambda e: e.tensor_tensor(out=tstart, in0=tend, in1=tl, op=ALU.subtract), reads=[F], writes=[F])
        tsl = fin[:, 160:160 + 2 * NTT].rearrange("p (t k) -> p t k", k=2)
        flr = fin[:, 256:256 + 2 * NTT].rearrange("p (t k) -> p t k", k=2)
        sres = fin[:, 384:384 + 2 * NTT].rearrange("p (t k) -> p t k", k=2)
        tmp = fin[:, 512:512 + 2 * NTT].rearrange("p (t k) -> p t k", k=2)
        prodA = hft[0][:, 0:NTT * NE].rearrange("p (t e) -> p t e", e=NE)
        tsb = tstart.unsqueeze(1).broadcast_to([P, NTT, NE])
        for kk, Mx in ((0, M1all), (1, M2all)):
            S.op("dve", lambda e, Mx=Mx: e.tensor_tensor(out=prodA, in0=Mx[:, :, :], in1=tsb, op=ALU.mult),
                 reads=[F, "Mall", "hft0"], writes=["hft0"])
            S.op("dve", lambda e, kk=kk: e.tensor_reduce(out=tsl[:, :, kk], in_=prodA, axis=AX.X, op=ALU.add),
                 reads=["hft0"], writes=[F])
        rank = RT[:, :, 0:2]
        hflat0 = hnT[:, :, :].rearrange("p k t -> p (k t)")
        cmpF = hflat0[:, 0:2 * NTT * (NTT - 1)].rearrange("p (t k m) -> p t k m", k=2, m=NTT - 1)
        S.op("dve", lambda e: e.tensor_tensor(
            out=cmpF, in0=rank.unsqueeze(3).broadcast_to([P, NTT, 2, NTT - 1]),
            in1=thr32[:, 1:NTT].unsqueeze(1).unsqueeze(1).broadcast_to([P, NTT, 2, NTT - 1]), op=ALU.is_ge),
            reads=["RT", F, "hnT"], writes=["hnT"])
        S.op("dve", lambda e: e.tensor_reduce(out=flr, in_=cmpF, axis=AX.X, op=ALU.add), reads=["hnT"], writes=[F])
        S.op("dve", lambda e: e.tensor_tensor(out=tsl, in0=tsl, in1=flr, op=ALU.add), reads=[F], writes=[F])
        S.op("dve", lambda e: e.scalar_tensor_tensor(out=sres, in0=flr, scalar=-float(P), in1=rank, op0=ALU.mult,
                                                     op1=ALU.add), reads=[F, "RT"], writes=[F])
        S.op("dve", lambda e: e.scalar_tensor_tensor(out=tmp, in0=tsl, scalar=float(P), in1=sres, op0=ALU.mult,
                                                     op1=ALU.add), reads=[F], writes=[F])
        S.op("dve", lambda e: e.tensor_copy(out=yidx[:, :, :], in_=tmp), reads=[F], writes=["yidx"])
        jio = fin[:, 640:640 + NST]
        ej = fin[:, 768:768 + NST]
        pio = fin[:, 900:901]
        S.op("pool", lambda e: e.iota(jio, pattern=[[1, NST]], base=0, channel_multiplier=0,
                                      allow_small_or_imprecise_dtypes=True), writes=[F])
        S.op("pool", lambda e: e.iota(pio, pattern=[[0, 1]], base=0, channel_multiplier=1,
                                      allow_small_or_imprecise_dtypes=True), writes=[F])
        for c0 in range(0, NST, 32):
            nj = min(32, NST - c0)
            cmpE = hft[0][:, 0:nj * NE].rearrange("p (j e) -> p j e", e=NE)
            S.op("dve", lambda e, c0=c0, nj=nj, cmpE=cmpE: e.tensor_tensor(
                out=cmpE, in0=jio[:, c0:c0 + nj].unsqueeze(2).broadcast_to([P, nj, NE]),
                in1=tend.unsqueeze(1).broadcast_to([P, nj, NE]), op=ALU.is_ge), reads=[F, "hft0"], writes=["hft0"])
            S.op("dve", lambda e, c0=c0, nj=nj, cmpE=cmpE: e.tensor_reduce(out=ej[:, c0:c0 + nj], in_=cmpE, axis=AX.X,
                                                                           op=ALU.add), reads=["hft0"], writes=[F])
        S.op("dve", lambda e: e.tensor_scalar(out=ej, in0=ej, scalar1=float(NE - 1), scalar2=float(P), op0=ALU.min,
                                              op1=ALU.mult), reads=[F], writes=[F])
        S.op("dve", lambda e: e.tensor_scalar(out=ej, in0=ej, scalar1=pio, scalar2=None, op0=ALU.add),
             reads=[F], writes=[F])
        S.op("dve", lambda e: e.tensor_copy(out=idxw[:, :], in_=ej), reads=[F], writes=["idxw"])
        hflat = hnT[:, :, :].rearrange("p k t -> p (k t)")
        OS = [hflat[:, 0:P], hflat[:, P:2 * P]]
        OT = [hflat[:, 256:256 + NST], hflat[:, 256 + NST:256 + 2 * NST]]
        io128 = hflat[:, 512:512 + P]
        tokf = hflat[:, 640:640 + NTT]
        S.op("pool", lambda e: e.iota(io128, pattern=[[1, P]], base=0, channel_multiplier=0,
                                      allow_small_or_imprecise_dtypes=True), writes=["hnT"])
        S.op("pool", lambda e: e.iota(tokf, pattern=[[P, NTT]], base=0, channel_multiplier=1,
                                      allow_small_or_imprecise_dtypes=True), writes=["hnT"])
        tb = mbank()
        tbt = "pm%d" % tb
        assert 1536 + 4 * NST <= 2048
        OT2 = [hflat[:, 1536:1536 + 2 * NST].rearrange("p (k j) -> p k j", k=2),
               hflat[:, 1536 + 2 * NST:1536 + 4 * NST].rearrange("p (k j) -> p k j", k=2)]
        OS2 = [hflat[:, 1024:1024 + 2 * P].rearrange("p (k s) -> p k s", k=2),
               hflat[:, 1024 + 2 * P:1024 + 4 * P].rearrange("p (k s) -> p k s", k=2)]
        for tt in range(NTT):
            sl = tt % 2
            for kk in range(2):
                S.op("dve", lambda e, sl=sl, tt=tt, kk=kk: e.tensor_scalar(
                    out=OS2[sl][:, kk, :], in0=io128, scalar1=sres[:, tt, kk:kk + 1], scalar2=tokf[:, tt:tt + 1],
                    op0=ALU.is_equal, op1=ALU.mult), reads=[F, "hnT"], writes=["OS%d" % sl])
            S.op("dve", lambda e, sl=sl, tt=tt: e.tensor_tensor(
                out=OT2[sl], in0=jio.unsqueeze(1).broadcast_to([P, 2, NST]),
                in1=tsl[:, tt, :].unsqueeze(2).broadcast_to([P, 2, NST]), op=ALU.is_equal),
                reads=[F, "hnT"], writes=["OT%d" % sl])
            for kk in range(2):
                n = 2 * tt + kk
                S.op("pe", lambda e, sl=sl, kk=kk, n=n: e.matmul(pm[tb][:, 0:NST], lhsT=OS2[sl][:, kk, :],
                                                                 rhs=OT2[sl][:, kk, :], start=(n == 0),
                                                                 stop=(n == 2 * NTT - 1)),
                     reads=["OS%d" % sl, "OT%d" % sl], writes=[tbt])
        S.op("dve", lambda e: e.tensor_copy(out=toki[:, :], in_=pm[tb][:, 0:NST]), reads=[tbt],
             writes=["toki", "hf_d", "h1_d"])

        NSL = 4
        wof = w_out[:, :, :].rearrange("p k d -> p (k d)")
        wgu = [big[:, 0:4096], big[:, 4096:8192], big[:, 8192:12288], wof[:, 4096:8192]]
        wdn = [big[:, 12288:14336], big[:, 14336:16384], wof[:, 0:2048], wof[:, 2048:4096]]
        xg = [xt[0], xt[1], hft[0]]
        xgt = ["xt0", "xt1", "hft0"]
        NXS = 3

        def gather_x(j):
            sl = j % NXS
            S.op("pool", lambda e: e.indirect_dma_start(
                out=xg[sl][:, :], out_offset=None, in_=hf_d[:, :],
                in_offset=bass.IndirectOffsetOnAxis(ap=toki[:, j:j + 1], axis=0)),
                reads=["toki", "hf_d"], writes=[xgt[sl]], dma="xg%d" % sl)

        def gather_gu(j):
            sl = j % NSL
            first = []
            S.op("pool", lambda e: e.indirect_dma_start(
                out=wgu[sl], out_offset=None, in_=wgu_d[:, :],
                in_offset=bass.IndirectOffsetOnAxis(ap=idxw[:, j:j + 1], axis=0)),
                reads=["idxw"], writes=["wgu%d" % sl] + first, dma="wgu%d" % sl)

        def gather_dn(j):
            sl = j % NSL
            first = []
            S.op("pool", lambda e: e.indirect_dma_start(
                out=wdn[sl], out_offset=None, in_=wdn_d[:, :],
                in_offset=bass.IndirectOffsetOnAxis(ap=idxw[:, j:j + 1], axis=0)),
                reads=["idxw"], writes=["wdn%d" % sl] + first, dma="wdn%d" % sl)

        def xt_b(j):
            sl = j % NXS
            for h in range(2):
                b = mbank()
                for kk in range(4):
                    k = h * 4 + kk
                    S.op("pe", lambda e, b=b, kk=kk, k=k: e.transpose(out=pm[b][:, kk * P:(kk + 1) * P],
                                                                      in_=xg[sl][:, k * P:(k + 1) * P], identity=ident),
                         reads=[xgt[sl], "consts"], writes=["pm%d" % b])
                S.op("act" if h == 0 else "dve", lambda e, b=b, h=h: (e.copy if h == 0 else e.tensor_copy)(
                    out=hfT[:, h * 4:(h + 1) * 4, :], in_=pm[b][:, :].rearrange("p (k t) -> p k t", k=4)),
                    reads=["pm%d" % b], writes=["hfT"])

        def gu_b(j):
            sl = j % NSL
            for k in range(8):
                S.op("pe", lambda e, k=k: e.matmul(pin[0][:, :], lhsT=hfT[:, k, :], rhs=wgu[sl][:, k * 512:(k + 1) * 512],
                                                   start=(k == 0), stop=(k == 7)),
                     reads=["hfT", "wgu%d" % sl], writes=["pin0"])
            S.op("act", lambda e: e.activation(out=sgS[:, :], in_=pin[0][:, 0:256], func=AF.Silu), reads=["pin0"],
                 writes=["sgS"])
            S.op("dve", lambda e: e.tensor_tensor(out=aS[:, :], in0=sgS[:, :], in1=pin[0][:, 256:512], op=ALU.mult),
                 reads=["sgS", "pin0"], writes=["aS"])

        def at_b(j):
            for c in range(2):
                S.op("pe", lambda e, c=c: e.transpose(out=pin[1][:, c * P:(c + 1) * P], in_=aS[:, c * P:(c + 1) * P],
                                                      identity=ident), reads=["aS", "consts"], writes=["pin1"])
            S.op("act", lambda e: e.copy(out=aT[:, :, :], in_=pin[1][:, 0:2 * P].rearrange("p (c t) -> p c t", c=2)),
                 reads=["pin1"], writes=["aT"])

        def down_b(j):
            sl = j % NSL
            for half in range(2):
                for c in range(2):
                    S.op("pe", lambda e, half=half, c=c: e.matmul(
                        po[:, half * 512:(half + 1) * 512], lhsT=aT[:, c, :],
                        rhs=wdn[sl][:, c * 1024 + half * 512:c * 1024 + (half + 1) * 512],
                        start=(c == 0), stop=(c == 1)), reads=["aT", "wdn%d" % sl], writes=["po"])
            ys = j % 2
            yb, ytok = x2[ys], "x2%d" % ys
            S.op("act", lambda e: e.copy(out=yb[:, 0:512], in_=po[:, 0:512]), reads=["po"], writes=[ytok])
            S.op("dve", lambda e: e.tensor_copy(out=yb[:, 512:1024], in_=po[:, 512:1024]), reads=["po"], writes=[ytok])
            S.op("sp", lambda e: e.dma_start(out=y_d[j * P:(j + 1) * P, :], in_=yb[:, :]), reads=[ytok, "y_d"],
                 dma="ys%d" % ys)

        S.op("pool", lambda e: e.memset(bar[:, 0:1], 0.0), writes=WIN + ["w_out", "bar"])
        for j in range(NSL):
            gather_gu(j)
            gather_dn(j)
        for j in range(NXS):
            gather_x(j)
        xt_b(0)
        gu_b(0)
        if NSL < NST:
            gather_gu(NSL)
        for j in range(NST):
            if j + 1 < NST:
                xt_b(j + 1)
            if j >= 1:
                down_b(j - 1)
                if j - 1 + NSL < NST:
                    gather_dn(j - 1 + NSL)
            at_b(j)
            if j + 1 < NST:
                gu_b(j + 1)
                if j + 1 + NSL < NST:
                    gather_gu(j + 1 + NSL)
            if j + NXS < NST:
                gather_x(j + NXS)
        down_b(NST - 1)

        fn_row = yT[:, :, :].rearrange("p k t -> p (k t)")[:, 0:D]
        S.op("sp", lambda e: e.dma_start(out=fn_row, in_=rowv_d[:, D:2 * D]), writes=YT_ALL + ["y_d"], dma="c5")
        wo = [w_out[:, k, :] for k in range(8)]
        NCS = 4
        cy1 = [xt[0][:, :], xt[1][:, :], wo[0], wo[1]]
        cy2 = [x2[0][:, :], x2[1][:, :], wo[2], wo[3]]
        chb = [hft[0][:, :], hft[1][:, :], wo[4], wo[5]]
        cout = [wo[6], wo[7]]
        first_c = set()

        def c_loads(tt):
            sl = tt % NCS
            r0 = tt * P
            extra = []
            first_c.add(sl)
            S.op("pool", lambda e: e.indirect_dma_start(
                out=cy1[sl], out_offset=None, in_=y_d[:, :],
                in_offset=bass.IndirectOffsetOnAxis(ap=yidx[:, tt, 0:1], axis=0)),
                reads=["yidx", "y_d"], writes=["cy1%d" % sl] + (["xt%d" % sl] if sl < 2 else extra), dma="cy1%d" % sl)
            S.op("pool", lambda e: e.indirect_dma_start(
                out=cy2[sl], out_offset=None, in_=y_d[:, :],
                in_offset=bass.IndirectOffsetOnAxis(ap=yidx[:, tt, 1:2], axis=0)),
                reads=["yidx", "y_d"], writes=["cy2%d" % sl] + (["x2%d" % sl] if sl < 2 else extra), dma="cy2%d" % sl)
            S.op("sp", lambda e: e.dma_start(out=chb[sl], in_=h1_d[r0:r0 + P, :]), reads=["h1_d", "barc"],
                 writes=["chb%d" % sl] + (["hft%d" % sl] if sl < 2 else extra), dma="ch%d" % sl)

        S.op("pool", lambda e: e.memset(bar[:, 1:2], 0.0),
             writes=["wgu%d" % i for i in range(4)] + ["wdn%d" % i for i in range(4)] + ["barc"])
        c_loads(0)
        c_loads(1)
        for tt in range(NTT):
            sl = tt % NCS
            r0 = tt * P
            y1, y1t = cy1[sl], "cy1%d" % sl
            y2, y2t = cy2[sl], "cy2%d" % sl
            hb, hbt = chb[sl], "chb%d" % sl
            ob, obt = cout[tt % 2], "cout%d" % (tt % 2)
            if tt + 2 < NTT:
                c_loads(tt + 2)
            S.op("dve", lambda e, tt=tt, y1=y1, hb=hb: e.scalar_tensor_tensor(
                out=hb, in0=y1, scalar=RT[:, tt, 2:3], in1=hb, op0=ALU.mult, op1=ALU.add),
                reads=[y1t, hbt, "RT"], writes=[hbt])
            S.op("dve", lambda e, tt=tt, y2=y2, hb=hb: e.scalar_tensor_tensor(
                out=hb, in0=y2, scalar=RT[:, tt, 3:4], in1=hb, op0=ALU.mult, op1=ALU.add),
                reads=[y2t, hbt, "RT"], writes=[hbt])
            c = sscol()
            sst = "ss%d" % c
            S.op("act", lambda e, hb=hb, c=c: e.activation(out=junk[:, :], in_=hb, func=AF.Square,
                                                           accum_out=ssb[:, c:c + 1]), reads=[hbt],
                 writes=["junk", sst])
            rstd_ops(ssb[:, c:c + 1], ssb[:, c:c + 1], float(D), [sst], [sst])
            first_o = []
            S.op("dve", lambda e, hb=hb, c=c, ob=ob: e.scalar_tensor_tensor(
                out=ob, in0=hb, scalar=ssb[:, c:c + 1], in1=fn_row, op0=ALU.mult, op1=ALU.mult),
                reads=[hbt, sst, "barc"] + YT_ALL, writes=[obt] + first_o)
            S.op("act", lambda e, ob=ob, r0=r0: e.dma_start(out=out_d[r0:r0 + P, :], in_=ob), reads=[obt],
                 dma="os%d" % (tt % 2))
        S.emit()
    return nc


def prep_shared(inp):
    f = np.float32
    w_in = np.ascontiguousarray(inp["w_in"][0], f)
    w_out = np.ascontiguousarray(inp["w_out"][0], f)
    wr = np.ascontiguousarray(np.concatenate([inp["w_router_group"][0], inp["w_router_expert"][0]], axis=1), f)
    poolw = np.ascontiguousarray(np.transpose(inp["pool_w"][0], (1, 0, 2)).reshape(P, 4 * P), f)
    colv = np.zeros((P, 32), f)
    colv[:, 0:8] = inp["norm_mix"][0].reshape(8, P).T
    cw = inp["conv_w"][0]
    for ch in range(4):
        for tap in range(3):
            colv[:, 8 + ch * 3 + tap] = cw[tap, ch * P:(ch + 1) * P]
    colv[:, 20:24] = inp["norm_conv_out"][0].reshape(4, P).T
    colv[:, 24:28] = inp["pool_scale"][0].reshape(4, P).T
    colv[:, 28:32] = inp["norm_pool_out"][0].reshape(4, P).T
    rowv = np.empty((P, 2 * D), f)
    rowv[:, 0:D] = inp["norm_ffn"][0][None, :]
    rowv[:, D:] = inp["final_norm"][None, :]
    consts = np.zeros((P, 3 * P), f)
    consts[:, 0:P] = np.eye(P, dtype=f)
    consts[:, P:2 * P] = np.triu(np.ones((P, P), f), 1)
    consts[:, 2 * P:] = 1.0
    wg, wu, wd = inp["w_gate"][0], inp["w_up"][0], inp["w_down"][0]
    wall_gu = np.empty((NE, P, 8, 512), f)
    wall_gu[:, :, :, 0:256] = wg.reshape(NE, 8, P, 256).transpose(0, 2, 1, 3)
    wall_gu[:, :, :, 256:512] = wu.reshape(NE, 8, P, 256).transpose(0, 2, 1, 3)
    wall_dn = np.ascontiguousarray(wd.reshape(NE, 2, P, D).transpose(0, 2, 1, 3))
    return dict(w_in=w_in, w_out=w_out, wr=wr, poolw=poolw, colv=colv, rowv=rowv, consts=consts,
                wall_gu=wall_gu.reshape(NE * P, 4096), wall_dn=wall_dn.reshape(NE * P, 2048))


def core_inputs(x, meta, shared, NT):
    B, Sq, _ = x.shape
    nh = Sq // NT
    maps = []
    for c in range(B * nh):
        b, h = divmod(c, nh)
        xm = np.ascontiguousarray(x[b, h * NT:(h + 1) * NT], np.float32)
        xp = np.zeros((P, D), np.float32)
        xp[P - HALO:] = meta if h == 0 else x[b, h * NT - HALO:h * NT]
        m = dict(shared)
        m["x"] = xm
        m["xp"] = xp
        maps.append(m)
    return maps


_NC_CACHE = {}


def kernel(x, meta_tokens, norm_mix, w_in, conv_w, norm_conv_out, pool_w, pool_scale, norm_pool_out, w_out,
           norm_ffn, w_router_group, w_router_expert, w_gate, w_up, w_down, final_norm):
    inp = dict(norm_mix=norm_mix, w_in=w_in, conv_w=conv_w, norm_conv_out=norm_conv_out, pool_w=pool_w,
               pool_scale=pool_scale, norm_pool_out=norm_pool_out, w_out=w_out, norm_ffn=norm_ffn,
               w_router_group=w_router_group, w_router_expert=w_router_expert, w_gate=w_gate, w_up=w_up,
               w_down=w_down, final_norm=final_norm)
    inp = {k: np.asarray(v, np.float32) for k, v in inp.items()}
    x = np.asarray(x, np.float32)
    meta = np.asarray(meta_tokens, np.float32)
    B, Sq, _ = x.shape
    NT = B * Sq // NCORES
    shared = prep_shared(inp)
    maps = core_inputs(x, meta, shared, NT)
    if NT not in _NC_CACHE:
        _NC_CACHE[NT] = build(NT)
    res = run_bass_kernel_spmd(_NC_CACHE[NT], maps, core_ids=list(range(NCORES)))
    out = np.empty((B, Sq, D), np.float32)
    nh = Sq // NT
    for c in range(NCORES):
        b, h = divmod(c, nh)
        out[b, h * NT:(h + 1) * NT] = res.results[c]["out"]
    return out
```
